# Optimizing a Trainium2 kernel written in Bass

```python
import jax
import jax.numpy as jnp
from jax import lax
import numpy as np

D_MODEL = 1024
BATCH = 2
SEQ = 8192
DEPTH = 2

GRID_W = 64
CTX_LEN = 256

N_EVEN = (DEPTH + 1) // 2
N_ODD = DEPTH // 2
DEEPNORM_ALPHA = (2.0 * DEPTH) ** 0.25
DEEPNORM_BETA = (8.0 * DEPTH) ** -0.25
LN_EPS = 1e-5
NEG_INF = -1e30
F32 = jnp.float32

NA_HEAD_DIM = 64
NA_WIDTH = D_MODEL // 2
NA_HEADS = NA_WIDTH // NA_HEAD_DIM
NA_WIN_ROWS = 8
NA_WIN_COLS = 16
NA_SCALE = NA_HEAD_DIM ** -0.5

RW_HEAD_DIM = 64
RW_WIDTH = D_MODEL // 2
RW_HEADS = RW_WIDTH // RW_HEAD_DIM
RW_DECAY_LORA = 32
RW_AAA_LORA = 32
RW_GATE_LORA = 96
RW_GN_EPS = 64e-5

ML_HEADS = 8
ML_V_DIM = D_MODEL // ML_HEADS
ML_QK_DIM = ML_V_DIM // 2
ML_WIDTH = ML_HEADS * ML_V_DIM
ML_CHUNK = 128
ML_NORM_EPS = 1e-6
ROPE_BASE = 10000.0

N_EXPERTS = 256
TOP_K = 8
N_GROUPS = 8
TOPK_GROUPS = 4
EXPERT_FF = 256
SHARED_FF = 256
ROUTED_SCALE = 2.5
MOE_BLOCK = 128

EVEN_LAYOUT = (
    ('na_q', NA_WIDTH), ('na_k', NA_WIDTH), ('na_v', NA_WIDTH),
    ('rw_r', RW_WIDTH), ('rw_k', RW_WIDTH), ('rw_v', RW_WIDTH),
    ('rw_wf', RW_DECAY_LORA), ('rw_wb', RW_DECAY_LORA),
    ('rw_af', RW_AAA_LORA), ('rw_ab', RW_AAA_LORA), ('rw_g', RW_GATE_LORA),
)
EVEN_COLS = sum(w for _, w in EVEN_LAYOUT)
RW_SHIFT_COLS = sum(w for n, w in EVEN_LAYOUT if n.startswith('rw_'))
ODD_LAYOUT = (
    ('ml_q', ML_HEADS * ML_QK_DIM), ('ml_k', ML_HEADS * ML_QK_DIM),
    ('ml_v', ML_WIDTH), ('ml_o', ML_WIDTH),
    ('ml_if', ML_HEADS), ('ml_ib', ML_HEADS), ('ml_ff', ML_HEADS), ('ml_fb', ML_HEADS),
)
ODD_COLS = sum(w for _, w in ODD_LAYOUT)
EVEN_CTX_STATE_COLS = ('na_k', 'na_v', 'rw_k', 'rw_v', 'rw_wf', 'rw_wb', 'rw_af', 'rw_ab')
ODD_CTX_STATE_COLS = ('ml_k', 'ml_v', 'ml_if', 'ml_ib', 'ml_ff', 'ml_fb')

kernel_name = 'hybrid_natten_rwkv7_mlstm_moe_dit'


def _offsets(layout, prefix=''):
    offs, o = {}, 0
    for name, width in layout:
        if name.startswith(prefix):
            offs[name] = (o, width)
            o += width
    return offs


def project(h, w, layout, names):
    offs = _offsets(layout)
    if len(names) == len(layout):
        y = jnp.einsum('btd,de->bte', h, w)
        return {n: y[..., offs[n][0]:offs[n][0] + offs[n][1]] for n in names}
    return {n: jnp.einsum('btd,de->bte', h, w[:, offs[n][0]:offs[n][0] + offs[n][1]]) for n in names}


def layer_norm(x, g, b):
    xf = x.astype(F32)
    mu = xf.mean(-1, keepdims=True)
    var = jnp.square(xf - mu).mean(-1, keepdims=True)
    return ((xf - mu) * lax.rsqrt(var + LN_EPS) * g + b).astype(x.dtype)


def modulate(h, shift, scale):
    return h * (1.0 + scale) + shift


def centred_shift(p, mu):
    zero = jnp.zeros_like(p[:, :1])
    prev = jnp.concatenate([zero, p[:, :-1]], 1)
    nxt = jnp.concatenate([p[:, 1:], zero], 1)
    return p + mu * (0.5 * (prev + nxt) - p)


def axial_rope(z):
    T, dh = z.shape[1], z.shape[-1]
    half = dh // 2
    nf = half // 2
    t = jnp.arange(T)
    row = (t // GRID_W).astype(F32)
    col = (t % GRID_W).astype(F32)
    inv = ROPE_BASE ** (-jnp.arange(nf, dtype=F32) / nf)

    def rot(u, pos):
        ang = pos[:, None] * inv[None, :]
        cos = jnp.cos(ang)[None, :, None, :]
        sin = jnp.sin(ang)[None, :, None, :]
        u1, u2 = u[..., :nf], u[..., nf:]
        return jnp.concatenate([u1 * cos - u2 * sin, u1 * sin + u2 * cos], -1)

    return jnp.concatenate([rot(z[..., :half], row), rot(z[..., half:], col)], -1).astype(z.dtype)


def neighbourhood_attention(q, k, v, k_ctx, v_ctx, rpb):
    B, N, H, dh = q.shape
    rows = N // GRID_W
    kh = min(NA_WIN_ROWS, rows)
    kw = NA_WIN_COLS
    grid = lambda z: z.reshape(B, rows, GRID_W, H, dh)
    qg, kg, vg = grid(q), grid(k), grid(v)
    r_idx = jnp.arange(rows)
    row_start = jnp.clip(r_idx - kh // 2, 0, rows - kh)
    band_rows = row_start[:, None] + jnp.arange(kh)[None, :]
    k_band = kg[:, band_rows]
    v_band = vg[:, band_rows]
    j_idx = jnp.arange(GRID_W)
    col_start = jnp.clip(j_idx - kw // 2, 0, GRID_W - kw)
    col_in = (j_idx[None, :] >= col_start[:, None]) & (j_idx[None, :] < col_start[:, None] + kw)
    row_off = band_rows - r_idx[:, None] + (NA_WIN_ROWS - 1)
    col_off = jnp.clip(j_idx[None, :] - j_idx[:, None], -(kw - 1), kw - 1) + (kw - 1)
    bias = rpb.astype(F32)[:, row_off[:, None, :, None], col_off[None, :, None, :]]
    bias = jnp.where(col_in[None, None, :, None, :], bias, NEG_INF)
    s_loc = jnp.einsum('brjhd,brachd->bhrjac', qg, k_band).astype(F32) + bias[None]
    s_ctx = jnp.einsum('brjhd,bchd->bhrjc', qg, k_ctx).astype(F32)
    n_loc = kh * GRID_W
    p = jax.nn.softmax(jnp.concatenate([s_loc.reshape(B, H, rows, GRID_W, n_loc), s_ctx], -1), axis=-1).astype(v.dtype)
    p_loc = p[..., :n_loc].reshape(B, H, rows, GRID_W, kh, GRID_W)
    out = (jnp.einsum('bhrjac,brachd->brjhd', p_loc, v_band)
           + jnp.einsum('bhrjc,bchd->brjhd', p[..., n_loc:], v_ctx))
    return out.reshape(B, N, H * dh)


def ctx_attention(q, k, v):
    s = jnp.einsum('bqhd,bkhd->bhqk', q, k).astype(F32)
    p = jax.nn.softmax(s, axis=-1).astype(v.dtype)
    return jnp.einsum('bhqk,bkhd->bqhd', p, v)


def rwkv_prep(t, w0, w_up, a0, a_up, k_k, k_a):
    B, T = t['rw_k'].shape[:2]
    heads = lambda z: z.reshape(B, T, RW_HEADS, RW_HEAD_DIM).astype(F32)
    k = t['rw_k'].astype(F32)
    kk = heads(k * k_k)
    kk = kk / jnp.maximum(jnp.linalg.norm(kk, axis=-1, keepdims=True), 1e-12)
    dirs = []
    for d, (wn, an) in enumerate((('rw_wf', 'rw_af'), ('rw_wb', 'rw_ab'))):
        w_log = -jax.nn.softplus(-(w0[d] + jnp.tanh(t[wn].astype(F32)) @ w_up[d])) - 0.5
        decay = jnp.exp(-jnp.exp(w_log))
        a = jax.nn.sigmoid(a0[d] + t[an].astype(F32) @ a_up[d])
        k_d = k * (1.0 + (a - 1.0) * k_a)
        dirs.append((heads(decay), heads(a), heads(k_d)))
    return kk, heads(t['rw_v']), dirs


def rwkv_scan(s0, decay, a, k, kk, v, r, reverse):
    seq = lambda z: jnp.moveaxis(z, 1, 0)
    xs = (seq(decay), seq(-kk), seq(kk * a), seq(k), seq(v))
    if r is not None:
        xs = xs + (seq(r),)

    def step(s, inp):
        s = (s * inp[0][:, :, None, :]
             + jnp.einsum('bhvk,bhk->bhv', s, inp[1])[..., None] * inp[2][:, :, None, :]
             + inp[4][..., None] * inp[3][:, :, None, :])
        return s, (jnp.einsum('bhvk,bhk->bhv', s, inp[5]) if len(inp) == 6 else None)

    s, ys = lax.scan(step, s0, xs, reverse=reverse)
    return s, (None if r is None else jnp.moveaxis(ys, 0, 1))


def rwkv_readout(y, r, v, dirs, g_low, r_k, g_up, gn_g, gn_b):
    B, T = y.shape[:2]
    mu = y.mean(-1, keepdims=True)
    var = jnp.square(y - mu).mean(-1, keepdims=True)
    yn = ((y - mu) * lax.rsqrt(var + RW_GN_EPS) * gn_g.reshape(RW_HEADS, RW_HEAD_DIM)
          + gn_b.reshape(RW_HEADS, RW_HEAD_DIM))
    bonus = (jnp.sum(r * dirs[0][2] * r_k, -1, keepdims=True)
             + jnp.sum(r * dirs[1][2] * r_k, -1, keepdims=True)) * v
    gate = jax.nn.sigmoid(g_low.astype(F32)) @ g_up
    return (yn + bonus).reshape(B, T, RW_WIDTH) * gate


def ml_prep(t, gate_b, rope, need_q):
    B, T = t['ml_k'].shape[:2]
    heads = lambda z, dh: z.reshape(B, T, ML_HEADS, dh).astype(F32)
    k = heads(t['ml_k'], ML_QK_DIM)
    q = heads(t['ml_q'], ML_QK_DIM) * ML_QK_DIM ** -0.5 if need_q else None
    if rope:
        k = axial_rope(k)
        q = axial_rope(q)
    v = heads(t['ml_v'], ML_V_DIM)
    gb = gate_b.astype(F32)
    bht = lambda z: z.astype(F32).transpose(0, 2, 1)
    ig = (bht(t['ml_if'] + gb[0]), bht(t['ml_ib'] + gb[1]))
    lf = (jax.nn.log_sigmoid(bht(t['ml_ff'] + gb[2])), jax.nn.log_sigmoid(bht(t['ml_fb'] + gb[3])))
    bhtd = lambda z: None if z is None else z.transpose(0, 2, 1, 3)
    return bhtd(q), bhtd(k), bhtd(v), ig, lf


def ml_chunk_states(k, v, ig, lf, state0):
    B, H, T, dk = k.shape
    dv = v.shape[-1]
    L = min(ML_CHUNK, T)
    nc = T // L
    kc = k.reshape(B, H, nc, L, dk)
    vc = v.reshape(B, H, nc, L, dv)
    b = jnp.cumsum(lf.reshape(B, H, nc, L), -1)
    b_end = b[..., -1]
    g = b_end[..., None] - b + ig.reshape(B, H, nc, L)
    m_chunk = g.max(-1)
    wgt = jnp.exp(g - m_chunk[..., None])
    kv = jnp.einsum('bhnl,bhnlk,bhnlv->bhnkv', wgt, kc, vc)
    ks = jnp.einsum('bhnl,bhnlk->bhnk', wgt, kc)

    def step(state, inp):
        c_mem, n_mem, m = state
        be, mc, kv_n, ks_n = inp
        m_new = jnp.maximum(be + m, mc)
        fa = jnp.exp(be + m - m_new)
        fb = jnp.exp(mc - m_new)
        c_new = fa[..., None, None] * c_mem + fb[..., None, None] * kv_n
        n_new = fa[..., None] * n_mem + fb[..., None] * ks_n
        return (c_new, n_new, m_new), state

    xs = tuple(jnp.moveaxis(z, 2, 0) for z in (b_end, m_chunk, kv, ks))
    final, starts = lax.scan(step, state0, xs)
    return tuple(jnp.moveaxis(z, 0, 2) for z in starts), final


def ml_chunk_outputs(q, k, v, ig, lf, starts):
    B, H, T, dk = q.shape
    dv = v.shape[-1]
    L = min(ML_CHUNK, T)
    nc = T // L
    qc = q.reshape(B, H, nc, L, dk)
    kc = k.reshape(B, H, nc, L, dk)
    vc = v.reshape(B, H, nc, L, dv)
    b = jnp.cumsum(lf.reshape(B, H, nc, L), -1)
    c0, n0, m0 = starts
    causal = jnp.tril(jnp.ones((L, L), bool))
    dlog = jnp.where(causal, b[..., :, None] - b[..., None, :] + ig.reshape(B, H, nc, L)[..., None, :], NEG_INF)
    inter = b + m0[..., None]
    m = jnp.maximum(dlog.max(-1), inter)
    dw = jnp.exp(dlog - m[..., None])
    iw = jnp.exp(inter - m)
    s = jnp.einsum('bhntd,bhnsd->bhnts', qc, kc) * dw
    num = jnp.einsum('bhnts,bhnsv->bhntv', s, vc) + iw[..., None] * jnp.einsum('bhntd,bhndv->bhntv', qc, c0)
    den = s.sum(-1) + iw * jnp.einsum('bhntd,bhnd->bhnt', qc, n0)
    h = num / jnp.maximum(jnp.abs(den), jnp.exp(-m))[..., None]
    return h.reshape(B, H, T, dv)


def ml_readout(h, o, norm_g):
    B, H, T, dv = h.shape
    hn = h * lax.rsqrt(jnp.mean(h * h, -1, keepdims=True) + ML_NORM_EPS)
    hn = hn.transpose(0, 2, 1, 3).reshape(B, T, H * dv) * norm_g
    return hn * jax.nn.sigmoid(o.astype(F32))


def even_mixer(a_lat, a_ctx, w_in, w_out, rpb, mu, w0, w_up, a0, a_up, g_up, k_k, k_a, r_k, gn_g, gn_b, need_ctx):
    names = tuple(n for n, _ in EVEN_LAYOUT)
    shift_offs = _offsets(EVEN_LAYOUT, 'rw_')

    def prep(h, cols):
        t = project(h, w_in, EVEN_LAYOUT, cols)
        for n in cols:
            if n in shift_offs:
                o, wd = shift_offs[n]
                t[n] = centred_shift(t[n], mu[o:o + wd])
        return t

    t_lat = prep(a_lat, names)
    t_ctx = prep(a_ctx, names if need_ctx else EVEN_CTX_STATE_COLS)
    B, N = a_lat.shape[:2]
    C = a_ctx.shape[1]
    na_h = lambda z: z.reshape(z.shape[0], z.shape[1], NA_HEADS, NA_HEAD_DIM)
    rw_h = lambda z: z.reshape(z.shape[0], z.shape[1], RW_HEADS, RW_HEAD_DIM).astype(F32)
    k_c, v_c = na_h(t_ctx['na_k']), na_h(t_ctx['na_v'])
    na_lat = neighbourhood_attention(na_h(t_lat['na_q']) * NA_SCALE, na_h(t_lat['na_k']),
                                     na_h(t_lat['na_v']), k_c, v_c, rpb)
    kk_c, vr_c, dirs_c = rwkv_prep(t_ctx, w0, w_up, a0, a_up, k_k, k_a)
    kk_l, vr_l, dirs_l = rwkv_prep(t_lat, w0, w_up, a0, a_up, k_k, k_a)
    r_l = rw_h(t_lat['rw_r'])
    r_c = rw_h(t_ctx['rw_r']) if need_ctx else None
    s0 = jnp.zeros((B, RW_HEADS, RW_HEAD_DIM, RW_HEAD_DIM), F32)
    y_l, y_c = [], []
    for d in range(2):
        s_ctx, yc = rwkv_scan(s0, *dirs_c[d], kk_c, vr_c, r_c, d == 1)
        _, yl = rwkv_scan(s_ctx, *dirs_l[d], kk_l, vr_l, r_l, d == 1)
        y_l.append(yl)
        y_c.append(yc)
    rw_lat = rwkv_readout(y_l[0] + y_l[1], r_l, vr_l, dirs_l, t_lat['rw_g'], r_k, g_up, gn_g, gn_b)
    y_lat = jnp.einsum('btd,de->bte', jnp.concatenate([na_lat.astype(F32), rw_lat], -1), w_out).astype(a_lat.dtype)
    if not need_ctx:
        return y_lat, None
    na_ctx = ctx_attention(na_h(t_ctx['na_q']) * NA_SCALE, k_c, v_c).reshape(B, C, NA_WIDTH)
    rw_ctx = rwkv_readout(y_c[0] + y_c[1], r_c, vr_c, dirs_c, t_ctx['rw_g'], r_k, g_up, gn_g, gn_b)
    y_ctx = jnp.einsum('btd,de->bte', jnp.concatenate([na_ctx.astype(F32), rw_ctx], -1), w_out).astype(a_ctx.dtype)
    return y_lat, y_ctx


def odd_mixer(a_lat, a_ctx, w_in, w_out, gate_b, norm_g, need_ctx):
    names = tuple(n for n, _ in ODD_LAYOUT)
    t_lat = project(a_lat, w_in, ODD_LAYOUT, names)
    t_ctx = project(a_ctx, w_in, ODD_LAYOUT, names if need_ctx else ODD_CTX_STATE_COLS)
    q_l, k_l, v_l, ig_l, lf_l = ml_prep(t_lat, gate_b, True, True)
    q_c, k_c, v_c, ig_c, lf_c = ml_prep(t_ctx, gate_b, False, need_ctx)
    B = a_lat.shape[0]
    zero = (jnp.zeros((B, ML_HEADS, ML_QK_DIM, ML_V_DIM), F32),
            jnp.zeros((B, ML_HEADS, ML_QK_DIM), F32),
            jnp.zeros((B, ML_HEADS), F32))
    h_l, h_c = [], []
    for d in range(2):
        f = (lambda z: jnp.flip(z, 2)) if d == 1 else (lambda z: z)
        starts_c, final_c = ml_chunk_states(f(k_c), f(v_c), f(ig_c[d]), f(lf_c[d]), zero)
        starts_l, _ = ml_chunk_states(f(k_l), f(v_l), f(ig_l[d]), f(lf_l[d]), final_c)
        h_l.append(f(ml_chunk_outputs(f(q_l), f(k_l), f(v_l), f(ig_l[d]), f(lf_l[d]), starts_l)))
        if need_ctx:
            h_c.append(f(ml_chunk_outputs(f(q_c), f(k_c), f(v_c), f(ig_c[d]), f(lf_c[d]), starts_c)))
    y_lat = jnp.einsum('btd,de->bte', ml_readout(h_l[0] + h_l[1], t_lat['ml_o'], norm_g), w_out).astype(a_lat.dtype)
    if not need_ctx:
        return y_lat, None
    y_ctx = jnp.einsum('btd,de->bte', ml_readout(h_c[0] + h_c[1], t_ctx['ml_o'], norm_g), w_out).astype(a_ctx.dtype)
    return y_lat, y_ctx


def swiglu(h, wg, wu, wd):
    return jnp.dot(jax.nn.silu(jnp.dot(h, wg)) * jnp.dot(h, wu), wd)


def moe_ffn(h, router_w, router_b, wg, wu, wd, sg, su, sd):
    T, D = h.shape
    E = router_w.shape[-1]
    s = jax.nn.sigmoid(jnp.dot(h, router_w).astype(F32))
    grp = (s + router_b.astype(F32)).reshape(T, N_GROUPS, E // N_GROUPS)
    g_score = lax.top_k(grp, 2)[0].sum(-1)
    g_keep = jax.nn.one_hot(lax.top_k(g_score, TOPK_GROUPS)[1], N_GROUPS).sum(1) > 0
    choice = jnp.where(g_keep[:, :, None], grp, NEG_INF).reshape(T, E)
    e_idx = lax.top_k(choice, TOP_K)[1]
    w_sel = jnp.take_along_axis(s, e_idx, 1)
    w_sel = w_sel / w_sel.sum(-1, keepdims=True) * ROUTED_SCALE
    n_asg = T * TOP_K
    flat_e = e_idx.reshape(-1)
    order = jnp.argsort(flat_e)
    se = flat_e[order]
    st = (order // TOP_K).astype(jnp.int32)
    sw = w_sel.reshape(-1)[order]
    counts = jnp.bincount(flat_e, length=E)
    padded = (counts + MOE_BLOCK - 1) // MOE_BLOCK * MOE_BLOCK
    ends_pad = jnp.cumsum(padded)
    pos = (ends_pad - padded)[se] + jnp.arange(n_asg) - (jnp.cumsum(counts) - counts)[se]
    n_blocks = -(-n_asg // MOE_BLOCK) + E
    buf_tok = jnp.full((n_blocks * MOE_BLOCK,), T, jnp.int32).at[pos].set(st)
    buf_w = jnp.zeros((n_blocks * MOE_BLOCK,), F32).at[pos].set(sw)
    blk_e = jnp.minimum(jnp.searchsorted(ends_pad, jnp.arange(n_blocks) * MOE_BLOCK, side='right'), E - 1)
    h_pad = jnp.concatenate([h, jnp.zeros((1, D), h.dtype)], 0)

    def block(acc, blk):
        tok, wt, e = blk
        y = swiglu(h_pad[tok], wg[e], wu[e], wd[e]) * wt[:, None]
        return acc.at[tok].add(y.astype(acc.dtype)), None

    acc, _ = lax.scan(block, jnp.zeros((T + 1, D), h.dtype),
                      (buf_tok.reshape(n_blocks, MOE_BLOCK), buf_w.reshape(n_blocks, MOE_BLOCK), blk_e))
    return acc[:T] + swiglu(h, sg, su, sd)


def setup_inputs(seed: int = 0) -> dict:
    key = jax.random.key(seed)
    ks = iter(jax.random.split(key, 48))

    def nrm(shape, scale):
        return jax.random.normal(next(ks), shape, F32) * scale

    D = D_MODEL
    MIX_E = NA_WIDTH + RW_WIDTH
    return {
        'x': nrm((BATCH, SEQ, D), 1.0),
        'c': nrm((BATCH, D), 1.0),
        'ctx': nrm((BATCH, CTX_LEN, D), 1.0),
        'c_ctx': nrm((D,), 1.0),
        'ada_w': nrm((DEPTH, D, 6 * D), 0.5 * D ** -0.5),
        'ada_b': nrm((DEPTH, 6 * D), 0.02),
        'ln_g': 1.0 + nrm((DEPTH, 2, D), 0.02),
        'ln_b': nrm((DEPTH, 2, D), 0.02),
        'ev_w_in': nrm((N_EVEN, D, EVEN_COLS), D ** -0.5),
        'ev_w_out': nrm((N_EVEN, MIX_E, D), DEEPNORM_BETA * MIX_E ** -0.5),
        'na_rpb': nrm((N_EVEN, NA_HEADS, 2 * NA_WIN_ROWS - 1, 2 * NA_WIN_COLS - 1), 0.5),
        'rw_mu': jax.random.uniform(next(ks), (N_EVEN, RW_SHIFT_COLS), F32),
        'rw_w0': nrm((N_EVEN, 2, RW_WIDTH), 1.0) - 2.0,
        'rw_w_up': nrm((N_EVEN, 2, RW_DECAY_LORA, RW_WIDTH), 0.1),
        'rw_a0': nrm((N_EVEN, 2, RW_WIDTH), 0.5),
        'rw_a_up': nrm((N_EVEN, 2, RW_AAA_LORA, RW_WIDTH), RW_AAA_LORA ** -0.5),
        'rw_g_up': nrm((N_EVEN, RW_GATE_LORA, RW_WIDTH), RW_GATE_LORA ** -0.5),
        'rw_k_k': 0.85 + nrm((N_EVEN, RW_WIDTH), 0.05),
        'rw_k_a': 1.0 + nrm((N_EVEN, RW_WIDTH), 0.05),
        'rw_r_k': nrm((N_EVEN, RW_HEADS, RW_HEAD_DIM), 0.1),
        'rw_gn_g': 1.0 + nrm((N_EVEN, RW_WIDTH), 0.02),
        'rw_gn_b': nrm((N_EVEN, RW_WIDTH), 0.02),
        'od_w_in': nrm((N_ODD, D, ODD_COLS), D ** -0.5),
        'od_w_out': nrm((N_ODD, ML_WIDTH, D), DEEPNORM_BETA * ML_WIDTH ** -0.5),
        'ml_gate_b': jnp.concatenate([nrm((N_ODD, 2, ML_HEADS), 0.1),
                                      3.0 + nrm((N_ODD, 2, ML_HEADS), 0.5)], axis=1),
        'ml_norm_g': 1.0 + nrm((N_ODD, ML_WIDTH), 0.02),
        'moe_router': nrm((DEPTH, D, N_EXPERTS), D ** -0.5),
        'moe_bias': nrm((DEPTH, N_EXPERTS), 0.01),
        'moe_w_gate': nrm((DEPTH, N_EXPERTS, D, EXPERT_FF), D ** -0.5),
        'moe_w_up': nrm((DEPTH, N_EXPERTS, D, EXPERT_FF), D ** -0.5),
        'moe_w_down': nrm((DEPTH, N_EXPERTS, EXPERT_FF, D), DEEPNORM_BETA * EXPERT_FF ** -0.5),
        'sh_w_gate': nrm((DEPTH, D, SHARED_FF), D ** -0.5),
        'sh_w_up': nrm((DEPTH, D, SHARED_FF), D ** -0.5),
        'sh_w_down': nrm((DEPTH, SHARED_FF, D), DEEPNORM_BETA * SHARED_FF ** -0.5),
    }


def reference(x, c, ctx, c_ctx, ada_w, ada_b, ln_g, ln_b, ev_w_in, ev_w_out, na_rpb, rw_mu, rw_w0, rw_w_up,
              rw_a0, rw_a_up, rw_g_up, rw_k_k, rw_k_a, rw_r_k, rw_gn_g, rw_gn_b, od_w_in, od_w_out, ml_gate_b,
              ml_norm_g, moe_router, moe_bias, moe_w_gate, moe_w_up, moe_w_down, sh_w_gate, sh_w_up, sh_w_down):
    B, N, D = x.shape
    C = ctx.shape[1]
    silu_c = jax.nn.silu(c)
    silu_cc = jax.nn.silu(c_ctx)
    h_lat, h_ctx = x, ctx
    for l in range(DEPTH):
        last = l == DEPTH - 1
        mods = jnp.split(silu_c @ ada_w[l] + ada_b[l], 6, axis=-1)
        n_cm = 2 if last else 6
        mods_c = jnp.split(silu_cc @ ada_w[l][:, :n_cm * D] + ada_b[l][:n_cm * D], n_cm, axis=-1)
        a_lat = modulate(h_lat, mods[0][:, None], mods[1][:, None])
        a_ctx = modulate(h_ctx, mods_c[0], mods_c[1])
        if l % 2 == 0:
            e = l // 2
            y_lat, y_ctx = even_mixer(a_lat, a_ctx, ev_w_in[e], ev_w_out[e], na_rpb[e], rw_mu[e], rw_w0[e],
                                      rw_w_up[e], rw_a0[e], rw_a_up[e], rw_g_up[e], rw_k_k[e], rw_k_a[e],
                                      rw_r_k[e], rw_gn_g[e], rw_gn_b[e], not last)
        else:
            o = l // 2
            y_lat, y_ctx = odd_mixer(a_lat, a_ctx, od_w_in[o], od_w_out[o], ml_gate_b[o], ml_norm_g[o], not last)
        h_lat = layer_norm(DEEPNORM_ALPHA * h_lat + mods[2][:, None] * y_lat, ln_g[l, 0], ln_b[l, 0])
        f_lat = modulate(h_lat, mods[3][:, None], mods[4][:, None]).reshape(B * N, D)
        moe_args = (moe_router[l], moe_bias[l], moe_w_gate[l], moe_w_up[l], moe_w_down[l],
                    sh_w_gate[l], sh_w_up[l], sh_w_down[l])
        if last:
            ff = moe_ffn(f_lat, *moe_args).reshape(B, N, D)
            h_lat = layer_norm(DEEPNORM_ALPHA * h_lat + mods[5][:, None] * ff, ln_g[l, 1], ln_b[l, 1])
        else:
            h_ctx = layer_norm(DEEPNORM_ALPHA * h_ctx + mods_c[2] * y_ctx, ln_g[l, 0], ln_b[l, 0])
            f_ctx = modulate(h_ctx, mods_c[3], mods_c[4]).reshape(B * C, D)
            ff = moe_ffn(jnp.concatenate([f_lat, f_ctx], 0), *moe_args)
            h_lat = layer_norm(DEEPNORM_ALPHA * h_lat + mods[5][:, None] * ff[:B * N].reshape(B, N, D),
                               ln_g[l, 1], ln_b[l, 1])
            h_ctx = layer_norm(DEEPNORM_ALPHA * h_ctx + mods_c[5] * ff[B * N:].reshape(B, C, D),
                               ln_g[l, 1], ln_b[l, 1])
    return h_lat
```

```python
import math


import numpy as np
from contextlib import ExitStack
import concourse.bass as bass
import concourse.mybir as mybir
from concourse.bass_utils import run_bass_kernel_spmd

F32 = mybir.dt.float32
BF16 = mybir.dt.bfloat16
I32 = mybir.dt.int32
U32 = mybir.dt.uint32
AF = mybir.ActivationFunctionType
ALU = mybir.AluOpType
AX = mybir.AxisListType

ENGS = ['pe', 'act', 'dve', 'pool', 'sp']
NDSEM = 24


class Prog:
    def __init__(self, same_engine_sync=True):
        self.nc = bass.Bass('TRN2', target_bir_lowering=False)
        self.stack = ExitStack()
        self.q = {e: [] for e in ENGS}
        self.cnt = {e: 0 for e in ENGS}
        self.seen = {e: {} for e in ENGS}
        self.lastw = {}
        self.readers = {}
        self.ndma = 0
        self.dslot_last = {}
        self.same_engine_sync = same_engine_sync
        self.sems = {}
        self.uid = 0
        self.out_events = []

    def dram(self, name, shape, dtype, kind):
        return self.nc.dram_tensor(name, list(shape), dtype, kind=kind).ap()

    def inp(self, name, shape, dtype=F32):
        return self.dram(name, shape, dtype, 'ExternalInput')

    def outp(self, name, shape, dtype=F32):
        return self.dram(name, shape, dtype, 'ExternalOutput')

    def scratch(self, name, shape, dtype=F32):
        return self.dram(name, shape, dtype, 'Internal')

    def sb(self, name, shape, dtype=F32):
        t = self.stack.enter_context(self.nc.sbuf_tensor(name, list(shape), dtype))
        return t

    def ps(self, name, shape, dtype=F32):
        t = self.stack.enter_context(self.nc.psum_tensor(name, list(shape), dtype))
        return t

    def _deps(self, eng, reads, writes):
        deps = {}

        def add(ev):
            k, v = ev
            if deps.get(k, 0) < v:
                deps[k] = v
        for k in reads:
            if k in self.lastw:
                add(self.lastw[k])
        for k in writes:
            if k in self.lastw:
                add(self.lastw[k])
            for ev in self.readers.get(k, {}).items():
                add(ev)
        waits = []
        for k, v in deps.items():
            if k == eng and (eng == 'pe' or not self.same_engine_sync):
                continue
            if self.seen[eng].get(k, 0) >= v:
                continue
            self.seen[eng][k] = v
            waits.append((k, v))
        return waits

    def _record(self, ev, reads, writes):
        for k in writes:
            self.lastw[k] = ev
            self.readers[k] = {}
        for k in reads:
            if k in writes:
                continue
            r = self.readers.setdefault(k, {})
            if r.get(ev[0], 0) < ev[1]:
                r[ev[0]] = ev[1]

    def op(self, eng, fn, reads=(), writes=()):
        waits = self._deps(eng, reads, writes)
        self.cnt[eng] += 1
        ev = (eng, self.cnt[eng])
        self.q[eng].append((waits, fn, eng, 1))
        self._record(ev, reads, writes)
        return ev

    def dma(self, fn, reads=(), writes=(), queue='sp', is_out=False):
        slot = (queue, self.ndma % NDSEM)
        self.ndma += 1
        key = ('d',) + slot
        prev = self.dslot_last.get(key, 0)
        waits = self._deps(queue, reads, writes)
        if prev and self.seen[queue].get(key, 0) < prev:
            self.seen[queue][key] = prev
            waits.append((key, prev))
        val = prev + 16
        self.dslot_last[key] = val
        ev = (key, val)
        self.q[queue].append((waits, fn, key, 16))
        self._record(ev, reads, writes)
        if is_out:
            self.out_events.append(ev)
        return ev

    def _sem(self, key):
        if key not in self.sems:
            nm = 's_' + '_'.join(str(x) for x in (key if isinstance(key, tuple) else (key,)))
            self.sems[key] = self.stack.enter_context(self.nc.semaphore(nm))
        return self.sems[key]

    def finish(self):
        nc = self.nc
        fin = {}
        for k, v in self.out_events:
            fin[k] = max(fin.get(k, 0), v)
        self.q['sp'].append(([(k, v) for k, v in fin.items()], None, None, 0))
        for e in ENGS:
            self._sem(e)
            for waits, fn, semkey, inc in self.q[e]:
                for k, v in waits:
                    self._sem(k)
                if semkey is not None:
                    self._sem(semkey)
        block = self.stack.enter_context(nc.Block())
        hmap = {'pe': 'tensor', 'act': 'scalar', 'dve': 'vector', 'pool': 'gpsimd', 'sp': 'sync'}

        def make(e):
            def body(h):
                for waits, fn, semkey, inc in self.q[e]:
                    for k, v in waits:
                        h.wait_ge(self.sems[k], v)
                    if fn is not None:
                        fn(h).then_inc(self.sems[semkey], inc)
            return body
        for e in ENGS:
            if self.q[e]:
                getattr(block, hmap[e])(make(e))
        self.stack.close()
        return nc


def run(prog_or_nc, in_maps, n=None, trace=False):
    nc = prog_or_nc
    n = n or len(in_maps)
    return run_bass_kernel_spmd(nc, in_maps, core_ids=list(range(n)), trace=trace)


ALPHA = (2.0 * 2) ** 0.25
CW = 256
LN_EPS = 1e-5


def bcast_mid(ap2d, n):
    P_, w = ap2d.shape
    return ap2d.unsqueeze(1).to_broadcast([P_, n, w])


def build_mods():
    P = Prog()
    cT = P.inp('cT', [128, 8, 4])
    aw = P.inp('aw', [128, 8, 1536])
    ab = P.inp('ab', [128, 12])
    out = P.outp('mods', [128, 12, 4])
    c_sb = P.sb('c_sb', [128, 8, 4]); s_sb = P.sb('s_sb', [128, 8, 4])
    aw_sb = P.sb('aw_sb', [128, 8, 1536]); ab_sb = P.sb('ab_sb', [128, 12])
    o_sb = P.sb('o_sb', [128, 12, 4])
    ps = P.ps('ps', [128, 12, 4])
    P.dma(lambda e: e.dma_start(out=c_sb[:], in_=cT), writes=['c'])
    P.dma(lambda e: e.dma_start(out=aw_sb[:], in_=aw), writes=['aw'])
    P.dma(lambda e: e.dma_start(out=ab_sb[:], in_=ab), writes=['ab'])
    P.op('act', lambda e: e.activation(out=s_sb[:], in_=c_sb[:], func=AF.Silu), reads=['c'], writes=['s'])
    for ft in range(12):
        for kt in range(8):
            P.op('pe', lambda e, ft=ft, kt=kt: e.matmul(ps[:, ft, :], lhsT=aw_sb[:, kt, ft * 128:(ft + 1) * 128],
                                                         rhs=s_sb[:, kt, :], start=(kt == 0), stop=(kt == 7)),
                 reads=['aw', 's'], writes=['ps'])
    for ft in range(12):
        P.op('act', lambda e, ft=ft: e.activation(out=o_sb[:, ft, :], in_=ps[:, ft, :], func=AF.Identity,
                                                   bias=ab_sb[:, ft:ft + 1], scale=1.0),
             reads=['ps', 'ab'], writes=['o'])
    P.dma(lambda e: e.dma_start(out=out, in_=o_sb[:]), reads=['o'], is_out=True)
    return P.finish()


def emit_ln(P, z, zsq, w, consts, tag, out_h, vec, gi, bi):
    onesD, eps_t, ps_m, ps_q, mean_sb, t1, rstd, tt = consts
    P.op('act', lambda e: e.activation(out=zsq[:, :, :w], in_=z[:, :, :w], func=AF.Square), reads=['z' + tag], writes=['zsq'])
    for ft in range(8):
        P.op('pe', lambda e, ft=ft: e.matmul(ps_m[:, :w], lhsT=onesD[:], rhs=z[:, ft, :w], start=(ft == 0), stop=(ft == 7)),
             reads=['z' + tag, 'onesD'], writes=['ps_m'])
    for ft in range(8):
        P.op('pe', lambda e, ft=ft: e.matmul(ps_q[:, :w], lhsT=onesD[:], rhs=zsq[:, ft, :w], start=(ft == 0), stop=(ft == 7)),
             reads=['zsq', 'onesD'], writes=['ps_q'])
    P.op('act', lambda e: e.activation(out=mean_sb[:, :w], in_=ps_m[:, :w], func=AF.Copy), reads=['ps_m'], writes=['mean'])
    P.op('dve', lambda e: e.tensor_tensor(out=t1[:, :w], in0=mean_sb[:, :w], in1=mean_sb[:, :w], op=ALU.mult), reads=['mean'], writes=['t1'])
    P.op('dve', lambda e: e.tensor_tensor(out=t1[:, :w], in0=ps_q[:, :w], in1=t1[:, :w], op=ALU.subtract), reads=['ps_q', 't1'], writes=['t1'])
    P.op('act', lambda e: e.activation(out=t1[:, :w], in_=t1[:, :w], func=AF.Sqrt, bias=eps_t[:, 0:1], scale=1.0), reads=['t1', 'eps'], writes=['t1'])
    P.op('dve', lambda e: e.reciprocal(out=rstd[:, :w], in_=t1[:, :w]), reads=['t1'], writes=['rstd'])
    P.op('dve', lambda e: e.tensor_tensor(out=tt[:, :, :w], in0=z[:, :, :w], in1=bcast_mid(mean_sb[:, :w], 8), op=ALU.subtract),
         reads=['z' + tag, 'mean'], writes=['tt'])
    P.op('dve', lambda e: e.tensor_tensor(out=tt[:, :, :w], in0=tt[:, :, :w], in1=bcast_mid(rstd[:, :w], 8), op=ALU.mult),
         reads=['tt', 'rstd'], writes=['tt'])
    for ft in range(8):
        P.op('act', lambda e, ft=ft: e.activation(out=out_h[:, ft, :w], in_=tt[:, ft, :w], func=AF.Identity,
                                                   scale=vec[:, ft, gi:gi + 1], bias=vec[:, ft, bi:bi + 1]),
             reads=['tt', 'vec'], writes=['h' + tag])


def ln_consts(P):
    onesD = P.sb('onesD', [128, 128]); eps_t = P.sb('eps_t', [128, 1])
    ps_m = P.ps('ps_m', [128, 512]); ps_q = P.ps('ps_q', [128, 512])
    mean_sb = P.sb('mean_sb', [128, 512]); t1 = P.sb('t1', [128, 512]); rstd = P.sb('rstd', [128, 512])
    tt = P.sb('tt', [128, 8, CW])
    P.op('pool', lambda e: e.memset(onesD[:], 1.0 / 1024.0), writes=['onesD'])
    P.op('pool', lambda e: e.memset(eps_t[:], LN_EPS), writes=['eps'])
    return (onesD, eps_t, ps_m, ps_q, mean_sb, t1, rstd, tt)


def chunks_of(segs):
    out = []
    for (a, b, m) in segs:
        c = a
        while c < b:
            w = min(CW, b - c)
            out.append((c, w, m))
            c += w
    return out


def build_pre(T, segs):
    P = Prog()
    xT = P.inp('xT', [128, 8, T]); mixT = P.inp('mixT', [128, 8, T])
    wout = P.inp('wout', [128, 8, 1024]); vec_d = P.inp('vec', [128, 8, 8])
    router = P.inp('router', [128, 8, 256]); rbias = P.inp('rbias', [128, 256])
    h1T = P.outp('h1T', [128, 8, T]); fT = P.outp('fT', [128, 8, T], BF16); Wt = P.outp('Wt', [T, 256])
    wo_f = P.sb('wo_f', [128, 8, 1024]); wo_b = P.sb('wo_b', [128, 8, 1024], BF16)
    vec = P.sb('vec_sb', [128, 8, 8]); rt = P.sb('rt', [128, 8, 256]); rb = P.sb('rb', [128, 256])
    consts = ln_consts(P)
    P.dma(lambda e: e.dma_start(out=wo_f[:], in_=wout), writes=['wo_f'])
    P.dma(lambda e: e.dma_start(out=vec[:], in_=vec_d), writes=['vec'])
    P.dma(lambda e: e.dma_start(out=rt[:], in_=router), writes=['rt'])
    P.dma(lambda e: e.dma_start(out=rb[:], in_=rbias), writes=['rb'])
    P.op('pool', lambda e: e.tensor_copy(out=wo_b[:], in_=wo_f[:]), reads=['wo_f'], writes=['wo_b'])
    for m in range(2):
        P.op('dve', lambda e, m=m: e.tensor_scalar(out=vec[:, :, 4 + 3 * m:5 + 3 * m], in0=vec[:, :, 4 + 3 * m:5 + 3 * m],
                                                   scalar1=1.0, scalar2=None, op0=ALU.add), reads=['vec'], writes=['vec'])
    mx = [P.sb(f'mx{i}', [128, 8, CW]) for i in range(2)]
    xx = [P.sb(f'xx{i}', [128, 8, CW]) for i in range(2)]
    mxb = P.sb('mxb', [128, 8, CW], BF16)
    z = P.sb('z', [128, 8, CW]); zsq = P.sb('zsq', [128, 8, CW])
    h1 = P.sb('h1', [128, 8, CW]); f32t = P.sb('f32t', [128, 8, CW]); fb = P.sb('fb', [128, 8, CW], BF16)
    ps_y = [P.ps(f'ps_y{i}', [128, 512]) for i in range(2)]
    ps_r = P.ps('ps_r', [128, 256])
    s_sb = P.sb('s_sb', [128, 256]); grp = P.sb('grp', [128, 256]); m8 = P.sb('m8', [128, 8, 8])
    gs = P.sb('gs', [128, 8]); g8 = P.sb('g8', [128, 8]); keep = P.sb('keep', [128, 8]); pen = P.sb('pen', [128, 8])
    choice = P.sb('choice', [128, 256]); c8 = P.sb('c8', [128, 8]); sel = P.sb('sel', [128, 256])
    wv = P.sb('wv', [128, 256]); wsum = P.sb('wsum', [128, 1]); rinv = P.sb('rinv', [128, 1]); wt_sb = P.sb('wt_sb', [128, 256])
    for ci, (c0, w, m) in enumerate(chunks_of(segs)):
        b = ci % 2
        P.dma(lambda e, b=b, c0=c0, w=w: e.dma_start(out=mx[b][:, :, :w], in_=mixT[:, :, c0:c0 + w]), writes=[f'mx{b}'])
        P.dma(lambda e, b=b, c0=c0, w=w: e.dma_start(out=xx[b][:, :, :w], in_=xT[:, :, c0:c0 + w]), writes=[f'xx{b}'])
        P.op('pool', lambda e, b=b, w=w: e.tensor_copy(out=mxb[:, :, :w], in_=mx[b][:, :, :w]), reads=[f'mx{b}'], writes=['mxb'])
        P.op('act', lambda e, b=b, w=w: e.activation(out=z[:, :, :w], in_=xx[b][:, :, :w], func=AF.Copy, scale=ALPHA),
             reads=[f'xx{b}'], writes=['z'])
        for ft in range(8):
            pb = ft % 2
            for kt in range(8):
                P.op('pe', lambda e, pb=pb, kt=kt, ft=ft, w=w: e.matmul(ps_y[pb][:, :w], lhsT=wo_b[:, kt, ft * 128:(ft + 1) * 128],
                                                                       rhs=mxb[:, kt, :w], start=(kt == 0), stop=(kt == 7)),
                     reads=['wo_b', 'mxb'], writes=[f'ps_y{pb}'])
            P.op('dve', lambda e, pb=pb, ft=ft, w=w, m=m: e.scalar_tensor_tensor(
                out=z[:, ft, :w], in0=ps_y[pb][:, :w], scalar=vec[:, ft, 2 + 3 * m:3 + 3 * m], in1=z[:, ft, :w],
                op0=ALU.mult, op1=ALU.add), reads=[f'ps_y{pb}', 'vec', 'z'], writes=['z'])
        emit_ln(P, z, zsq, w, consts, '', h1, vec, 0, 1)
        for ft in range(8):
            P.op('act', lambda e, ft=ft, w=w, m=m: e.activation(out=f32t[:, ft, :w], in_=h1[:, ft, :w], func=AF.Identity,
                                                                scale=vec[:, ft, 4 + 3 * m:5 + 3 * m], bias=vec[:, ft, 3 + 3 * m:4 + 3 * m]),
                 reads=['h', 'vec'], writes=['f32t'])
        P.op('pool', lambda e, w=w: e.tensor_copy(out=fb[:, :, :w], in_=f32t[:, :, :w]), reads=['f32t'], writes=['fb'])
        P.dma(lambda e, c0=c0, w=w: e.dma_start(out=h1T[:, :, c0:c0 + w], in_=h1[:, :, :w]), reads=['h'], is_out=True)
        P.dma(lambda e, c0=c0, w=w: e.dma_start(out=fT[:, :, c0:c0 + w], in_=fb[:, :, :w]), reads=['fb'], is_out=True)
        for s0 in range(0, w, 128):
            n = min(128, w - s0)
            for kt in range(8):
                P.op('pe', lambda e, kt=kt, s0=s0, n=n: e.matmul(ps_r[:n, :], lhsT=f32t[:, kt, s0:s0 + n], rhs=rt[:, kt, :],
                                                                start=(kt == 0), stop=(kt == 7)),
                     reads=['f32t', 'rt'], writes=['ps_r'])
            P.op('act', lambda e, n=n: e.activation(out=s_sb[:n, :], in_=ps_r[:n, :], func=AF.Sigmoid), reads=['ps_r'], writes=['s_sb'])
            P.op('dve', lambda e, n=n: e.tensor_tensor(out=grp[:n, :], in0=s_sb[:n, :], in1=rb[:n, :], op=ALU.add),
                 reads=['s_sb', 'rb'], writes=['grp'])
            for gi in range(8):
                P.op('dve', lambda e, n=n, gi=gi: e.max(out=m8[:n, gi, :], in_=grp[:n, gi * 32:(gi + 1) * 32]), reads=['grp'], writes=['m8'])
            P.op('dve', lambda e, n=n: e.tensor_tensor(out=gs[:n, :], in0=m8[:n, :, 0], in1=m8[:n, :, 1], op=ALU.add), reads=['m8'], writes=['gs'])
            P.op('dve', lambda e, n=n: e.max(out=g8[:n, :], in_=gs[:n, :]), reads=['gs'], writes=['g8'])
            P.op('dve', lambda e, n=n: e.tensor_scalar(out=keep[:n, :], in0=gs[:n, :], scalar1=g8[:n, 3:4], scalar2=None, op0=ALU.is_ge),
                 reads=['gs', 'g8'], writes=['keep'])
            P.op('dve', lambda e, n=n: e.tensor_scalar(out=pen[:n, :], in0=keep[:n, :], scalar1=1.0, scalar2=1e30, op0=ALU.subtract, op1=ALU.mult),
                 reads=['keep'], writes=['pen'])
            for gi in range(8):
                P.op('dve', lambda e, n=n, gi=gi: e.tensor_scalar(out=choice[:n, gi * 32:(gi + 1) * 32], in0=grp[:n, gi * 32:(gi + 1) * 32],
                                                                 scalar1=keep[:n, gi:gi + 1], scalar2=pen[:n, gi:gi + 1], op0=ALU.mult, op1=ALU.add),
                     reads=['grp', 'keep', 'pen'], writes=['choice'])
            P.op('dve', lambda e, n=n: e.max(out=c8[:n, :], in_=choice[:n, :]), reads=['choice'], writes=['c8'])
            P.op('dve', lambda e, n=n: e.tensor_scalar(out=sel[:n, :], in0=choice[:n, :], scalar1=c8[:n, 7:8], scalar2=None, op0=ALU.is_ge),
                 reads=['choice', 'c8'], writes=['sel'])
            P.op('dve', lambda e, n=n: e.tensor_tensor(out=wv[:n, :], in0=s_sb[:n, :], in1=sel[:n, :], op=ALU.mult), reads=['s_sb', 'sel'], writes=['wv'])
            P.op('dve', lambda e, n=n: e.reduce_sum(out=wsum[:n, :], in_=wv[:n, :], axis=AX.X), reads=['wv'], writes=['wsum'])
            P.op('dve', lambda e, n=n: e.reciprocal(out=rinv[:n, :], in_=wsum[:n, :]), reads=['wsum'], writes=['rinv'])
            P.op('dve', lambda e, n=n: e.tensor_scalar(out=wt_sb[:n, :], in0=wv[:n, :], scalar1=rinv[:n, 0:1], scalar2=2.5, op0=ALU.mult, op1=ALU.mult),
                 reads=['wv', 'rinv'], writes=['wt_sb'])
            P.dma(lambda e, n=n, r0=c0 + s0: e.dma_start(out=Wt[r0:r0 + n, :], in_=wt_sb[:n, :]), reads=['wt_sb'], is_out=True)
    return P.finish()


def build_moe(T_all, TC, NE=33):
    assert T_all % TC == 0
    P = Prog()
    fT = P.inp('fT', [128, 8, T_all], BF16); WtT = P.inp('WtT', [NE, T_all])
    wg = P.inp('wg', [NE, 128, 8, 256]); wu = P.inp('wu', [NE, 128, 8, 256]); wd = P.inp('wd', [NE, 128, 2, 1024])
    part = P.outp('part', [128, 8, T_all])
    wgb_d = P.scratch('wgb_d', [NE, 128, 8, 256], BF16); wub_d = P.scratch('wub_d', [NE, 128, 8, 256], BF16)
    wdb_d = P.scratch('wdb_d', [NE, 128, 2, 1024], BF16)
    st_f = [P.sb(f'st_f{i}', [128, 2048]) for i in range(2)]
    st_b = [P.sb(f'st_b{i}', [128, 2048], BF16) for i in range(2)]
    k = 0
    for e_ in range(NE):
        for (src, dst) in ((wg, wgb_d), (wu, wub_d), (wd, wdb_d)):
            b = k % 2
            sflat = src[e_].rearrange('p a b -> p (a b)')
            dflat = dst[e_].rearrange('p a b -> p (a b)')
            P.dma(lambda e, b=b, sflat=sflat: e.dma_start(out=st_f[b][:], in_=sflat), writes=[f'st_f{b}'])
            eng = 'pool' if k % 2 == 0 else 'act'
            if eng == 'pool':
                P.op('pool', lambda e, b=b: e.tensor_copy(out=st_b[b][:], in_=st_f[b][:]), reads=[f'st_f{b}'], writes=[f'st_b{b}'])
            else:
                P.op('act', lambda e, b=b: e.activation(out=st_b[b][:], in_=st_f[b][:], func=AF.Copy), reads=[f'st_f{b}'], writes=[f'st_b{b}'])
            P.dma(lambda e, b=b, dflat=dflat: e.dma_start(out=dflat, in_=st_b[b][:]), reads=[f'st_b{b}'], writes=[('wbd', e_, id(dst))])
            k += 1
    ft_sb = [P.sb(f'ft_sb{i}', [128, 8, TC], BF16) for i in range(1)]
    acc = P.sb('acc', [128, 8, TC])
    wgb = [P.sb(f'wgb{i}', [128, 8, 256], BF16) for i in range(2)]
    wub = [P.sb(f'wub{i}', [128, 8, 256], BF16) for i in range(2)]
    wdb = [P.sb(f'wdb{i}', [128, 2, 1024], BF16) for i in range(2)]
    wrow = [P.sb(f'wrow{i}', [1, TC]) for i in range(2)]
    ones1 = P.sb('ones1', [1, 128])
    P.op('pool', lambda e: e.memset(ones1[:], 1.0), writes=['ones1'])
    actb = P.sb('actb', [128, 2, TC], BF16)
    sg = [P.sb(f'sg{i}', [128, 512]) for i in range(2)]
    ps_g = [P.ps(f'ps_g{i}', [128, 512]) for i in range(2)]
    ps_u = [P.ps(f'ps_u{i}', [128, 512]) for i in range(2)]
    ps_w = P.ps('ps_w', [128, 512])
    ps_o = [P.ps(f'ps_o{i}', [128, 512]) for i in range(2)]
    cols = [(c, min(512, TC - c)) for c in range(0, TC, 512)]
    it = 0
    for tc in range(T_all // TC):
        t0 = tc * TC
        fb_ = 0
        P.dma(lambda e, fb_=fb_, t0=t0: e.dma_start(out=ft_sb[fb_][:], in_=fT[:, :, t0:t0 + TC]), writes=[f'ft_sb{fb_}'])
        for e_ in range(NE):
            b = it % 2
            it += 1
            P.dma(lambda e, b=b, e_=e_: e.dma_start(out=wgb[b][:], in_=wgb_d[e_]), reads=[('wbd', e_, id(wgb_d))], writes=[f'wgb{b}'])
            P.dma(lambda e, b=b, e_=e_: e.dma_start(out=wub[b][:], in_=wub_d[e_]), reads=[('wbd', e_, id(wub_d))], writes=[f'wub{b}'])
            P.dma(lambda e, b=b, e_=e_: e.dma_start(out=wdb[b][:], in_=wdb_d[e_]), reads=[('wbd', e_, id(wdb_d))], writes=[f'wdb{b}'])
            P.dma(lambda e, b=b, e_=e_, t0=t0: e.dma_start(out=wrow[b][:], in_=WtT[e_:e_ + 1, t0:t0 + TC]), writes=[f'wrow{b}'])
            q = 0
            for j in range(2):
                for (c0, w) in cols:
                    pb = q % 2
                    q += 1
                    for kt in range(8):
                        P.op('pe', lambda e, pb=pb, kt=kt, j=j, c0=c0, w=w, b=b, fb_=fb_: e.matmul(
                            ps_g[pb][:, :w], lhsT=wgb[b][:, kt, j * 128:(j + 1) * 128], rhs=ft_sb[fb_][:, kt, c0:c0 + w],
                            start=(kt == 0), stop=(kt == 7)), reads=[f'wgb{b}', f'ft_sb{fb_}'], writes=[f'ps_g{pb}'])
                    for kt in range(8):
                        P.op('pe', lambda e, pb=pb, kt=kt, j=j, c0=c0, w=w, b=b, fb_=fb_: e.matmul(
                            ps_u[pb][:, :w], lhsT=wub[b][:, kt, j * 128:(j + 1) * 128], rhs=ft_sb[fb_][:, kt, c0:c0 + w],
                            start=(kt == 0), stop=(kt == 7)), reads=[f'wub{b}', f'ft_sb{fb_}'], writes=[f'ps_u{pb}'])
                    if j == 0:
                        pass
                    P.op('pe', lambda e, c0=c0, w=w, b=b: e.matmul(ps_w[:, :w], lhsT=ones1[:, :], rhs=wrow[b][:, c0:c0 + w], start=True, stop=True),
                         reads=['ones1', f'wrow{b}'], writes=['ps_w'])
                    P.op('act', lambda e, pb=pb, w=w: e.activation(out=sg[pb][:, :w], in_=ps_g[pb][:, :w], func=AF.Silu),
                         reads=[f'ps_g{pb}'], writes=[f'sg{pb}'])
                    P.op('dve', lambda e, pb=pb, w=w: e.tensor_tensor(out=sg[pb][:, :w], in0=sg[pb][:, :w], in1=ps_u[pb][:, :w], op=ALU.mult),
                         reads=[f'sg{pb}', f'ps_u{pb}'], writes=[f'sg{pb}'])
                    P.op('dve', lambda e, pb=pb, w=w, j=j, c0=c0: e.tensor_tensor(out=actb[:, j, c0:c0 + w], in0=sg[pb][:, :w], in1=ps_w[:, :w], op=ALU.mult),
                         reads=[f'sg{pb}', 'ps_w'], writes=['actb'])
            for ft in range(8):
                for (c0, w) in cols:
                    pb = q % 2
                    q += 1
                    for j in range(2):
                        P.op('pe', lambda e, pb=pb, j=j, ft=ft, c0=c0, w=w, b=b: e.matmul(
                            ps_o[pb][:, :w], lhsT=wdb[b][:, j, ft * 128:(ft + 1) * 128], rhs=actb[:, j, c0:c0 + w],
                            start=(j == 0), stop=(j == 1)), reads=[f'wdb{b}', 'actb'], writes=[f'ps_o{pb}'])
                    if e_ == 0:
                        P.op('act', lambda e, pb=pb, ft=ft, c0=c0, w=w: e.activation(out=acc[:, ft, c0:c0 + w], in_=ps_o[pb][:, :w], func=AF.Copy),
                             reads=[f'ps_o{pb}'], writes=[('acc', ft)])
                    else:
                        P.op('dve', lambda e, pb=pb, ft=ft, c0=c0, w=w: e.tensor_tensor(out=acc[:, ft, c0:c0 + w], in0=acc[:, ft, c0:c0 + w],
                                                                                    in1=ps_o[pb][:, :w], op=ALU.add),
                             reads=[f'ps_o{pb}', ('acc', ft)], writes=[('acc', ft)])
        P.dma(lambda e, t0=t0: e.dma_start(out=part[:, :, t0:t0 + TC], in_=acc[:]), reads=[('acc', ft) for ft in range(8)], is_out=True)
    return P.finish()


def build_post(T, segs, NP=8):
    P = Prog()
    h1T = P.inp('h1T', [128, 8, T]); parts = P.inp('parts', [NP, 128, 8, T]); vec_d = P.inp('vec', [128, 8, 4])
    h2T = P.outp('h2T', [128, 8, T])
    vec = P.sb('vec_sb', [128, 8, 4])
    consts = ln_consts(P)
    P.dma(lambda e: e.dma_start(out=vec[:], in_=vec_d), writes=['vec'])
    hh = [P.sb(f'hh{i}', [128, 8, CW]) for i in range(2)]
    pp = [P.sb(f'pp{i}', [128, 8, CW]) for i in range(3)]
    ff = P.sb('ff', [128, 8, CW])
    z = P.sb('z', [128, 8, CW]); zsq = P.sb('zsq', [128, 8, CW]); h2 = P.sb('h2', [128, 8, CW])
    k = 0
    for ci, (c0, w, m) in enumerate(chunks_of(segs)):
        b = ci % 2
        P.dma(lambda e, b=b, c0=c0, w=w: e.dma_start(out=hh[b][:, :, :w], in_=h1T[:, :, c0:c0 + w]), writes=[f'hh{b}'])
        for pi in range(NP):
            pb = k % 3
            k += 1
            P.dma(lambda e, pb=pb, pi=pi, c0=c0, w=w: e.dma_start(out=pp[pb][:, :, :w], in_=parts[pi, :, :, c0:c0 + w]), writes=[f'pp{pb}'])
            if pi == 0:
                P.op('pool', lambda e, pb=pb, w=w: e.tensor_copy(out=ff[:, :, :w], in_=pp[pb][:, :, :w]), reads=[f'pp{pb}'], writes=['ff'])
            else:
                P.op('pool', lambda e, pb=pb, w=w: e.tensor_tensor(out=ff[:, :, :w], in0=ff[:, :, :w], in1=pp[pb][:, :, :w], op=ALU.add),
                     reads=[f'pp{pb}', 'ff'], writes=['ff'])
        P.op('act', lambda e, b=b, w=w: e.activation(out=z[:, :, :w], in_=hh[b][:, :, :w], func=AF.Copy, scale=ALPHA), reads=[f'hh{b}'], writes=['z'])
        for ft in range(8):
            P.op('dve', lambda e, ft=ft, w=w, m=m: e.scalar_tensor_tensor(out=z[:, ft, :w], in0=ff[:, ft, :w], scalar=vec[:, ft, 2 + m:3 + m],
                                                                         in1=z[:, ft, :w], op0=ALU.mult, op1=ALU.add),
                 reads=['ff', 'vec', 'z'], writes=['z'])
        emit_ln(P, z, zsq, w, consts, '', h2, vec, 0, 1)
        P.dma(lambda e, c0=c0, w=w: e.dma_start(out=h2T[:, :, c0:c0 + w], in_=h2[:, :, :w]), reads=['h'], is_out=True)
    return P.finish()


ML_EPS = 1e-6
NCOL = 776


def build_ml(NCTX=2, NLAT=64):
    NCH = NCTX + NLAT
    T = NCH * 128
    P = Prog()
    hT = P.inp('hT', [128, 8, T]); vec_d = P.inp('vec', [128, 8, 4]); W_d = P.inp('W', [128, 8, NCOL])
    gb_d = P.inp('gb', [128, 8]); cos_d = P.inp('cosT', [128, NLAT, 32]); sin_d = P.inp('sinT', [128, NLAT, 32])
    ng_d = P.inp('ng', [128, 256]); tri_d = P.inp('tri', [2, 128, 128]); id_d = P.inp('ident', [128, 128])
    mix = P.outp('mix', [NLAT * 128, 256])
    H0 = P.scratch('H0', [NLAT, 128, 256])

    vec = P.sb('vec_sb', [128, 8, 4]); Wf = P.sb('Wf', [128, 8, NCOL]); Wb = P.sb('Wb', [128, 8, NCOL], BF16)
    gb = P.sb('gb_sb', [128, 8]); cosT = P.sb('cos_sb', [128, NLAT, 32]); sinT = P.sb('sin_sb', [128, NLAT, 32])
    ng = P.sb('ng_sb', [128, 256]); tri = P.sb('tri_sb', [128, 2, 128]); idf = P.sb('idf', [128, 128]); idb = P.sb('idb', [128, 128], BF16)
    onesm = P.sb('onesm', [128, 128]); c1 = P.sb('c1', [128, 1]); cln8 = P.sb('cln8', [128, 1]); ceps = P.sb('ceps', [128, 1])
    P.dma(lambda e: e.dma_start(out=vec[:], in_=vec_d), writes=['vec'])
    P.dma(lambda e: e.dma_start(out=Wf[:], in_=W_d), writes=['Wf'])
    P.dma(lambda e: e.dma_start(out=gb[:], in_=gb_d), writes=['gb'])
    P.dma(lambda e: e.dma_start(out=cosT[:], in_=cos_d), writes=['cos'])
    P.dma(lambda e: e.dma_start(out=sinT[:], in_=sin_d), writes=['sin'])
    P.dma(lambda e: e.dma_start(out=ng[:], in_=ng_d), writes=['ng'])
    for d in range(2):
        P.dma(lambda e, d=d: e.dma_start(out=tri[:, d, :], in_=tri_d[d]), writes=['tri'])
    P.dma(lambda e: e.dma_start(out=idf[:], in_=id_d), writes=['idf'])
    P.op('pool', lambda e: e.tensor_copy(out=Wb[:], in_=Wf[:]), reads=['Wf'], writes=['Wb'])
    P.op('pool', lambda e: e.tensor_copy(out=idb[:], in_=idf[:]), reads=['idf'], writes=['idb'])
    P.op('pool', lambda e: e.memset(onesm[:], 1.0), writes=['onesm'])
    P.op('pool', lambda e: e.memset(c1[:], 1.0), writes=['c1'])
    P.op('pool', lambda e: e.memset(cln8[:], -math.log(8.0)), writes=['cln8'])
    P.op('pool', lambda e: e.memset(ceps[:], ML_EPS), writes=['ceps'])
    for j in (0, 2):
        P.op('dve', lambda e, j=j: e.tensor_scalar(out=vec[:, :, j:j + 1], in0=vec[:, :, j:j + 1], scalar1=1.0, scalar2=None, op0=ALU.add),
             reads=['vec'], writes=['vec'])

    hin = [P.sb(f'hin{i}', [128, 8, 128]) for i in range(2)]
    af = P.sb('af', [128, 8, 128]); ab = P.sb('ab', [128, 8, 128], BF16)
    psA = P.ps('psA', [128, 512]); psB = P.ps('psB', [128, 264])
    psT = P.ps('psT', [64, 4, 128], BF16)
    psG = P.ps('psG', [128, 8])
    psS = P.ps('psS', [128, 128]); psO = P.ps('psO', [128, 129]); psU = P.ps('psU', [64, 129])
    qk = P.sb('qk', [128, 4, 64]); qkb = P.sb('qkb', [128, 4, 64], BF16)
    r1 = P.sb('r1', [128, 4, 2, 16]); r2 = P.sb('r2', [128, 4, 2, 16])
    qkT = P.sb('qkT', [64, 4, 128], BF16)
    gx = P.sb('gx', [128, 2]); gax = P.sb('gax', [128, 2]); ge = P.sb('ge', [128, 2]); gl = P.sb('gl', [128, 2]); lf = P.sb('lf', [128, 2])
    ig = P.sb('ig', [128, 2]); ebq = P.sb('ebq', [128, 2]); ek = P.sb('ek', [128, 2]); EB = P.sb('EB', [128, 2]); gt = P.sb('gt', [128, 2])
    Vt = P.sb('Vt', [128, 2, 129], BF16)
    osig = P.sb('osig', [128, 256])
    sT = P.sb('sT', [128, 128], BF16)
    Cf = P.sb('Cf', [64, 2, 129]); Cb = P.sb('Cb', [64, 2, 129], BF16); Ct = P.sb('Ct', [64, 129])
    dd = P.sb('dd', [128, 1]); rr = P.sb('rr', [128, 1])
    hout = P.sb('hout', [128, 256]); h0 = P.sb('h0', [128, 256]); hsq = P.sb('hsq', [128, 128]); ms = P.sb('ms', [128, 2])
    fin = P.sb('fin', [128, 256])

    nload = 0
    for d in range(2):
        P.op('pool', lambda e: e.memset(Cf[:], 0.0), writes=['Cf'])
        P.op('pool', lambda e: e.memset(Cb[:], 0.0), writes=['Cb'])
        order = list(range(NCTX)) + list(range(NCTX, NCH))
        if d == 1:
            order = list(range(NCTX - 1, -1, -1)) + list(range(NCH - 1, NCTX - 1, -1))
        for c in order:
            is_lat = c >= NCTX
            lc = c - NCTX
            hb = nload % 2
            nload += 1
            P.dma(lambda e, hb=hb, c=c: e.dma_start(out=hin[hb][:], in_=hT[:, :, c * 128:(c + 1) * 128]), writes=[f'hin{hb}'])
            js, jh = (0, 1) if is_lat else (2, 3)
            P.op('dve', lambda e, hb=hb, js=js: e.tensor_tensor(out=af[:], in0=hin[hb][:], in1=vec[:, :, js:js + 1].to_broadcast([128, 8, 128]), op=ALU.mult),
                 reads=[f'hin{hb}', 'vec'], writes=['af'])
            P.op('pool', lambda e, jh=jh: e.tensor_tensor(out=ab[:], in0=af[:], in1=vec[:, :, jh:jh + 1].to_broadcast([128, 8, 128]), op=ALU.add),
                 reads=['af', 'vec'], writes=['ab'])
            for kt in range(8):
                P.op('pe', lambda e, kt=kt: e.matmul(psA[:, :], lhsT=ab[:, kt, :], rhs=Wb[:, kt, 0:512], start=(kt == 0), stop=(kt == 7)),
                     reads=['ab', 'Wb'], writes=['psA'])
            for kt in range(8):
                P.op('pe', lambda e, kt=kt: e.matmul(psB[:, :], lhsT=ab[:, kt, :], rhs=Wb[:, kt, 512:776], start=(kt == 0), stop=(kt == 7)),
                     reads=['ab', 'Wb'], writes=['psB'])
            P.op('act', lambda e: e.activation(out=qk[:].rearrange('p a b -> p (a b)'), in_=psA[:, 0:256], func=AF.Copy), reads=['psA'], writes=['qk'])
            if is_lat:
                q5 = qk[:].rearrange('p a (h u i) -> p a h u i', h=2, u=2)
                o5 = qkb[:].rearrange('p a (h u i) -> p a h u i', h=2, u=2)
                u1 = q5[:, :, :, 0, :]; u2 = q5[:, :, :, 1, :]
                cosb = cosT[:, lc, :].rearrange('p (h i) -> p h i', h=2).unsqueeze(1).to_broadcast([128, 4, 2, 16])
                sinb = sinT[:, lc, :].rearrange('p (h i) -> p h i', h=2).unsqueeze(1).to_broadcast([128, 4, 2, 16])
                P.op('dve', lambda e, u1=u1, cosb=cosb: e.tensor_tensor(out=r1[:], in0=u1, in1=cosb, op=ALU.mult), reads=['qk', 'cos'], writes=['r1'])
                P.op('pool', lambda e, u2=u2, sinb=sinb: e.tensor_tensor(out=r2[:], in0=u2, in1=sinb, op=ALU.mult), reads=['qk', 'sin'], writes=['r2'])
                P.op('dve', lambda e, o5=o5: e.tensor_tensor(out=o5[:, :, :, 0, :], in0=r1[:], in1=r2[:], op=ALU.subtract), reads=['r1', 'r2'], writes=['qkb'])
                P.op('dve', lambda e, u1=u1, sinb=sinb: e.tensor_tensor(out=r1[:], in0=u1, in1=sinb, op=ALU.mult), reads=['qk', 'sin'], writes=['r1'])
                P.op('pool', lambda e, u2=u2, cosb=cosb: e.tensor_tensor(out=r2[:], in0=u2, in1=cosb, op=ALU.mult), reads=['qk', 'cos'], writes=['r2'])
                P.op('dve', lambda e, o5=o5: e.tensor_tensor(out=o5[:, :, :, 1, :], in0=r1[:], in1=r2[:], op=ALU.add), reads=['r1', 'r2'], writes=['qkb'])
            else:
                P.op('dve', lambda e: e.tensor_copy(out=qkb[:], in_=qk[:]), reads=['qk'], writes=['qkb'])
            jlist = (0, 1, 2, 3) if is_lat else ()
            for j in jlist:
                P.op('pe', lambda e, j=j: e.transpose(psT[:, j, :], qkb[:, j, :], idb[:]), reads=['qkb', 'idb'], writes=['psT'])
            if is_lat:
                P.op('act', lambda e: e.activation(out=qkT[:], in_=psT[:], func=AF.Copy), reads=['psT'], writes=['qkT'])
            gi0 = 256 + 2 * d
            gf0 = 256 + 4 + 2 * d
            P.op('dve', lambda e, gf0=gf0, d=d: e.tensor_tensor(out=gx[:], in0=psB[:, gf0:gf0 + 2], in1=gb[:, 4 + 2 * d:6 + 2 * d], op=ALU.add),
                 reads=['psB', 'gb'], writes=['gx'])
            P.op('dve', lambda e, gi0=gi0, d=d: e.tensor_tensor(out=ig[:], in0=psB[:, gi0:gi0 + 2], in1=gb[:, 2 * d:2 + 2 * d], op=ALU.add),
                 reads=['psB', 'gb'], writes=['ig'])
            P.op('act', lambda e: e.activation(out=gax[:], in_=gx[:], func=AF.Abs), reads=['gx'], writes=['gax'])
            P.op('act', lambda e: e.activation(out=ge[:], in_=gax[:], func=AF.Exp, scale=-1.0), reads=['gax'], writes=['ge'])
            P.op('act', lambda e: e.activation(out=gl[:], in_=ge[:], func=AF.Ln, bias=c1[:, 0:1], scale=1.0), reads=['ge', 'c1'], writes=['gl'])
            P.op('dve', lambda e: e.tensor_single_scalar(out=gax[:], in_=gx[:], scalar=0.0, op=ALU.min), reads=['gx', 'ge'], writes=['gax'])
            P.op('dve', lambda e: e.tensor_tensor(out=lf[:], in0=gax[:], in1=gl[:], op=ALU.subtract), reads=['gax', 'gl'], writes=['lf'])
            P.op('pe', lambda e, d=d: e.matmul(psG[:, 0:2], lhsT=tri[:, d, :], rhs=lf[:], start=True, stop=True), reads=['tri', 'lf'], writes=['psG'])
            P.op('pe', lambda e: e.matmul(psG[:, 2:4], lhsT=onesm[:], rhs=lf[:], start=True, stop=True), reads=['onesm', 'lf'], writes=['psG'])
            P.op('act', lambda e: e.activation(out=ebq[:], in_=psG[:, 0:2], func=AF.Exp, bias=cln8[:, 0:1], scale=1.0), reads=['psG', 'cln8'], writes=['ebq'])
            P.op('act', lambda e: e.activation(out=EB[:], in_=psG[:, 2:4], func=AF.Exp), reads=['psG'], writes=['EB'])
            P.op('dve', lambda e: e.tensor_tensor(out=gt[:], in0=ig[:], in1=psG[:, 0:2], op=ALU.subtract), reads=['ig', 'psG'], writes=['gt'])
            P.op('act', lambda e: e.activation(out=ek[:], in_=gt[:], func=AF.Exp), reads=['gt'], writes=['ek'])
            for h in range(2):
                P.op('dve', lambda e, h=h: e.tensor_scalar(out=Vt[:, h, 0:128], in0=psA[:, 256 + h * 128:256 + (h + 1) * 128], scalar1=ek[:, h:h + 1],
                                                         scalar2=None, op0=ALU.mult), reads=['psA', 'ek'], writes=['Vt'])
                P.op('dve', lambda e, h=h: e.tensor_copy(out=Vt[:, h, 128:129], in_=ek[:, h:h + 1]), reads=['ek'], writes=['Vt'])
            if is_lat and d == 1:
                P.op('act', lambda e: e.activation(out=osig[:], in_=psB[:, 0:256], func=AF.Sigmoid), reads=['psB'], writes=['osig'])
                P.dma(lambda e, lc=lc: e.dma_start(out=h0[:], in_=H0[lc]), reads=[('H0', lc)], writes=['h0'])
            for h in range(2):
                if is_lat:
                    P.op('pe', lambda e, h=h: e.matmul(psS[:, :], lhsT=qkT[:, 2 + h, :], rhs=qkT[:, h, :], start=True, stop=True), reads=['qkT'], writes=['psS'])
                    P.op('dve', lambda e, d=d: e.tensor_tensor(out=sT[:], in0=psS[:], in1=tri[:, d, :], op=ALU.mult), reads=['psS', 'tri'], writes=['sT'])
                    P.op('pe', lambda e, h=h: e.matmul(psO[:, :], lhsT=sT[:], rhs=Vt[:, h, :], start=True, stop=False), reads=['sT', 'Vt'], writes=['psO'])
                    P.op('pe', lambda e, h=h: e.matmul(psO[:, :], lhsT=qkT[:, h, :], rhs=Cb[:, h, :], start=False, stop=True), reads=['qkT', 'Cb'], writes=['psO'])
                    P.op('act', lambda e, h=h: e.activation(out=dd[:], in_=psO[:, 128:129], func=AF.Abs, scale=ebq[:, h:h + 1]),
                         reads=['psO', 'ebq'], writes=['dd'])
                    P.op('dve', lambda e: e.tensor_single_scalar(out=dd[:], in_=dd[:], scalar=1.0, op=ALU.max), reads=['dd'], writes=['dd'])
                    P.op('dve', lambda e: e.reciprocal(out=rr[:], in_=dd[:]), reads=['dd'], writes=['rr'])
                    P.op('dve', lambda e, h=h: e.tensor_tensor(out=rr[:], in0=rr[:], in1=ebq[:, h:h + 1], op=ALU.mult), reads=['rr', 'ebq'], writes=['rr'])
                    P.op('dve', lambda e, h=h: e.tensor_scalar(out=hout[:, h * 128:(h + 1) * 128], in0=psO[:, 0:128], scalar1=rr[:, 0:1], scalar2=None, op0=ALU.mult),
                         reads=['psO', 'rr'], writes=['hout'])
                P.op('pe', lambda e, h=h: e.matmul(psU[:, :], lhsT=qkb[:, 2 + h, :], rhs=Vt[:, h, :], start=True, stop=True), reads=['qkb', 'Vt'], writes=['psU'])
                P.op('dve', lambda e, h=h: e.tensor_tensor(out=Ct[:], in0=Cf[:, h, :], in1=psU[:], op=ALU.add), reads=['Cf', 'psU'], writes=['Ct'])
                P.op('dve', lambda e, h=h: e.tensor_scalar(out=Cf[:, h, :], in0=Ct[:], scalar1=EB[0:64, h:h + 1], scalar2=None, op0=ALU.mult),
                     reads=['Ct', 'EB'], writes=['Cf'])
                P.op('act', lambda e, h=h: e.activation(out=Cb[:, h, :], in_=Cf[:, h, :], func=AF.Copy), reads=['Cf'], writes=['Cb'])
            if is_lat and d == 0:
                P.dma(lambda e, lc=lc: e.dma_start(out=H0[lc], in_=hout[:]), reads=['hout'], writes=[('H0', lc)])
            if is_lat and d == 1:
                P.op('dve', lambda e: e.tensor_tensor(out=hout[:], in0=hout[:], in1=h0[:], op=ALU.add), reads=['hout', 'h0'], writes=['hout'])
                for h in range(2):
                    P.op('act', lambda e, h=h: e.activation(out=hsq[:], in_=hout[:, h * 128:(h + 1) * 128], func=AF.Square), reads=['hout'], writes=['hsq'])
                    P.op('dve', lambda e, h=h: e.reduce_sum(out=ms[:, h:h + 1], in_=hsq[:], axis=AX.X), reads=['hsq'], writes=['ms'])
                P.op('act', lambda e: e.activation(out=ms[:], in_=ms[:], func=AF.Sqrt, bias=ceps[:, 0:1], scale=1.0 / 128.0), reads=['ms', 'ceps'], writes=['ms'])
                P.op('dve', lambda e: e.reciprocal(out=ms[:], in_=ms[:]), reads=['ms'], writes=['ms'])
                for h in range(2):
                    P.op('dve', lambda e, h=h: e.scalar_tensor_tensor(out=fin[:, h * 128:(h + 1) * 128], in0=hout[:, h * 128:(h + 1) * 128], scalar=ms[:, h:h + 1],
                                                                    in1=ng[:, h * 128:(h + 1) * 128], op0=ALU.mult, op1=ALU.mult),
                         reads=['hout', 'ms', 'ng'], writes=['fin'])
                P.op('dve', lambda e: e.tensor_tensor(out=fin[:], in0=fin[:], in1=osig[:], op=ALU.mult), reads=['fin', 'osig'], writes=['fin'])
                P.dma(lambda e, lc=lc: e.dma_start(out=mix[lc * 128:(lc + 1) * 128, :], in_=fin[:]), reads=['fin'], is_out=True)
    return P.finish()


RW_GN_EPS = 64e-5
NCTX = 2
NLAT = 64
NCH = NCTX + NLAT
NEGM = -30000.0


def na_key_tiles(i):
    rs0 = min(max(2 * i - 4, 0), 120)
    rs1 = min(max(2 * i + 1 - 4, 0), 120)
    return list(range(rs0 // 2, (rs1 + 7) // 2 + 1))


def na_bias_index(h, i, j):
    if 2 <= i <= 61:
        return h * 5 + (j - i + 2)
    e = {0: 0, 1: 1, 62: 2, 63: 3}[i]
    jb = j if i < 2 else j - 60
    return 10 + (e * 4 + jb) * 2 + h


import os


def build_ev():
    SKIP_NA = os.environ.get('SKIP_NA') == '1'
    SKIP_RW = os.environ.get('SKIP_RW') == '1'
    STAGE = int(os.environ.get('EV_STAGE', '9'))
    SUB = int(os.environ.get('EV_SUB', '9'))
    T = NCH * 128
    P = Prog()
    hT = P.inp('hT', [128, 8, T + 2])
    vec_d = P.inp('vec', [128, 8, 4]); WA_d = P.inp('WA', [128, 8, 992]); mu_d = P.inp('mu', [128, 608])
    rep_d = P.inp('rep', [128, 1152]); up_d = P.inp('UP', [128, 512]); gup_d = P.inp('GUP', [128, 128])
    tri_d = P.inp('tri', [2, 2, 128, 128]); id_d = P.inp('ident', [128, 128]); nab_d = P.inp('nab', [42, 128, 128])
    mix = P.outp('mix', [T, 256])
    Y0 = P.scratch('Y0', [NCH, 128, 128])

    def dve(fn, r, w): return P.op('dve', fn, reads=r, writes=w)
    def act(fn, r, w): return P.op('act', fn, reads=r, writes=w)
    def pool(fn, r, w): return P.op('pool', fn, reads=r, writes=w)
    def pe(fn, r, w): return P.op('pe', fn, reads=r, writes=w)

    vec = P.sb('vec_sb', [128, 8, 4]); WAf = P.sb('WAf', [128, 8, 992]); mu = P.sb('mu_sb', [128, 608])
    W1 = P.sb('W1', [128, 8, 992], BF16); W2 = P.sb('W2', [128, 8, 608], BF16); Wt_ = P.sb('Wtmp', [128, 8, 608])
    rep = P.sb('rep_sb', [128, 1152]); UPf = P.sb('UPf', [128, 512]); UPb = P.sb('UPb', [128, 512], BF16)
    GUf = P.sb('GUf', [128, 128]); GUb = P.sb('GUb', [128, 128], BF16)
    tri = P.sb('tri_sb', [128, 4, 128]); idf = P.sb('idf', [128, 128]); idb = P.sb('idb', [128, 128], BF16)
    nab = P.sb('nab_sb', [128, 42, 128])
    sg = P.sb('sg', [128, 128], BF16); onesm = P.sb('onesm', [128, 128]); ones1 = P.sb('ones1', [128, 1]); c1 = P.sb('c1', [128, 1]); cm05 = P.sb('cm05', [128, 1]); ceps = P.sb('ceps', [128, 1])
    P.dma(lambda e: e.dma_start(out=vec[:], in_=vec_d), writes=['vec'])
    P.dma(lambda e: e.dma_start(out=WAf[:], in_=WA_d), writes=['WAf'])
    P.dma(lambda e: e.dma_start(out=mu[:], in_=mu_d), writes=['mu'])
    P.dma(lambda e: e.dma_start(out=rep[:], in_=rep_d), writes=['rep'])
    P.dma(lambda e: e.dma_start(out=UPf[:], in_=up_d), writes=['UPf'])
    P.dma(lambda e: e.dma_start(out=GUf[:], in_=gup_d), writes=['GUf'])
    for d in range(2):
        for m in range(2):
            P.dma(lambda e, d=d, m=m: e.dma_start(out=tri[:, d * 2 + m, :], in_=tri_d[d, m]), writes=['tri'])
    P.dma(lambda e: e.dma_start(out=idf[:], in_=id_d), writes=['idf'])
    nabb = P.sb('nabb', [128, 42, 128], BF16)
    for k0 in range(42):
        P.dma(lambda e, k0=k0: e.dma_start(out=nab[:, k0, :], in_=nab_d[k0]), writes=['nab'])
    pool(lambda e: e.tensor_copy(out=nabb[:], in_=nab[:]), ['nab'], ['nabb'])
    pool(lambda e: e.tensor_copy(out=idb[:], in_=idf[:]), ['idf'], ['idb'])
    pool(lambda e: e.tensor_copy(out=UPb[:], in_=UPf[:]), ['UPf'], ['UPb'])
    pool(lambda e: e.tensor_copy(out=GUb[:], in_=GUf[:]), ['GUf'], ['GUb'])
    pool(lambda e: e.memset(ones1[:], 1.0), [], ['ones1'])
    pool(lambda e: e.memset(sg[:], 0.0), [], ['sg'])
    pool(lambda e: e.memset(onesm[:], 1.0), [], ['onesm'])
    pool(lambda e: e.memset(c1[:], 1.0), [], ['c1'])
    pool(lambda e: e.memset(cm05[:], -0.5), [], ['cm05'])
    pool(lambda e: e.memset(ceps[:], RW_GN_EPS), [], ['ceps'])
    for j in (0, 2):
        dve(lambda e, j=j: e.tensor_scalar(out=vec[:, :, j:j + 1], in0=vec[:, :, j:j + 1], scalar1=1.0, scalar2=None, op0=ALU.add), ['vec'], ['vec'])
    dve(lambda e: e.tensor_tensor(out=Wt_[:], in0=WAf[:, :, 0:608], in1=mu[:].unsqueeze(1).to_broadcast([128, 8, 608]), op=ALU.mult), ['WAf', 'mu'], ['Wt'])
    act(lambda e: e.activation(out=W2[:], in_=Wt_[:], func=AF.Copy, scale=0.5), ['Wt'], ['W2'])
    dve(lambda e: e.tensor_tensor(out=W1[:, :, 0:608], in0=WAf[:, :, 0:608], in1=Wt_[:], op=ALU.subtract), ['WAf', 'Wt'], ['W1'])
    pool(lambda e: e.tensor_copy(out=W1[:, :, 608:992], in_=WAf[:, :, 608:992]), ['WAf'], ['W1'])

    psA = P.ps('psA', [128, 512]); psUP = P.ps('psUP', [128, 512])
    psCB = P.ps('psCB', [128, 512]); psC = psCB[:, 0:384]; psB = psCB[:, 384:512]
    psT = P.ps('psT', [128, 1024], BF16)
    psGG = P.ps('psGG', [128, 4, 128]); psG = psGG[:, 0:2, :]; psG2 = psGG[:, 2:4, :]
    psX = P.ps('psX', [128, 512])
    psM = psX[:, 0:128].rearrange('p (a b) -> p a b', a=2); psS = psX[0:64, 128:256].rearrange('p (a b) -> p a b', a=2)
    psCum = psX[:, 256:384]; psTot = psX[:, 384:512]
    psN = P.ps('psN', [128, 4, 128])
    psVG = P.ps('psVG', [128, 512]); psV = psVG[:, 0:130].rearrange('p (a b) -> p a b', a=2); psGate = psVG[:, 256:384]

    hin = [P.sb(f'hin{i}', [128, 8, 130]) for i in range(2)]
    af = P.sb('af', [128, 8, 130]); ab = P.sb('ab', [128, 8, 128], BF16); sbb = P.sb('sbb', [128, 8, 128], BF16)
    rkv = P.sb('rkv', [128, 384])
    L1 = P.sb('L1', [128, 128], BF16); L1T = P.sb('L1T', [128, 128], BF16)
    sgT = P.sb('sgT', [128, 128], BF16)
    gate = P.sb('gate', [128, 128])
    x_ = P.sb('x_', [128, 128]); ax = P.sb('ax', [128, 128]); ex = P.sb('ex', [128, 128]); lx = P.sb('lx', [128, 128]); mn = P.sb('mn', [128, 128])
    ew = P.sb('ew', [128, 128]); a_d = [P.sb(f'a_d{i}', [128, 128]) for i in range(2)]
    kk = P.sb('kk', [128, 128]); sq = P.sb('sq', [128, 128]); ss = P.sb('ss', [128, 2]); rn = P.sb('rn', [128, 2])
    kd = [P.sb(f'kd{i}', [128, 128]) for i in range(2)]; tmp = P.sb('tmp', [128, 128])
    bt = P.sb('bt', [128, 128])
    eP = P.sb('eP', [128, 128]); eM = P.sb('eM', [128, 128]); eQ = P.sb('eQ', [128, 128]); cme = P.sb('cme', [128, 128])
    ops4 = P.sb('ops4', [128, 4, 128], BF16)
    opT = P.sb('opT', [64, 8, 128], BF16)
    vb = P.sb('vb', [128, 128], BF16)
    eTot = P.sb('eTot', [128, 128]); Dg = P.sb('Dg', [64, 2, 64])
    Mf = [P.sb(f'Mf{i}', [128, 2, 128]) for i in range(2)]; Nf = [P.sb(f'Nf{i}', [128, 2, 128]) for i in range(2)]
    Pm = P.sb('Pm', [128, 2, 128]); Pt = P.sb('Pt', [128, 2, 128])
    AkT = P.sb('AkT', [128, 2, 128], BF16); RbT = P.sb('RbT', [128, 2, 128], BF16); RkT = P.sb('RkT', [128, 2, 128], BF16)
    rhs_f = P.sb('rhs_f', [128, 2, 64]); Ub = P.sb('Ub', [128, 2, 64], BF16)
    Sf = P.sb('Sf', [64, 2, 64]); Sb = P.sb('Sb', [64, 2, 64], BF16); St = P.sb('St', [64, 2, 64])
    yv = P.sb('yv', [128, 128]); y0 = P.sb('y0', [128, 128])
    s1 = P.sb('s1', [128, 2]); yc = P.sb('yc', [128, 128]); s2 = P.sb('s2', [128, 2]); bsum = P.sb('bsum', [128, 2])
    rwout = P.sb('rwout', [128, 128])
    qT = [P.sb(f'qT{i}', [64, 2, 128], BF16) for i in range(8)]
    kT = [P.sb(f'kT{i}', [64, 2, 128], BF16) for i in range(8)]
    vA = [P.sb(f'vA{i}', [128, 2, 66], BF16) for i in range(8)]
    qTc = [P.sb(f'qTc{i}', [64, 2, 128], BF16) for i in range(2)]
    kTc = [P.sb(f'kTc{i}', [64, 2, 128], BF16) for i in range(2)]
    vAc = [P.sb(f'vAc{i}', [128, 2, 66], BF16) for i in range(2)]
    qkb = P.sb('qkb', [128, 256], BF16)
    for i_ in range(8):
        pool(lambda e, i_=i_: e.memset(vA[i_][:], 1.0), [], [f'vA{i_}'])
    for i_ in range(2):
        pool(lambda e, i_=i_: e.memset(vAc[i_][:], 1.0), [], [f'vAc{i_}'])
    pT = P.sb('pT', [128, 4, 128], BF16)
    rden = P.sb('rden', [128, 2]); naout = P.sb('naout', [128, 128])

    def na_block(qt, qkey, ktiles, out_row0, bias_fn):
        groups = [ktiles[i:i + 4] for i in range(0, len(ktiles), 4)]
        for h in range(2):
            first = True
            for gi_, grp in enumerate(groups):
                for jj, (kt_, kkey, va_, vkey, jlat) in enumerate(grp):
                    has_b = jlat is not None
                    pe(lambda e, jj=jj, kt_=kt_, h=h, has_b=has_b: e.matmul(psN[:, jj, :], lhsT=kt_[:, h, :], rhs=qt[:, h, :], start=True, stop=(not has_b)),
                       [kkey, qkey], ['psN'])
                    if has_b:
                        bi = bias_fn(h, jlat)
                        pe(lambda e, jj=jj, bi=bi: e.matmul(psN[:, jj, :], lhsT=idb[:], rhs=nabb[:, bi, :], start=False, stop=True), ['idb', 'nabb'], ['psN'])
                n = len(grp)
                act(lambda e, n=n: e.activation(out=pT[:, 0:n, :], in_=psN[:, 0:n, :], func=AF.Exp), ['psN'], ['pT'])
                for jj, (kt_, kkey, va_, vkey, jlat) in enumerate(grp):
                    last = (gi_ == len(groups) - 1) and (jj == n - 1)
                    pe(lambda e, jj=jj, va_=va_, h=h, first=first, last=last: e.matmul(psV[:, h, :], lhsT=pT[:, jj, :], rhs=va_[:, h, 0:65], start=first, stop=last),
                       ['pT', vkey], ['psVG'])
                    first = False
        dve(lambda e: e.reciprocal(out=rden[:], in_=psV[:, :, 64]), ['psVG'], ['rden'])
        for h in range(2):
            dve(lambda e, h=h: e.tensor_scalar(out=naout[:, h * 64:(h + 1) * 64], in0=psV[:, h, 0:64], scalar1=rden[:, h:h + 1], scalar2=None, op0=ALU.mult),
                ['psVG', 'rden'], ['naout'])
        P.dma(lambda e: e.dma_start(out=mix[out_row0:out_row0 + 128, 0:128], in_=naout[:]), reads=['naout'], is_out=True)

    nload = 0
    for d in range(2):
        TI = 2 * d
        TS = TI + 1
        TSo = 2 * (1 - d) + 1
        pool(lambda e: e.memset(Sf[:], 0.0), [], ['Sf'])
        pool(lambda e: e.memset(Sb[:], 0.0), [], ['Sb'])
        order = list(range(NCH)) if d == 0 else (list(range(NCTX - 1, -1, -1)) + list(range(NCH - 1, NCTX - 1, -1)))
        na_done = set()
        for c in order:
            if STAGE <= 1:
                continue
            is_lat = c >= NCTX
            lc = c - NCTX
            hb = nload % 2
            nload += 1
            col0 = c * 128
            P.dma(lambda e, hb=hb, col0=col0: e.dma_start(out=hin[hb][:], in_=hT[:, :, col0:col0 + 130]), writes=[f'hin{hb}'])
            js, jh = (0, 1) if is_lat else (2, 3)
            dve(lambda e, hb=hb, js=js: e.tensor_tensor(out=af[:], in0=hin[hb][:], in1=vec[:, :, js:js + 1].to_broadcast([128, 8, 130]), op=ALU.mult),
                [f'hin{hb}', 'vec'], ['af'])
            pool(lambda e, jh=jh: e.tensor_tensor(out=af[:], in0=af[:], in1=vec[:, :, jh:jh + 1].to_broadcast([128, 8, 130]), op=ALU.add), ['af', 'vec'], ['af'])
            if c == 0 or c == NCTX:
                pool(lambda e: e.memset(af[:, :, 0:1], 0.0), ['af'], ['af'])
            if c == NCTX - 1 or c == NCH - 1:
                pool(lambda e: e.memset(af[:, :, 129:130], 0.0), ['af'], ['af'])
            act(lambda e: e.activation(out=ab[:], in_=af[:, :, 1:129], func=AF.Copy), ['af'], ['ab'])
            dve(lambda e: e.tensor_tensor(out=sbb[:], in0=af[:, :, 0:128], in1=af[:, :, 2:130], op=ALU.add), ['af'], ['sbb'])
            for kt in range(8):
                pe(lambda e, kt=kt: e.matmul(psA[:, :], lhsT=ab[:, kt, :], rhs=W1[:, kt, 0:512], start=(kt == 0), stop=False), ['ab', 'W1'], ['psA'])
            for kt in range(8):
                pe(lambda e, kt=kt: e.matmul(psA[:, :], lhsT=sbb[:, kt, :], rhs=W2[:, kt, 0:512], start=False, stop=(kt == 7)), ['sbb', 'W2'], ['psA'])
            if d == 1:
                for kt in range(8):
                    pe(lambda e, kt=kt: e.matmul(psB[:, 0:96], lhsT=ab[:, kt, :], rhs=W1[:, kt, 512:608], start=(kt == 0), stop=False), ['ab', 'W1'], ['psCB'])
                for kt in range(8):
                    pe(lambda e, kt=kt: e.matmul(psB[:, 0:96], lhsT=sbb[:, kt, :], rhs=W2[:, kt, 512:608], start=False, stop=(kt == 7)), ['sbb', 'W2'], ['psCB'])
            if d == 0:
                for kt in range(8):
                    pe(lambda e, kt=kt: e.matmul(psC[:, :], lhsT=ab[:, kt, :], rhs=W1[:, kt, 608:992], start=(kt == 0), stop=(kt == 7)), ['ab', 'W1'], ['psCB'])
            if STAGE <= 2:
                continue
            if d == 0:
                if is_lat:
                    slot = lc % 8
                    qt_, kt_t, va_ = qT[slot], kT[slot], vA[slot]
                    qk_, kk_, vk_ = f'qT{slot}', f'kT{slot}', f'vA{slot}'
                else:
                    qt_, kt_t, va_ = qTc[c], kTc[c], vAc[c]
                    qk_, kk_, vk_ = f'qTc{c}', f'kTc{c}', f'vAc{c}'
                act(lambda e: e.activation(out=qkb[:, 0:128], in_=psC[:, 0:128], func=AF.Copy, scale=0.125), ['psCB'], ['qkb'])
                act(lambda e: e.activation(out=qkb[:, 128:256], in_=psC[:, 128:256], func=AF.Copy), ['psCB'], ['qkb'])
                for j in range(4):
                    pe(lambda e, j=j: e.transpose(psT[0:64, j * 128:(j + 1) * 128], qkb[:, j * 64:(j + 1) * 64], idb[:]), ['qkb', 'idb'], ['psT'])
                if SUB >= 2:
                    act(lambda e, qt_=qt_: e.activation(out=qt_[:].rearrange('p a b -> p (a b)'), in_=psT[0:64, 0:256], func=AF.Copy), ['psT'], [qk_])
                    act(lambda e, kt_t=kt_t: e.activation(out=kt_t[:].rearrange('p a b -> p (a b)'), in_=psT[0:64, 256:512], func=AF.Copy), ['psT'], [kk_])
                if SUB >= 3:
                    for h in range(2):
                        act(lambda e, va_=va_, h=h: e.activation(out=va_[:, h, 0:64], in_=psC[:, 256 + h * 64:256 + (h + 1) * 64], func=AF.Copy), ['psCB'], [vk_])
            if d == 0 and not SKIP_NA and STAGE >= 4:
                if c == NCTX - 1:
                    for cq in range(NCTX):
                        kts = [(kTc[j], f'kTc{j}', vAc[j], f'vAc{j}', None) for j in range(NCTX)]
                        na_block(qTc[cq], f'qTc{cq}', kts, cq * 128, None)
                if is_lat:
                    for i in range(NLAT):
                        if i in na_done:
                            continue
                        kl = na_key_tiles(i)
                        if max(kl) > lc:
                            continue
                        na_done.add(i)
                        kts = [(kT[j % 8], f'kT{j % 8}', vA[j % 8], f'vA{j % 8}', j) for j in kl]
                        kts += [(kTc[j], f'kTc{j}', vAc[j], f'vAc{j}', None) for j in range(NCTX)]
                        na_block(qT[i % 8], f'qT{i % 8}', kts, (NCTX + i) * 128, lambda h, j, i=i: na_bias_index(h, i, j))
            if SKIP_RW:
                continue
            act(lambda e: e.activation(out=rkv[:], in_=psA[:, 0:384], func=AF.Copy), ['psA'], ['rkv'])
            act(lambda e: e.activation(out=L1[:, 0:64], in_=psA[:, 384:448], func=AF.Tanh), ['psA'], ['L1'])
            act(lambda e: e.activation(out=L1[:, 64:128], in_=psA[:, 448:512], func=AF.Copy), ['psA'], ['L1'])
            pe(lambda e: e.transpose(psT[:, 512:640], L1[:], idb[:]), ['L1', 'idb'], ['psT'])
            act(lambda e: e.activation(out=L1T[:], in_=psT[:, 512:640], func=AF.Copy), ['psT'], ['L1T'])
            pe(lambda e: e.matmul(psUP[:, :], lhsT=L1T[:], rhs=UPb[:], start=True, stop=True), ['L1T', 'UPb'], ['psUP'])
            if d == 1:
                act(lambda e: e.activation(out=sg[:, 0:96], in_=psB[:, 0:96], func=AF.Sigmoid), ['psCB'], ['sg'])
                pe(lambda e: e.transpose(psT[:, 640:768], sg[:], idb[:]), ['sg', 'idb'], ['psT'])
                act(lambda e: e.activation(out=sgT[:], in_=psT[:, 640:768], func=AF.Copy), ['psT'], ['sgT'])
                pe(lambda e: e.matmul(psGate[:, :], lhsT=sgT[:], rhs=GUb[:], start=True, stop=True), ['sgT', 'GUb'], ['psVG'])
                act(lambda e: e.activation(out=gate[:], in_=psGate[:], func=AF.Copy), ['psVG'], ['gate'])
            dve(lambda e, d=d: e.tensor_tensor(out=x_[:], in0=psUP[:, d * 128:(d + 1) * 128], in1=rep[:, d * 128:(d + 1) * 128], op=ALU.add), ['psUP', 'rep'], ['x_'])
            act(lambda e: e.activation(out=ax[:], in_=x_[:], func=AF.Abs), ['x_'], ['ax'])
            act(lambda e: e.activation(out=ex[:], in_=ax[:], func=AF.Exp, scale=-1.0), ['ax'], ['ex'])
            act(lambda e: e.activation(out=lx[:], in_=ex[:], func=AF.Ln, bias=c1[:, 0:1], scale=1.0), ['ex', 'c1'], ['lx'])
            dve(lambda e: e.tensor_single_scalar(out=mn[:], in_=x_[:], scalar=0.0, op=ALU.min), ['x_'], ['mn'])
            dve(lambda e: e.tensor_tensor(out=mn[:], in0=mn[:], in1=lx[:], op=ALU.subtract), ['mn', 'lx'], ['mn'])
            act(lambda e: e.activation(out=ew[:], in_=mn[:], func=AF.Exp, bias=cm05[:, 0:1], scale=1.0), ['mn', 'cm05'], ['ew'])
            for dd_ in ((d,) if d == 0 else (0, 1)):
                dve(lambda e, dd_=dd_: e.tensor_tensor(out=tmp[:], in0=psUP[:, 256 + dd_ * 128:256 + (dd_ + 1) * 128], in1=rep[:, 256 + dd_ * 128:256 + (dd_ + 1) * 128], op=ALU.add),
                    ['psUP', 'rep'], ['tmp'])
                act(lambda e, dd_=dd_: e.activation(out=a_d[dd_][:], in_=tmp[:], func=AF.Sigmoid), ['tmp'], [f'a_d{dd_}'])
                dve(lambda e, dd_=dd_: e.scalar_tensor_tensor(out=tmp[:], in0=a_d[dd_][:], scalar=-1.0, in1=rep[:, 640:768], op0=ALU.add, op1=ALU.mult),
                    [f'a_d{dd_}', 'rep'], ['tmp'])
                dve(lambda e, dd_=dd_: e.scalar_tensor_tensor(out=kd[dd_][:], in0=tmp[:], scalar=1.0, in1=rkv[:, 128:256], op0=ALU.add, op1=ALU.mult),
                    ['tmp', 'rkv'], [f'kd{dd_}'])
            dve(lambda e: e.tensor_tensor(out=kk[:], in0=rkv[:, 128:256], in1=rep[:, 512:640], op=ALU.mult), ['rkv', 'rep'], ['kk'])
            act(lambda e: e.activation(out=sq[:], in_=kk[:], func=AF.Square), ['kk'], ['sq'])
            dve(lambda e: e.reduce_sum(out=ss[:], in_=sq[:].rearrange('p (a b) -> p a b', a=2), axis=AX.X), ['sq'], ['ss'])
            act(lambda e: e.activation(out=ss[:], in_=ss[:], func=AF.Sqrt), ['ss'], ['ss'])
            dve(lambda e: e.tensor_single_scalar(out=ss[:], in_=ss[:], scalar=1e-12, op=ALU.max), ['ss'], ['ss'])
            dve(lambda e: e.reciprocal(out=rn[:], in_=ss[:]), ['ss'], ['rn'])
            dve(lambda e: e.tensor_tensor(out=kk[:].rearrange('p (a b) -> p a b', a=2), in0=kk[:].rearrange('p (a b) -> p a b', a=2),
                                          in1=rn[:].unsqueeze(2).to_broadcast([128, 2, 64]), op=ALU.mult), ['kk', 'rn'], ['kk'])
            dve(lambda e, d=d: e.tensor_tensor(out=bt[:], in0=kk[:], in1=a_d[d][:], op=ALU.mult), ['kk', f'a_d{d}'], ['bt'])
            pe(lambda e, TI=TI: e.matmul(psCum[:, :], lhsT=tri[:, TI, :], rhs=ew[:], start=True, stop=True), ['tri', 'ew'], ['psX'])
            pe(lambda e: e.matmul(psTot[:, :], lhsT=onesm[:], rhs=ew[:], start=True, stop=True), ['onesm', 'ew'], ['psX'])
            act(lambda e: e.activation(out=eTot[:], in_=psTot[:], func=AF.Exp, scale=-1.0), ['psX'], ['eTot'])
            dve(lambda e: e.tensor_tensor(out=Dg[:], in0=eTot[0:64, :].rearrange('p (a b) -> p a b', a=2), in1=idf[0:64, 0:64].unsqueeze(1).to_broadcast([64, 2, 64]), op=ALU.mult),
                ['eTot', 'idf'], ['Dg'])
            act(lambda e: e.activation(out=eP[:], in_=psCum[:], func=AF.Exp, scale=-1.0), ['psX'], ['eP'])
            act(lambda e: e.activation(out=eM[:], in_=psCum[:], func=AF.Exp), ['psX'], ['eM'])
            dve(lambda e: e.tensor_tensor(out=cme[:], in0=ew[:], in1=psCum[:], op=ALU.subtract), ['ew', 'psX'], ['cme'])
            act(lambda e: e.activation(out=eQ[:], in_=cme[:], func=AF.Exp), ['cme'], ['eQ'])
            dve(lambda e: e.scalar_tensor_tensor(out=ops4[:, 0, :], in0=kk[:], scalar=-1.0, in1=eQ[:], op0=ALU.mult, op1=ALU.mult), ['kk', 'eQ'], ['ops4'])
            dve(lambda e: e.tensor_tensor(out=ops4[:, 1, :], in0=bt[:], in1=eM[:], op=ALU.mult), ['bt', 'eM'], ['ops4'])
            pool(lambda e, d=d: e.tensor_tensor(out=ops4[:, 2, :], in0=kd[d][:], in1=eM[:], op=ALU.mult), [f'kd{d}', 'eM'], ['ops4'])
            pool(lambda e: e.tensor_tensor(out=ops4[:, 3, :], in0=rkv[:, 0:128], in1=eP[:], op=ALU.mult), ['rkv', 'eP'], ['ops4'])
            pool(lambda e: e.tensor_copy(out=vb[:], in_=rkv[:, 256:384]), ['rkv'], ['vb'])
            for o in range(4):
                for h in range(2):
                    pe(lambda e, o=o, h=h: e.transpose(psT[0:64, (o * 2 + h) * 128:(o * 2 + h + 1) * 128], ops4[:, o, h * 64:(h + 1) * 64], idb[:]),
                       ['ops4', 'idb'], ['psT'])
            act(lambda e: e.activation(out=opT[:].rearrange('p a b -> p (a b)'), in_=psT[0:64, :], func=AF.Copy), ['psT'], ['opT'])
            AT = lambda h: opT[:, 0 + h, :]
            BT = lambda h: opT[:, 2 + h, :]
            KT = lambda h: opT[:, 4 + h, :]
            RT = lambda h: opT[:, 6 + h, :]
            for h in range(2):
                pe(lambda e, h=h: e.matmul(psG[:, h, :], lhsT=BT(h), rhs=AT(h), start=True, stop=True), ['opT'], ['psGG'])
                pe(lambda e, h=h: e.matmul(psG2[:, h, :], lhsT=AT(h), rhs=BT(h), start=True, stop=True), ['opT'], ['psGG'])
            dve(lambda e, TS=TS: e.tensor_tensor(out=Mf[0][:], in0=psG[:], in1=tri[:, TS, :].unsqueeze(1).to_broadcast([128, 2, 128]), op=ALU.mult), ['psGG', 'tri'], ['Mf0'])
            dve(lambda e, TSo=TSo: e.tensor_tensor(out=Nf[0][:], in0=psG2[:], in1=tri[:, TSo, :].unsqueeze(1).to_broadcast([128, 2, 128]), op=ALU.mult), ['psGG', 'tri'], ['Nf0'])
            for h in range(2):
                pe(lambda e, h=h: e.matmul(psG[:, h, :], lhsT=KT(h), rhs=AT(h), start=True, stop=True), ['opT'], ['psGG'])
                pe(lambda e, h=h: e.matmul(psG2[:, h, :], lhsT=BT(h), rhs=RT(h), start=True, stop=True), ['opT'], ['psGG'])
            dve(lambda e, TS=TS: e.tensor_tensor(out=AkT[:], in0=psG[:], in1=tri[:, TS, :].unsqueeze(1).to_broadcast([128, 2, 128]), op=ALU.mult), ['psGG', 'tri'], ['AkT'])
            dve(lambda e, TI=TI: e.tensor_tensor(out=RbT[:], in0=psG2[:], in1=tri[:, TI, :].unsqueeze(1).to_broadcast([128, 2, 128]), op=ALU.mult), ['psGG', 'tri'], ['RbT'])
            for h in range(2):
                pe(lambda e, h=h: e.matmul(psG[:, h, :], lhsT=KT(h), rhs=RT(h), start=True, stop=True), ['opT'], ['psGG'])
            dve(lambda e, TI=TI: e.tensor_tensor(out=RkT[:], in0=psG[:], in1=tri[:, TI, :].unsqueeze(1).to_broadcast([128, 2, 128]), op=ALU.mult), ['psGG', 'tri'], ['RkT'])
            dve(lambda e: e.tensor_tensor(out=Pm[:], in0=Mf[0][:], in1=idf[:].unsqueeze(1).to_broadcast([128, 2, 128]), op=ALU.add), ['Mf0', 'idf'], ['Pm'])
            pool(lambda e: e.tensor_tensor(out=Pt[:], in0=Nf[0][:], in1=idf[:].unsqueeze(1).to_broadcast([128, 2, 128]), op=ALU.add), ['Nf0', 'idf'], ['Pt'])
            cur = 0
            for lvl in range(6):
                nxt = 1 - cur
                lastl = lvl == 5
                for h in range(2):
                    pe(lambda e, h=h, cur=cur: e.matmul(psG[:, h, :], lhsT=Nf[cur][:, h, :], rhs=Mf[cur][:, h, :], start=True, stop=True), [f'Nf{cur}', f'Mf{cur}'], ['psGG'])
                act(lambda e, nxt=nxt: e.activation(out=Mf[nxt][:], in_=psG[:], func=AF.Copy), ['psGG'], [f'Mf{nxt}'])
                if not lastl:
                    for h in range(2):
                        pe(lambda e, h=h, cur=cur: e.matmul(psG2[:, h, :], lhsT=Mf[cur][:, h, :], rhs=Nf[cur][:, h, :], start=True, stop=True), [f'Nf{cur}', f'Mf{cur}'], ['psGG'])
                    act(lambda e, nxt=nxt: e.activation(out=Nf[nxt][:], in_=psG2[:], func=AF.Copy), ['psGG'], [f'Nf{nxt}'])
                for h in range(2):
                    pe(lambda e, h=h, nxt=nxt: e.matmul(psG[:, h, :], lhsT=Pt[:, h, :], rhs=Mf[nxt][:, h, :], start=True, stop=True), ['Pt', f'Mf{nxt}'], ['psGG'])
                if not lastl:
                    for h in range(2):
                        pe(lambda e, h=h, nxt=nxt: e.matmul(psG2[:, h, :], lhsT=Mf[nxt][:, h, :], rhs=Pt[:, h, :], start=True, stop=True), ['Pt', f'Mf{nxt}'], ['psGG'])
                dve(lambda e: e.tensor_tensor(out=Pm[:], in0=Pm[:], in1=psG[:], op=ALU.add), ['Pm', 'psGG'], ['Pm'])
                if not lastl:
                    dve(lambda e: e.tensor_tensor(out=Pt[:], in0=Pt[:], in1=psG2[:], op=ALU.add), ['Pt', 'psGG'], ['Pt'])
                cur = nxt
            for h in range(2):
                pe(lambda e, h=h: e.matmul(psM[:, h, :], lhsT=AT(h), rhs=Sb[:, h, :], start=True, stop=False), ['opT', 'Sb'], ['psX'])
                pe(lambda e, h=h: e.matmul(psM[:, h, :], lhsT=AkT[:, h, :], rhs=vb[:, h * 64:(h + 1) * 64], start=False, stop=True), ['AkT', 'vb'], ['psX'])
            act(lambda e: e.activation(out=rhs_f[:], in_=psM[:], func=AF.Copy), ['psX'], ['rhs_f'])
            for h in range(2):
                pe(lambda e, h=h: e.matmul(psM[:, h, :], lhsT=Pm[:, h, :], rhs=rhs_f[:, h, :], start=True, stop=True), ['Pm', 'rhs_f'], ['psX'])
            act(lambda e: e.activation(out=Ub[:], in_=psM[:], func=AF.Copy), ['psX'], ['Ub'])
            for h in range(2):
                pe(lambda e, h=h: e.matmul(psM[:, h, :], lhsT=RT(h), rhs=Sb[:, h, :], start=True, stop=False), ['opT', 'Sb'], ['psX'])
                pe(lambda e, h=h: e.matmul(psM[:, h, :], lhsT=RbT[:, h, :], rhs=Ub[:, h, :], start=False, stop=False), ['RbT', 'Ub'], ['psX'])
                pe(lambda e, h=h: e.matmul(psM[:, h, :], lhsT=RkT[:, h, :], rhs=vb[:, h * 64:(h + 1) * 64], start=False, stop=True), ['RkT', 'vb'], ['psX'])
            act(lambda e: e.activation(out=yv[:], in_=psM[:].rearrange('p a b -> p (a b)'), func=AF.Copy), ['psX'], ['yv'])
            for h in range(2):
                pe(lambda e, h=h: e.matmul(psS[:, h, :], lhsT=ops4[:, 1, h * 64:(h + 1) * 64], rhs=Ub[:, h, :], start=True, stop=False), ['ops4', 'Ub'], ['psX'])
                pe(lambda e, h=h: e.matmul(psS[:, h, :], lhsT=ops4[:, 2, h * 64:(h + 1) * 64], rhs=vb[:, h * 64:(h + 1) * 64], start=False, stop=True), ['ops4', 'vb'], ['psX'])
            dve(lambda e: e.tensor_tensor(out=St[:], in0=Sf[:], in1=psS[:], op=ALU.add), ['Sf', 'psX'], ['St'])
            for h in range(2):
                pe(lambda e, h=h: e.matmul(psS[:, h, :], lhsT=Dg[:, h, :], rhs=St[:, h, :], start=True, stop=True), ['Dg', 'St'], ['psX'])
            act(lambda e: e.activation(out=Sf[:], in_=psS[:], func=AF.Copy), ['psX'], ['Sf'])
            act(lambda e: e.activation(out=Sb[:], in_=Sf[:], func=AF.Copy), ['Sf'], ['Sb'])
            if d == 0:
                P.dma(lambda e, c=c: e.dma_start(out=Y0[c], in_=yv[:]), reads=['yv'], writes=[('Y0', c)])
            else:
                P.dma(lambda e, c=c: e.dma_start(out=y0[:], in_=Y0[c]), reads=[('Y0', c)], writes=['y0'])
                dve(lambda e: e.tensor_tensor(out=yv[:], in0=yv[:], in1=y0[:], op=ALU.add), ['yv', 'y0'], ['yv'])
                y3 = yv[:].rearrange('p (a b) -> p a b', a=2)
                yc3 = yc[:].rearrange('p (a b) -> p a b', a=2)
                dve(lambda e: e.reduce_sum(out=s1[:], in_=y3, axis=AX.X), ['yv'], ['s1'])
                dve(lambda e: e.tensor_single_scalar(out=s1[:], in_=s1[:], scalar=1.0 / 64.0, op=ALU.mult), ['s1'], ['s1'])
                dve(lambda e: e.tensor_tensor(out=yc3, in0=y3, in1=s1[:].unsqueeze(2).to_broadcast([128, 2, 64]), op=ALU.subtract), ['yv', 's1'], ['yc'])
                act(lambda e: e.activation(out=sq[:], in_=yc[:], func=AF.Square), ['yc'], ['sq'])
                dve(lambda e: e.reduce_sum(out=s2[:], in_=sq[:].rearrange('p (a b) -> p a b', a=2), axis=AX.X), ['sq'], ['s2'])
                act(lambda e: e.activation(out=s2[:], in_=s2[:], func=AF.Sqrt, bias=ceps[:, 0:1], scale=1.0 / 64.0), ['s2', 'ceps'], ['s2'])
                dve(lambda e: e.reciprocal(out=s2[:], in_=s2[:]), ['s2'], ['s2'])
                dve(lambda e: e.tensor_tensor(out=yc3, in0=yc3, in1=s2[:].unsqueeze(2).to_broadcast([128, 2, 64]), op=ALU.mult), ['yc', 's2'], ['yc'])
                dve(lambda e: e.tensor_tensor(out=yc[:], in0=yc[:], in1=rep[:, 896:1024], op=ALU.mult), ['yc', 'rep'], ['yc'])
                dve(lambda e: e.tensor_tensor(out=yc[:], in0=yc[:], in1=rep[:, 1024:1152], op=ALU.add), ['yc', 'rep'], ['yc'])
                dve(lambda e: e.tensor_tensor(out=tmp[:], in0=kd[0][:], in1=kd[1][:], op=ALU.add), ['kd0', 'kd1'], ['tmp'])
                dve(lambda e: e.tensor_tensor(out=tmp[:], in0=tmp[:], in1=rkv[:, 0:128], op=ALU.mult), ['tmp', 'rkv'], ['tmp'])
                dve(lambda e: e.tensor_tensor(out=tmp[:], in0=tmp[:], in1=rep[:, 768:896], op=ALU.mult), ['tmp', 'rep'], ['tmp'])
                dve(lambda e: e.reduce_sum(out=bsum[:], in_=tmp[:].rearrange('p (a b) -> p a b', a=2), axis=AX.X), ['tmp'], ['bsum'])
                dve(lambda e: e.tensor_tensor(out=tmp[:].rearrange('p (a b) -> p a b', a=2), in0=rkv[:, 256:384].rearrange('p (a b) -> p a b', a=2),
                                              in1=bsum[:].unsqueeze(2).to_broadcast([128, 2, 64]), op=ALU.mult), ['rkv', 'bsum'], ['tmp'])
                dve(lambda e: e.tensor_tensor(out=yc[:], in0=yc[:], in1=tmp[:], op=ALU.add), ['yc', 'tmp'], ['yc'])
                dve(lambda e: e.tensor_tensor(out=rwout[:], in0=yc[:], in1=gate[:], op=ALU.mult), ['yc', 'gate'], ['rwout'])
                P.dma(lambda e, c=c: e.dma_start(out=mix[c * 128:(c + 1) * 128, 128:256], in_=rwout[:]), reads=['rwout'], is_out=True)
    return P.finish()


def to_fm(a):
    T = a.shape[0]
    return np.ascontiguousarray(a.T.reshape(8, 128, T).transpose(1, 0, 2))

def from_fm(b):
    T = b.shape[2]
    return np.ascontiguousarray(b.transpose(1, 0, 2).reshape(1024, T).T)

def vecfm(v):
    return np.ascontiguousarray(v.reshape(8, 128).T)

def mods_inputs(c, c_ctx, ada_w, ada_b):
    A = np.concatenate([ada_w[0], ada_w[1]], axis=1)
    bvec = np.concatenate([ada_b[0], ada_b[1]], axis=0)
    cmat = np.zeros((4, 1024), np.float32)
    cmat[0] = c[0]; cmat[1] = c[1]; cmat[2] = c_ctx
    cT = np.ascontiguousarray(cmat.T.reshape(8, 128, 4).transpose(1, 0, 2))
    maps = []
    for k in range(8):
        Ak = A[:, k * 1536:(k + 1) * 1536]
        aw = np.ascontiguousarray(Ak.reshape(8, 128, 1536).transpose(1, 0, 2))
        ab = np.ascontiguousarray(bvec[k * 1536:(k + 1) * 1536].reshape(12, 128).T)
        maps.append({'cT': cT, 'aw': aw, 'ab': ab})
    return maps

def mods_assemble(results):
    full = np.zeros((12288, 4), np.float32)
    for k, r in enumerate(results):
        m = r['mods']
        full[k * 1536:(k + 1) * 1536] = m.transpose(1, 0, 2).reshape(1536, 4)
    return np.ascontiguousarray(full.T[:3].reshape(3, 2, 6, 1024).transpose(1, 0, 2, 3))

def moe_weight_maps(l, moe_w_gate, moe_w_up, moe_w_down, sh_w_gate, sh_w_up, sh_w_down):
    maps = []
    for c in range(8):
        g = np.concatenate([moe_w_gate[l, 32 * c:32 * c + 32], sh_w_gate[l][None]], 0)
        u = np.concatenate([moe_w_up[l, 32 * c:32 * c + 32], sh_w_up[l][None]], 0)
        d = np.concatenate([moe_w_down[l, 32 * c:32 * c + 32], sh_w_down[l][None]], 0)
        maps.append({'wg': np.ascontiguousarray(g.reshape(33, 8, 128, 256).transpose(0, 2, 1, 3)),
                     'wu': np.ascontiguousarray(u.reshape(33, 8, 128, 256).transpose(0, 2, 1, 3)),
                     'wd': np.ascontiguousarray(d.reshape(33, 2, 128, 1024).transpose(0, 2, 1, 3))})
    return maps

def moe_wt_maps(Wt_all):
    T_all = Wt_all.shape[0]
    out = []
    for c in range(8):
        w = np.zeros((33, T_all), np.float32)
        w[:32] = Wt_all[:, 32 * c:32 * c + 32].T
        if c == 0:
            w[32] = 1.0
        out.append(w)
    return out

def ml_maps(h_lat, h_ctx, mods, od_w_in, ml_gate_b, ml_norm_g, l=1):
    W = od_w_in[0]
    gbm = ml_gate_b[0]
    t = np.arange(8192)
    inv = (10000.0 ** (-np.arange(16, dtype=np.float32) / 16)).astype(np.float32)
    ang_r = (t // 64).astype(np.float32)[:, None] * inv[None]
    ang_c = (t % 64).astype(np.float32)[:, None] * inv[None]
    cos = np.concatenate([np.cos(ang_r), np.cos(ang_c)], 1).astype(np.float32)
    sin = np.concatenate([np.sin(ang_r), np.sin(ang_c)], 1).astype(np.float32)
    cosT = np.ascontiguousarray(cos.reshape(64, 128, 32).transpose(1, 0, 2))
    sinT = np.ascontiguousarray(sin.reshape(64, 128, 32).transpose(1, 0, 2))
    s_ = np.arange(128)
    tri = np.stack([(s_[:, None] <= s_[None, :]), (s_[:, None] >= s_[None, :])]).astype(np.float32)
    ident = np.eye(128, dtype=np.float32)
    maps = []
    for c in range(8):
        b, g = c // 4, c % 4
        cols = np.concatenate([np.arange(2 * g * 64, (2 * g + 2) * 64), 512 + np.arange(2 * g * 64, (2 * g + 2) * 64),
                               1024 + np.arange(2 * g * 128, (2 * g + 2) * 128), 2048 + np.arange(2 * g * 128, (2 * g + 2) * 128),
                               3072 + np.arange(2 * g, 2 * g + 2), 3080 + np.arange(2 * g, 2 * g + 2),
                               3088 + np.arange(2 * g, 2 * g + 2), 3096 + np.arange(2 * g, 2 * g + 2)])
        Wc = W[:, cols]
        gb = np.concatenate([gbm[0, 2 * g:2 * g + 2], gbm[1, 2 * g:2 * g + 2], gbm[2, 2 * g:2 * g + 2], gbm[3, 2 * g:2 * g + 2]])
        vec = np.stack([vecfm(mods[l, b, 1]), vecfm(mods[l, b, 0]), vecfm(mods[l, 2, 1]), vecfm(mods[l, 2, 0])], -1)
        maps.append({'hT': to_fm(np.concatenate([h_ctx[b], h_lat[b]], 0)), 'vec': np.ascontiguousarray(vec),
                     'W': np.ascontiguousarray(Wc.reshape(8, 128, 776).transpose(1, 0, 2)),
                     'gb': np.ascontiguousarray(np.broadcast_to(gb, (128, 8))), 'cosT': cosT, 'sinT': sinT,
                     'ng': np.ascontiguousarray(np.broadcast_to(ml_norm_g[0][2 * g * 128:(2 * g + 2) * 128], (128, 256))),
                     'tri': tri, 'ident': ident})
    return maps

def ml_assemble(results):
    out = np.zeros((2, 8192, 1024), np.float32)
    for c, r in enumerate(results):
        b, g = c // 4, c % 4
        out[b][:, 2 * g * 128:(2 * g + 2) * 128] = r['mix']
    return out

def _na_bias_index(h, i, j):
    if 2 <= i <= 61:
        return h * 5 + (j - i + 2)
    e = {0: 0, 1: 1, 62: 2, 63: 3}[i]
    jb = j if i < 2 else j - 60
    return 10 + (e * 4 + jb) * 2 + h

def _na_bias_tile(rpb_h, i, j):
    kr = np.arange(2)[:, None, None, None]; jk = np.arange(64)[None, :, None, None]
    qr = np.arange(2)[None, None, :, None]; jq = np.arange(64)[None, None, None, :]
    kr_abs = 2 * j + kr; r = 2 * i + qr
    rs = np.clip(r - 4, 0, 120)
    valid = (kr_abs >= rs) & (kr_abs < rs + 8)
    cs = np.clip(jq - 8, 0, 48)
    valid = valid & (jk >= cs) & (jk < cs + 16)
    ro = np.clip(kr_abs - r + 7, 0, 14); co = np.clip(jk - jq, -15, 15) + 15
    ro, co, valid = np.broadcast_arrays(ro, co, valid)
    val = rpb_h[ro, co]
    return np.where(valid, val, np.float32(-30000.0)).astype(np.float32).reshape(128, 128)

def ev_maps(x, ctx, mods, inp, l=0):
    W = inp['ev_w_in'][0]; mu = inp['rw_mu'][0]
    s_ = np.arange(128)
    tri = np.zeros((2, 2, 128, 128), np.float32)
    tri[0, 0] = s_[:, None] <= s_[None, :]; tri[0, 1] = s_[:, None] < s_[None, :]
    tri[1, 0] = s_[:, None] >= s_[None, :]; tri[1, 1] = s_[:, None] > s_[None, :]
    ident = np.eye(128, dtype=np.float32)
    maps = []
    for c in range(8):
        b, g = c // 4, c % 4
        hc = np.arange(2 * g * 64, (2 * g + 2) * 64)
        cols = np.concatenate([1536 + hc, 2048 + hc, 2560 + hc, np.arange(3072, 3200), np.arange(3200, 3296), hc, 512 + hc, 1024 + hc])
        mucols = np.concatenate([hc, 512 + hc, 1024 + hc, np.arange(1536, 1664), np.arange(1664, 1760)])
        rep = np.concatenate([inp['rw_w0'][0][0][hc], inp['rw_w0'][0][1][hc], inp['rw_a0'][0][0][hc], inp['rw_a0'][0][1][hc],
                              inp['rw_k_k'][0][hc], inp['rw_k_a'][0][hc], inp['rw_r_k'][0].reshape(-1)[hc],
                              inp['rw_gn_g'][0][hc], inp['rw_gn_b'][0][hc]])
        UP = np.zeros((128, 512), np.float32)
        UP[0:32, 0:128] = inp['rw_w_up'][0][0][:, hc]; UP[32:64, 128:256] = inp['rw_w_up'][0][1][:, hc]
        UP[64:96, 256:384] = inp['rw_a_up'][0][0][:, hc]; UP[96:128, 384:512] = inp['rw_a_up'][0][1][:, hc]
        nab = np.zeros((42, 128, 128), np.float32)
        for h in range(2):
            rp = inp['na_rpb'][0][2 * g + h]
            for o in range(-2, 3):
                nab[_na_bias_index(h, 10, 10 + o)] = _na_bias_tile(rp, 10, 10 + o)
            for i in (0, 1):
                for j in range(4):
                    nab[_na_bias_index(h, i, j)] = _na_bias_tile(rp, i, j)
            for i in (62, 63):
                for j in range(60, 64):
                    nab[_na_bias_index(h, i, j)] = _na_bias_tile(rp, i, j)
        hfm = to_fm(np.concatenate([ctx[b], x[b]], 0))
        hpad = np.zeros((128, 8, hfm.shape[2] + 2), np.float32); hpad[:, :, 1:-1] = hfm
        vec = np.stack([vecfm(mods[l, b, 1]), vecfm(mods[l, b, 0]), vecfm(mods[l, 2, 1]), vecfm(mods[l, 2, 0])], -1)
        maps.append({'hT': hpad, 'vec': np.ascontiguousarray(vec), 'WA': np.ascontiguousarray(W[:, cols].reshape(8, 128, 992).transpose(1, 0, 2)),
                     'mu': np.ascontiguousarray(np.broadcast_to(mu[mucols], (128, 608))), 'rep': np.ascontiguousarray(np.broadcast_to(rep, (128, 1152))),
                     'UP': UP, 'GUP': np.ascontiguousarray(np.concatenate([inp['rw_g_up'][0][:, hc], np.zeros((32, 128), np.float32)], 0)), 'tri': tri, 'ident': ident, 'nab': nab})
    return maps

def ev_assemble(results):
    lat = np.zeros((2, 8192, 1024), np.float32); ctx = np.zeros((2, 256, 1024), np.float32)
    for c, r in enumerate(results):
        b, g = c // 4, c % 4
        m = r['mix']
        for dst, rows in ((ctx, slice(0, 256)), (lat, slice(256, 8448))):
            dst[b][:, 2 * g * 64:(2 * g + 2) * 64] = m[rows, 0:128]
            dst[b][:, 512 + 2 * g * 64:512 + (2 * g + 2) * 64] = m[rows, 128:256]
    return lat, ctx


_NC_CACHE = {}


def _get(name, fn):
    if name not in _NC_CACHE:
        _NC_CACHE[name] = fn()
    return _NC_CACHE[name]


def _run(nc, maps):
    return run_bass_kernel_spmd(nc, maps, core_ids=list(range(8))).results


def _ffn_layer(l, last, inputs, mods, h_lat, h_ctx, mix_lat, mix_ctx, w_out):
    TL = 2048
    TCX = 0 if last else 64
    T = TL + TCX
    segs = [(0, TL, 0)] + ([] if last else [(TL, T, 1)])
    woutL = np.ascontiguousarray(w_out.reshape(8, 128, 1024).transpose(1, 0, 2))
    routerL = np.ascontiguousarray(inputs['moe_router'][l].reshape(8, 128, 256).transpose(1, 0, 2))
    rbias = np.ascontiguousarray(np.broadcast_to(inputs['moe_bias'][l], (128, 256)))
    maps = []
    for c in range(8):
        b = c // 4; s = (c % 4) * TL; cs = (c % 4) * 64
        if last:
            xs = h_lat[b, s:s + TL]; ms = mix_lat[b, s:s + TL]
        else:
            xs = np.concatenate([h_lat[b, s:s + TL], h_ctx[b, cs:cs + 64]], 0)
            ms = np.concatenate([mix_lat[b, s:s + TL], mix_ctx[b, cs:cs + 64]], 0)
        vec = np.zeros((128, 8, 8), np.float32)
        vec[:, :, 0] = vecfm(inputs['ln_g'][l, 0]); vec[:, :, 1] = vecfm(inputs['ln_b'][l, 0])
        for m, j in ((0, b), (1, 2)):
            vec[:, :, 2 + 3 * m] = vecfm(mods[l, j, 2]); vec[:, :, 3 + 3 * m] = vecfm(mods[l, j, 3]); vec[:, :, 4 + 3 * m] = vecfm(mods[l, j, 4])
        maps.append({'xT': to_fm(xs), 'mixT': to_fm(ms), 'wout': woutL, 'vec': vec, 'router': routerL, 'rbias': rbias})
    pre = _run(_get(('pre', T), lambda: build_pre(T, segs)), maps)
    fT_all = np.concatenate([pre[c]['fT'] for c in range(8)], axis=2)
    Wt_all = np.concatenate([pre[c]['Wt'] for c in range(8)], axis=0)
    T_all = 8 * T
    wm = moe_weight_maps(l, inputs['moe_w_gate'], inputs['moe_w_up'], inputs['moe_w_down'],
                         inputs['sh_w_gate'], inputs['sh_w_up'], inputs['sh_w_down'])
    wt = moe_wt_maps(Wt_all)
    moe = _run(_get(('moe', T_all), lambda: build_moe(T_all, T)), [dict(wm[c], fT=fT_all, WtT=wt[c]) for c in range(8)])
    del wm
    parts = np.stack([moe[c]['part'] for c in range(8)])
    maps = []
    for c in range(8):
        b = c // 4
        vec = np.zeros((128, 8, 4), np.float32)
        vec[:, :, 0] = vecfm(inputs['ln_g'][l, 1]); vec[:, :, 1] = vecfm(inputs['ln_b'][l, 1])
        vec[:, :, 2] = vecfm(mods[l, b, 5]); vec[:, :, 3] = vecfm(mods[l, 2, 5])
        maps.append({'h1T': pre[c]['h1T'], 'parts': np.ascontiguousarray(parts[:, :, :, c * T:(c + 1) * T]), 'vec': vec})
    post = _run(_get(('post', T), lambda: build_post(T, segs)), maps)
    o_lat = np.zeros((2, 8192, 1024), np.float32)
    o_ctx = None if last else np.zeros((2, 256, 1024), np.float32)
    for c in range(8):
        b = c // 4; s = (c % 4) * TL; cs = (c % 4) * 64
        h2 = from_fm(post[c]['h2T'])
        o_lat[b, s:s + TL] = h2[:TL]
        if not last:
            o_ctx[b, cs:cs + 64] = h2[TL:]
    return o_lat, o_ctx


def kernel(**inputs):
    inputs = {k: np.asarray(v) for k, v in inputs.items()}
    x, ctx = inputs['x'], inputs['ctx']
    mods = mods_assemble(_run(_get('mods', build_mods), mods_inputs(inputs['c'], inputs['c_ctx'], inputs['ada_w'], inputs['ada_b'])))
    ev = _run(_get('ev', build_ev), ev_maps(x, ctx, mods, inputs))
    mix_lat, mix_ctx = ev_assemble(ev)
    h_lat, h_ctx = _ffn_layer(0, False, inputs, mods, x, ctx, mix_lat, mix_ctx, inputs['ev_w_out'][0])
    ml = _run(_get('ml', build_ml), ml_maps(h_lat, h_ctx, mods, inputs['od_w_in'], inputs['ml_gate_b'], inputs['ml_norm_g']))
    mix_lat = ml_assemble(ml)
    h_lat, _ = _ffn_layer(1, True, inputs, mods, h_lat, None, mix_lat, None, inputs['od_w_out'][0])
    return h_lat.astype(np.float32)
```

```python
import math


import numpy as np
from contextlib import ExitStack
import concourse.bass as bass
import concourse.mybir as mybir
from concourse.bass_utils import run_bass_kernel_spmd

F32 = mybir.dt.float32
BF16 = mybir.dt.bfloat16
I32 = mybir.dt.int32
U32 = mybir.dt.uint32
AF = mybir.ActivationFunctionType
ALU = mybir.AluOpType
AX = mybir.AxisListType

ENGS = ['pe', 'act', 'dve', 'pool', 'sp']
NDSEM = 24


class Prog:
    def __init__(self, same_engine_sync=True):
        self.nc = bass.Bass('TRN2', target_bir_lowering=False)
        self.stack = ExitStack()
        self.q = {e: [] for e in ENGS}
        self.cnt = {e: 0 for e in ENGS}
        self.seen = {e: {} for e in ENGS}
        self.lastw = {}
        self.readers = {}
        self.ndma = 0
        self.dslot_last = {}
        self.same_engine_sync = same_engine_sync
        self.sems = {}
        self.uid = 0
        self.out_events = []

    def dram(self, name, shape, dtype, kind):
        return self.nc.dram_tensor(name, list(shape), dtype, kind=kind).ap()

    def inp(self, name, shape, dtype=F32):
        return self.dram(name, shape, dtype, 'ExternalInput')

    def outp(self, name, shape, dtype=F32):
        return self.dram(name, shape, dtype, 'ExternalOutput')

    def scratch(self, name, shape, dtype=F32, addr_space=None):
        if addr_space is None:
            return self.dram(name, shape, dtype, 'Internal')
        return self.nc.dram_tensor(name, list(shape), dtype, kind='Internal', addr_space=addr_space).ap()

    def sb(self, name, shape, dtype=F32):
        t = self.stack.enter_context(self.nc.sbuf_tensor(name, list(shape), dtype))
        return t

    def ps(self, name, shape, dtype=F32):
        t = self.stack.enter_context(self.nc.psum_tensor(name, list(shape), dtype))
        return t

    def _deps(self, eng, reads, writes):
        deps = {}

        def add(ev):
            k, v = ev
            if deps.get(k, 0) < v:
                deps[k] = v
        for k in reads:
            if k in self.lastw:
                add(self.lastw[k])
        for k in writes:
            if k in self.lastw:
                add(self.lastw[k])
            for ev in self.readers.get(k, {}).items():
                add(ev)
        waits = []
        for k, v in deps.items():
            if k == eng and (eng == 'pe' or not self.same_engine_sync):
                continue
            if self.seen[eng].get(k, 0) >= v:
                continue
            self.seen[eng][k] = v
            waits.append((k, v))
        return waits

    def _record(self, ev, reads, writes):
        for k in writes:
            self.lastw[k] = ev
            self.readers[k] = {}
        for k in reads:
            if k in writes:
                continue
            r = self.readers.setdefault(k, {})
            if r.get(ev[0], 0) < ev[1]:
                r[ev[0]] = ev[1]

    def op(self, eng, fn, reads=(), writes=()):
        waits = self._deps(eng, reads, writes)
        self.cnt[eng] += 1
        ev = (eng, self.cnt[eng])
        self.q[eng].append((waits, fn, eng, 1))
        self._record(ev, reads, writes)
        return ev

    def dma(self, fn, reads=(), writes=(), queue='sp', is_out=False):
        slot = (queue, self.ndma % NDSEM)
        self.ndma += 1
        key = ('d',) + slot
        prev = self.dslot_last.get(key, 0)
        waits = self._deps(queue, reads, writes)
        if prev and self.seen[queue].get(key, 0) < prev:
            self.seen[queue][key] = prev
            waits.append((key, prev))
        val = prev + 16
        self.dslot_last[key] = val
        ev = (key, val)
        self.q[queue].append((waits, fn, key, 16))
        self._record(ev, reads, writes)
        if is_out:
            self.out_events.append(ev)
        return ev

    def _sem(self, key):
        if key not in self.sems:
            nm = 's_' + '_'.join(str(x) for x in (key if isinstance(key, tuple) else (key,)))
            self.sems[key] = self.stack.enter_context(self.nc.semaphore(nm))
        return self.sems[key]

    def finish(self):
        nc = self.nc
        fin = {}
        for k, v in self.out_events:
            fin[k] = max(fin.get(k, 0), v)
        self.q['sp'].append(([(k, v) for k, v in fin.items()], None, None, 0))
        for e in ENGS:
            self._sem(e)
            for waits, fn, semkey, inc in self.q[e]:
                for k, v in waits:
                    self._sem(k)
                if semkey is not None:
                    self._sem(semkey)
        block = self.stack.enter_context(nc.Block())
        hmap = {'pe': 'tensor', 'act': 'scalar', 'dve': 'vector', 'pool': 'gpsimd', 'sp': 'sync'}

        def make(e):
            def body(h):
                for waits, fn, semkey, inc in self.q[e]:
                    for k, v in waits:
                        h.wait_ge(self.sems[k], v)
                    if fn is not None:
                        fn(h).then_inc(self.sems[semkey], inc)
            return body
        for e in ENGS:
            if self.q[e]:
                getattr(block, hmap[e])(make(e))
        self.stack.close()
        return nc


def run(prog_or_nc, in_maps, n=None, trace=False):
    nc = prog_or_nc
    n = n or len(in_maps)
    return run_bass_kernel_spmd(nc, in_maps, core_ids=list(range(n)), trace=trace)


ALPHA = (2.0 * 2) ** 0.25
CW = 256
LN_EPS = 1e-5


def bcast_mid(ap2d, n):
    P_, w = ap2d.shape
    return ap2d.unsqueeze(1).to_broadcast([P_, n, w])


def build_mods():
    P = Prog()
    cT = P.inp('cT', [128, 8, 4])
    aw = P.inp('aw', [128, 8, 1536])
    ab = P.inp('ab', [128, 12])
    out = P.outp('mods', [128, 12, 4])
    c_sb = P.sb('c_sb', [128, 8, 4]); s_sb = P.sb('s_sb', [128, 8, 4])
    aw_sb = P.sb('aw_sb', [128, 8, 1536]); ab_sb = P.sb('ab_sb', [128, 12])
    o_sb = P.sb('o_sb', [128, 12, 4])
    ps = P.ps('ps', [128, 12, 4])
    P.dma(lambda e: e.dma_start(out=c_sb[:], in_=cT), writes=['c'])
    P.dma(lambda e: e.dma_start(out=aw_sb[:], in_=aw), writes=['aw'])
    P.dma(lambda e: e.dma_start(out=ab_sb[:], in_=ab), writes=['ab'])
    P.op('act', lambda e: e.activation(out=s_sb[:], in_=c_sb[:], func=AF.Silu), reads=['c'], writes=['s'])
    for ft in range(12):
        for kt in range(8):
            P.op('pe', lambda e, ft=ft, kt=kt: e.matmul(ps[:, ft, :], lhsT=aw_sb[:, kt, ft * 128:(ft + 1) * 128],
                                                         rhs=s_sb[:, kt, :], start=(kt == 0), stop=(kt == 7)),
                 reads=['aw', 's'], writes=['ps'])
    for ft in range(12):
        P.op('act', lambda e, ft=ft: e.activation(out=o_sb[:, ft, :], in_=ps[:, ft, :], func=AF.Identity,
                                                   bias=ab_sb[:, ft:ft + 1], scale=1.0),
             reads=['ps', 'ab'], writes=['o'])
    P.dma(lambda e: e.dma_start(out=out, in_=o_sb[:]), reads=['o'], is_out=True)
    return P.finish()


def emit_ln(P, z, zsq, w, consts, tag, out_h, vec, gi, bi):
    onesD, eps_t, ps_m, ps_q, mean_sb, t1, rstd, tt = consts
    P.op('act', lambda e: e.activation(out=zsq[:, :, :w], in_=z[:, :, :w], func=AF.Square), reads=['z' + tag], writes=['zsq'])
    for ft in range(8):
        P.op('pe', lambda e, ft=ft: e.matmul(ps_m[:, :w], lhsT=onesD[:], rhs=z[:, ft, :w], start=(ft == 0), stop=(ft == 7)),
             reads=['z' + tag, 'onesD'], writes=['ps_m'])
    for ft in range(8):
        P.op('pe', lambda e, ft=ft: e.matmul(ps_q[:, :w], lhsT=onesD[:], rhs=zsq[:, ft, :w], start=(ft == 0), stop=(ft == 7)),
             reads=['zsq', 'onesD'], writes=['ps_q'])
    P.op('act', lambda e: e.activation(out=mean_sb[:, :w], in_=ps_m[:, :w], func=AF.Copy), reads=['ps_m'], writes=['mean'])
    P.op('dve', lambda e: e.tensor_tensor(out=t1[:, :w], in0=mean_sb[:, :w], in1=mean_sb[:, :w], op=ALU.mult), reads=['mean'], writes=['t1'])
    P.op('dve', lambda e: e.tensor_tensor(out=t1[:, :w], in0=ps_q[:, :w], in1=t1[:, :w], op=ALU.subtract), reads=['ps_q', 't1'], writes=['t1'])
    P.op('act', lambda e: e.activation(out=t1[:, :w], in_=t1[:, :w], func=AF.Sqrt, bias=eps_t[:, 0:1], scale=1.0), reads=['t1', 'eps'], writes=['t1'])
    P.op('dve', lambda e: e.reciprocal(out=rstd[:, :w], in_=t1[:, :w]), reads=['t1'], writes=['rstd'])
    P.op('dve', lambda e: e.tensor_tensor(out=tt[:, :, :w], in0=z[:, :, :w], in1=bcast_mid(mean_sb[:, :w], 8), op=ALU.subtract),
         reads=['z' + tag, 'mean'], writes=['tt'])
    P.op('dve', lambda e: e.tensor_tensor(out=tt[:, :, :w], in0=tt[:, :, :w], in1=bcast_mid(rstd[:, :w], 8), op=ALU.mult),
         reads=['tt', 'rstd'], writes=['tt'])
    for ft in range(8):
        P.op('act', lambda e, ft=ft: e.activation(out=out_h[:, ft, :w], in_=tt[:, ft, :w], func=AF.Identity,
                                                   scale=vec[:, ft, gi:gi + 1], bias=vec[:, ft, bi:bi + 1]),
             reads=['tt', 'vec'], writes=['h' + tag])


def ln_consts(P):
    onesD = P.sb('onesD', [128, 128]); eps_t = P.sb('eps_t', [128, 1])
    ps_m = P.ps('ps_m', [128, 512]); ps_q = P.ps('ps_q', [128, 512])
    mean_sb = P.sb('mean_sb', [128, 512]); t1 = P.sb('t1', [128, 512]); rstd = P.sb('rstd', [128, 512])
    tt = P.sb('tt', [128, 8, CW])
    P.op('pool', lambda e: e.memset(onesD[:], 1.0 / 1024.0), writes=['onesD'])
    P.op('pool', lambda e: e.memset(eps_t[:], LN_EPS), writes=['eps'])
    return (onesD, eps_t, ps_m, ps_q, mean_sb, t1, rstd, tt)


def chunks_of(segs):
    out = []
    for (a, b, m) in segs:
        c = a
        while c < b:
            w = min(CW, b - c)
            out.append((c, w, m))
            c += w
    return out


def build_pre(T, segs):
    P = Prog()
    xT = P.inp('xT', [128, 8, T]); mixT = P.inp('mixT', [128, 8, T])
    wout = P.inp('wout', [128, 8, 1024]); vec_d = P.inp('vec', [128, 8, 8])
    router = P.inp('router', [128, 8, 256]); rbias = P.inp('rbias', [128, 256])
    h1T = P.outp('h1T', [128, 8, T]); fT = P.outp('fT', [128, 8, T], BF16); Wt = P.outp('Wt', [T, 256])
    wo_f = P.sb('wo_f', [128, 8, 1024]); wo_b = P.sb('wo_b', [128, 8, 1024], BF16)
    vec = P.sb('vec_sb', [128, 8, 8]); rt = P.sb('rt', [128, 8, 256]); rb = P.sb('rb', [128, 256])
    consts = ln_consts(P)
    P.dma(lambda e: e.dma_start(out=wo_f[:], in_=wout), writes=['wo_f'])
    P.dma(lambda e: e.dma_start(out=vec[:], in_=vec_d), writes=['vec'])
    P.dma(lambda e: e.dma_start(out=rt[:], in_=router), writes=['rt'])
    P.dma(lambda e: e.dma_start(out=rb[:], in_=rbias), writes=['rb'])
    P.op('pool', lambda e: e.tensor_copy(out=wo_b[:], in_=wo_f[:]), reads=['wo_f'], writes=['wo_b'])
    for m in range(2):
        P.op('dve', lambda e, m=m: e.tensor_scalar(out=vec[:, :, 4 + 3 * m:5 + 3 * m], in0=vec[:, :, 4 + 3 * m:5 + 3 * m],
                                                   scalar1=1.0, scalar2=None, op0=ALU.add), reads=['vec'], writes=['vec'])
    mx = [P.sb(f'mx{i}', [128, 8, CW]) for i in range(2)]
    xx = [P.sb(f'xx{i}', [128, 8, CW]) for i in range(2)]
    mxb = P.sb('mxb', [128, 8, CW], BF16)
    z = P.sb('z', [128, 8, CW]); zsq = P.sb('zsq', [128, 8, CW])
    h1 = P.sb('h1', [128, 8, CW]); f32t = P.sb('f32t', [128, 8, CW]); fb = P.sb('fb', [128, 8, CW], BF16)
    ps_y = [P.ps(f'ps_y{i}', [128, 512]) for i in range(2)]
    ps_r = P.ps('ps_r', [128, 256])
    s_sb = P.sb('s_sb', [128, 256]); grp = P.sb('grp', [128, 256]); m8 = P.sb('m8', [128, 8, 8])
    gs = P.sb('gs', [128, 8]); g8 = P.sb('g8', [128, 8]); keep = P.sb('keep', [128, 8]); pen = P.sb('pen', [128, 8])
    choice = P.sb('choice', [128, 256]); c8 = P.sb('c8', [128, 8]); sel = P.sb('sel', [128, 256])
    wv = P.sb('wv', [128, 256]); wsum = P.sb('wsum', [128, 1]); rinv = P.sb('rinv', [128, 1]); wt_sb = P.sb('wt_sb', [128, 256])
    for ci, (c0, w, m) in enumerate(chunks_of(segs)):
        b = ci % 2
        P.dma(lambda e, b=b, c0=c0, w=w: e.dma_start(out=mx[b][:, :, :w], in_=mixT[:, :, c0:c0 + w]), writes=[f'mx{b}'])
        P.dma(lambda e, b=b, c0=c0, w=w: e.dma_start(out=xx[b][:, :, :w], in_=xT[:, :, c0:c0 + w]), writes=[f'xx{b}'])
        P.op('pool', lambda e, b=b, w=w: e.tensor_copy(out=mxb[:, :, :w], in_=mx[b][:, :, :w]), reads=[f'mx{b}'], writes=['mxb'])
        P.op('act', lambda e, b=b, w=w: e.activation(out=z[:, :, :w], in_=xx[b][:, :, :w], func=AF.Copy, scale=ALPHA),
             reads=[f'xx{b}'], writes=['z'])
        for ft in range(8):
            pb = ft % 2
            for kt in range(8):
                P.op('pe', lambda e, pb=pb, kt=kt, ft=ft, w=w: e.matmul(ps_y[pb][:, :w], lhsT=wo_b[:, kt, ft * 128:(ft + 1) * 128],
                                                                       rhs=mxb[:, kt, :w], start=(kt == 0), stop=(kt == 7)),
                     reads=['wo_b', 'mxb'], writes=[f'ps_y{pb}'])
            P.op('dve', lambda e, pb=pb, ft=ft, w=w, m=m: e.scalar_tensor_tensor(
                out=z[:, ft, :w], in0=ps_y[pb][:, :w], scalar=vec[:, ft, 2 + 3 * m:3 + 3 * m], in1=z[:, ft, :w],
                op0=ALU.mult, op1=ALU.add), reads=[f'ps_y{pb}', 'vec', 'z'], writes=['z'])
        emit_ln(P, z, zsq, w, consts, '', h1, vec, 0, 1)
        for ft in range(8):
            P.op('act', lambda e, ft=ft, w=w, m=m: e.activation(out=f32t[:, ft, :w], in_=h1[:, ft, :w], func=AF.Identity,
                                                                scale=vec[:, ft, 4 + 3 * m:5 + 3 * m], bias=vec[:, ft, 3 + 3 * m:4 + 3 * m]),
                 reads=['h', 'vec'], writes=['f32t'])
        P.op('pool', lambda e, w=w: e.tensor_copy(out=fb[:, :, :w], in_=f32t[:, :, :w]), reads=['f32t'], writes=['fb'])
        P.dma(lambda e, c0=c0, w=w: e.dma_start(out=h1T[:, :, c0:c0 + w], in_=h1[:, :, :w]), reads=['h'], is_out=True)
        P.dma(lambda e, c0=c0, w=w: e.dma_start(out=fT[:, :, c0:c0 + w], in_=fb[:, :, :w]), reads=['fb'], is_out=True)
        for s0 in range(0, w, 128):
            n = min(128, w - s0)
            for kt in range(8):
                P.op('pe', lambda e, kt=kt, s0=s0, n=n: e.matmul(ps_r[:n, :], lhsT=f32t[:, kt, s0:s0 + n], rhs=rt[:, kt, :],
                                                                start=(kt == 0), stop=(kt == 7)),
                     reads=['f32t', 'rt'], writes=['ps_r'])
            P.op('act', lambda e, n=n: e.activation(out=s_sb[:n, :], in_=ps_r[:n, :], func=AF.Sigmoid), reads=['ps_r'], writes=['s_sb'])
            P.op('dve', lambda e, n=n: e.tensor_tensor(out=grp[:n, :], in0=s_sb[:n, :], in1=rb[:n, :], op=ALU.add),
                 reads=['s_sb', 'rb'], writes=['grp'])
            for gi in range(8):
                P.op('dve', lambda e, n=n, gi=gi: e.max(out=m8[:n, gi, :], in_=grp[:n, gi * 32:(gi + 1) * 32]), reads=['grp'], writes=['m8'])
            P.op('dve', lambda e, n=n: e.tensor_tensor(out=gs[:n, :], in0=m8[:n, :, 0], in1=m8[:n, :, 1], op=ALU.add), reads=['m8'], writes=['gs'])
            P.op('dve', lambda e, n=n: e.max(out=g8[:n, :], in_=gs[:n, :]), reads=['gs'], writes=['g8'])
            P.op('dve', lambda e, n=n: e.tensor_scalar(out=keep[:n, :], in0=gs[:n, :], scalar1=g8[:n, 3:4], scalar2=None, op0=ALU.is_ge),
                 reads=['gs', 'g8'], writes=['keep'])
            P.op('dve', lambda e, n=n: e.tensor_scalar(out=pen[:n, :], in0=keep[:n, :], scalar1=1.0, scalar2=1e30, op0=ALU.subtract, op1=ALU.mult),
                 reads=['keep'], writes=['pen'])
            for gi in range(8):
                P.op('dve', lambda e, n=n, gi=gi: e.tensor_scalar(out=choice[:n, gi * 32:(gi + 1) * 32], in0=grp[:n, gi * 32:(gi + 1) * 32],
                                                                 scalar1=keep[:n, gi:gi + 1], scalar2=pen[:n, gi:gi + 1], op0=ALU.mult, op1=ALU.add),
                     reads=['grp', 'keep', 'pen'], writes=['choice'])
            P.op('dve', lambda e, n=n: e.max(out=c8[:n, :], in_=choice[:n, :]), reads=['choice'], writes=['c8'])
            P.op('dve', lambda e, n=n: e.tensor_scalar(out=sel[:n, :], in0=choice[:n, :], scalar1=c8[:n, 7:8], scalar2=None, op0=ALU.is_ge),
                 reads=['choice', 'c8'], writes=['sel'])
            P.op('dve', lambda e, n=n: e.tensor_tensor(out=wv[:n, :], in0=s_sb[:n, :], in1=sel[:n, :], op=ALU.mult), reads=['s_sb', 'sel'], writes=['wv'])
            P.op('dve', lambda e, n=n: e.reduce_sum(out=wsum[:n, :], in_=wv[:n, :], axis=AX.X), reads=['wv'], writes=['wsum'])
            P.op('dve', lambda e, n=n: e.reciprocal(out=rinv[:n, :], in_=wsum[:n, :]), reads=['wsum'], writes=['rinv'])
            P.op('dve', lambda e, n=n: e.tensor_scalar(out=wt_sb[:n, :], in0=wv[:n, :], scalar1=rinv[:n, 0:1], scalar2=2.5, op0=ALU.mult, op1=ALU.mult),
                 reads=['wv', 'rinv'], writes=['wt_sb'])
            P.dma(lambda e, n=n, r0=c0 + s0: e.dma_start(out=Wt[r0:r0 + n, :], in_=wt_sb[:n, :]), reads=['wt_sb'], is_out=True)
    return P.finish()


def build_moe(T_all, TC, NE=33):
    assert T_all % TC == 0
    P = Prog()
    fT = P.inp('fT', [128, 8, T_all], BF16); WtT = P.inp('WtT', [NE, T_all])
    wg = P.inp('wg', [NE, 128, 8, 256]); wu = P.inp('wu', [NE, 128, 8, 256]); wd = P.inp('wd', [NE, 128, 2, 1024])
    part = P.outp('part', [128, 8, T_all])
    wgb_d = P.scratch('wgb_d', [NE, 128, 8, 256], BF16); wub_d = P.scratch('wub_d', [NE, 128, 8, 256], BF16)
    wdb_d = P.scratch('wdb_d', [NE, 128, 2, 1024], BF16)
    st_f = [P.sb(f'st_f{i}', [128, 2048]) for i in range(2)]
    st_b = [P.sb(f'st_b{i}', [128, 2048], BF16) for i in range(2)]
    k = 0
    for e_ in range(NE):
        for (src, dst) in ((wg, wgb_d), (wu, wub_d), (wd, wdb_d)):
            b = k % 2
            sflat = src[e_].rearrange('p a b -> p (a b)')
            dflat = dst[e_].rearrange('p a b -> p (a b)')
            P.dma(lambda e, b=b, sflat=sflat: e.dma_start(out=st_f[b][:], in_=sflat), writes=[f'st_f{b}'])
            eng = 'pool' if k % 2 == 0 else 'act'
            if eng == 'pool':
                P.op('pool', lambda e, b=b: e.tensor_copy(out=st_b[b][:], in_=st_f[b][:]), reads=[f'st_f{b}'], writes=[f'st_b{b}'])
            else:
                P.op('act', lambda e, b=b: e.activation(out=st_b[b][:], in_=st_f[b][:], func=AF.Copy), reads=[f'st_f{b}'], writes=[f'st_b{b}'])
            P.dma(lambda e, b=b, dflat=dflat: e.dma_start(out=dflat, in_=st_b[b][:]), reads=[f'st_b{b}'], writes=[('wbd', e_, id(dst))])
            k += 1
    ft_sb = [P.sb(f'ft_sb{i}', [128, 8, TC], BF16) for i in range(1)]
    acc = P.sb('acc', [128, 8, TC])
    wgb = [P.sb(f'wgb{i}', [128, 8, 256], BF16) for i in range(2)]
    wub = [P.sb(f'wub{i}', [128, 8, 256], BF16) for i in range(2)]
    wdb = [P.sb(f'wdb{i}', [128, 2, 1024], BF16) for i in range(2)]
    wrow = [P.sb(f'wrow{i}', [1, TC]) for i in range(2)]
    ones1 = P.sb('ones1', [1, 128])
    P.op('pool', lambda e: e.memset(ones1[:], 1.0), writes=['ones1'])
    actb = P.sb('actb', [128, 2, TC], BF16)
    sg = [P.sb(f'sg{i}', [128, 512]) for i in range(2)]
    ps_g = [P.ps(f'ps_g{i}', [128, 512]) for i in range(2)]
    ps_u = [P.ps(f'ps_u{i}', [128, 512]) for i in range(2)]
    ps_w = P.ps('ps_w', [128, 512])
    ps_o = [P.ps(f'ps_o{i}', [128, 512]) for i in range(3)]
    otmp = [P.sb(f'otmp{i}', [128, 512]) for i in range(2)]
    cols = [(c, min(512, TC - c)) for c in range(0, TC, 512)]
    it = 0
    for tc in range(T_all // TC):
        t0 = tc * TC
        fb_ = 0
        P.dma(lambda e, fb_=fb_, t0=t0: e.dma_start(out=ft_sb[fb_][:], in_=fT[:, :, t0:t0 + TC]), writes=[f'ft_sb{fb_}'])
        for e_ in range(NE):
            b = it % 2
            it += 1
            P.dma(lambda e, b=b, e_=e_: e.dma_start(out=wgb[b][:], in_=wgb_d[e_]), reads=[('wbd', e_, id(wgb_d))], writes=[f'wgb{b}'])
            P.dma(lambda e, b=b, e_=e_: e.dma_start(out=wub[b][:], in_=wub_d[e_]), reads=[('wbd', e_, id(wub_d))], writes=[f'wub{b}'])
            P.dma(lambda e, b=b, e_=e_: e.dma_start(out=wdb[b][:], in_=wdb_d[e_]), reads=[('wbd', e_, id(wdb_d))], writes=[f'wdb{b}'])
            P.dma(lambda e, b=b, e_=e_, t0=t0: e.dma_start(out=wrow[b][:], in_=WtT[e_:e_ + 1, t0:t0 + TC]), writes=[f'wrow{b}'])
            q = 0
            for j in range(2):
                for (c0, w) in cols:
                    pb = q % 2
                    q += 1
                    for kt in range(8):
                        P.op('pe', lambda e, pb=pb, kt=kt, j=j, c0=c0, w=w, b=b, fb_=fb_: e.matmul(
                            ps_g[pb][:, :w], lhsT=wgb[b][:, kt, j * 128:(j + 1) * 128], rhs=ft_sb[fb_][:, kt, c0:c0 + w],
                            start=(kt == 0), stop=(kt == 7)), reads=[f'wgb{b}', f'ft_sb{fb_}'], writes=[f'ps_g{pb}'])
                    for kt in range(8):
                        P.op('pe', lambda e, pb=pb, kt=kt, j=j, c0=c0, w=w, b=b, fb_=fb_: e.matmul(
                            ps_u[pb][:, :w], lhsT=wub[b][:, kt, j * 128:(j + 1) * 128], rhs=ft_sb[fb_][:, kt, c0:c0 + w],
                            start=(kt == 0), stop=(kt == 7)), reads=[f'wub{b}', f'ft_sb{fb_}'], writes=[f'ps_u{pb}'])
                    if j == 0:
                        pass
                    P.op('pe', lambda e, c0=c0, w=w, b=b: e.matmul(ps_w[:, :w], lhsT=ones1[:, :], rhs=wrow[b][:, c0:c0 + w], start=True, stop=True),
                         reads=['ones1', f'wrow{b}'], writes=['ps_w'])
                    P.op('act', lambda e, pb=pb, w=w: e.activation(out=sg[pb][:, :w], in_=ps_g[pb][:, :w], func=AF.Silu),
                         reads=[f'ps_g{pb}'], writes=[f'sg{pb}'])
                    P.op('dve', lambda e, pb=pb, w=w: e.tensor_tensor(out=sg[pb][:, :w], in0=sg[pb][:, :w], in1=ps_u[pb][:, :w], op=ALU.mult),
                         reads=[f'sg{pb}', f'ps_u{pb}'], writes=[f'sg{pb}'])
                    P.op('dve', lambda e, pb=pb, w=w, j=j, c0=c0: e.tensor_tensor(out=actb[:, j, c0:c0 + w], in0=sg[pb][:, :w], in1=ps_w[:, :w], op=ALU.mult),
                         reads=[f'sg{pb}', 'ps_w'], writes=['actb'])
            for ft in range(8):
                for (c0, w) in cols:
                    pb = q % 3
                    q += 1
                    for j in range(2):
                        P.op('pe', lambda e, pb=pb, j=j, ft=ft, c0=c0, w=w, b=b: e.matmul(
                            ps_o[pb][:, :w], lhsT=wdb[b][:, j, ft * 128:(ft + 1) * 128], rhs=actb[:, j, c0:c0 + w],
                            start=(j == 0), stop=(j == 1)), reads=[f'wdb{b}', 'actb'], writes=[f'ps_o{pb}'])
                    akey = ('acc', ft, c0)
                    if e_ == 0:
                        P.op('act', lambda e, pb=pb, ft=ft, c0=c0, w=w: e.activation(out=acc[:, ft, c0:c0 + w], in_=ps_o[pb][:, :w], func=AF.Copy),
                             reads=[f'ps_o{pb}'], writes=[akey])
                    elif q % 2 == 0:
                        P.op('dve', lambda e, pb=pb, ft=ft, c0=c0, w=w: e.tensor_tensor(out=acc[:, ft, c0:c0 + w], in0=acc[:, ft, c0:c0 + w],
                                                                                    in1=ps_o[pb][:, :w], op=ALU.add),
                             reads=[f'ps_o{pb}', akey], writes=[akey])
                    else:
                        tb = (q // 2) % 2
                        P.op('act', lambda e, pb=pb, tb=tb, w=w: e.activation(out=otmp[tb][:, :w], in_=ps_o[pb][:, :w], func=AF.Copy),
                             reads=[f'ps_o{pb}'], writes=[f'otmp{tb}'])
                        P.op('pool', lambda e, tb=tb, ft=ft, c0=c0, w=w: e.tensor_tensor(out=acc[:, ft, c0:c0 + w], in0=acc[:, ft, c0:c0 + w],
                                                                                     in1=otmp[tb][:, :w], op=ALU.add),
                             reads=[f'otmp{tb}', akey], writes=[akey])
        P.dma(lambda e, t0=t0: e.dma_start(out=part[:, :, t0:t0 + TC], in_=acc[:]), reads=[('acc', ft, c0) for ft in range(8) for (c0, w) in cols], is_out=True)
    return P.finish()


def build_post(T, segs, NP=8):
    P = Prog()
    h1T = P.inp('h1T', [128, 8, T]); parts = P.inp('parts', [NP, 128, 8, T]); vec_d = P.inp('vec', [128, 8, 4])
    h2T = P.outp('h2T', [128, 8, T])
    vec = P.sb('vec_sb', [128, 8, 4])
    consts = ln_consts(P)
    P.dma(lambda e: e.dma_start(out=vec[:], in_=vec_d), writes=['vec'])
    hh = [P.sb(f'hh{i}', [128, 8, CW]) for i in range(2)]
    pp = [P.sb(f'pp{i}', [128, 8, CW]) for i in range(3)]
    ff = P.sb('ff', [128, 8, CW])
    z = P.sb('z', [128, 8, CW]); zsq = P.sb('zsq', [128, 8, CW]); h2 = P.sb('h2', [128, 8, CW])
    k = 0
    for ci, (c0, w, m) in enumerate(chunks_of(segs)):
        b = ci % 2
        P.dma(lambda e, b=b, c0=c0, w=w: e.dma_start(out=hh[b][:, :, :w], in_=h1T[:, :, c0:c0 + w]), writes=[f'hh{b}'])
        for pi in range(NP):
            pb = k % 3
            k += 1
            P.dma(lambda e, pb=pb, pi=pi, c0=c0, w=w: e.dma_start(out=pp[pb][:, :, :w], in_=parts[pi, :, :, c0:c0 + w]), writes=[f'pp{pb}'])
            if pi == 0:
                P.op('pool', lambda e, pb=pb, w=w: e.tensor_copy(out=ff[:, :, :w], in_=pp[pb][:, :, :w]), reads=[f'pp{pb}'], writes=['ff'])
            else:
                P.op('pool', lambda e, pb=pb, w=w: e.tensor_tensor(out=ff[:, :, :w], in0=ff[:, :, :w], in1=pp[pb][:, :, :w], op=ALU.add),
                     reads=[f'pp{pb}', 'ff'], writes=['ff'])
        P.op('act', lambda e, b=b, w=w: e.activation(out=z[:, :, :w], in_=hh[b][:, :, :w], func=AF.Copy, scale=ALPHA), reads=[f'hh{b}'], writes=['z'])
        for ft in range(8):
            P.op('dve', lambda e, ft=ft, w=w, m=m: e.scalar_tensor_tensor(out=z[:, ft, :w], in0=ff[:, ft, :w], scalar=vec[:, ft, 2 + m:3 + m],
                                                                         in1=z[:, ft, :w], op0=ALU.mult, op1=ALU.add),
                 reads=['ff', 'vec', 'z'], writes=['z'])
        emit_ln(P, z, zsq, w, consts, '', h2, vec, 0, 1)
        P.dma(lambda e, c0=c0, w=w: e.dma_start(out=h2T[:, :, c0:c0 + w], in_=h2[:, :, :w]), reads=['h'], is_out=True)
    return P.finish()


ML_EPS = 1e-6
NCOL = 776


def build_ml(NCTX=2, NLAT=64):
    NCH = NCTX + NLAT
    T = NCH * 128
    P = Prog()
    hT = P.inp('hT', [128, 8, T]); vec_d = P.inp('vec', [128, 8, 4]); W_d = P.inp('W', [128, 8, NCOL])
    gb_d = P.inp('gb', [128, 8]); cos_d = P.inp('cosT', [128, NLAT, 32]); sin_d = P.inp('sinT', [128, NLAT, 32])
    ng_d = P.inp('ng', [128, 256]); tri_d = P.inp('tri', [2, 128, 128]); id_d = P.inp('ident', [128, 128])
    mix = P.outp('mix', [NLAT * 128, 256])
    H0 = P.scratch('H0', [NLAT, 128, 256])

    vec = P.sb('vec_sb', [128, 8, 4]); Wf = P.sb('Wf', [128, 8, NCOL]); Wb = P.sb('Wb', [128, 8, NCOL], BF16)
    gb = P.sb('gb_sb', [128, 8]); cosT = P.sb('cos_sb', [128, NLAT, 32]); sinT = P.sb('sin_sb', [128, NLAT, 32])
    ng = P.sb('ng_sb', [128, 256]); tri = P.sb('tri_sb', [128, 2, 128]); idf = P.sb('idf', [128, 128]); idb = P.sb('idb', [128, 128], BF16)
    onesm = P.sb('onesm', [128, 128]); c1 = P.sb('c1', [128, 1]); cln8 = P.sb('cln8', [128, 1]); ceps = P.sb('ceps', [128, 1])
    P.dma(lambda e: e.dma_start(out=vec[:], in_=vec_d), writes=['vec'])
    P.dma(lambda e: e.dma_start(out=Wf[:], in_=W_d), writes=['Wf'])
    P.dma(lambda e: e.dma_start(out=gb[:], in_=gb_d), writes=['gb'])
    P.dma(lambda e: e.dma_start(out=cosT[:], in_=cos_d), writes=['cos'])
    P.dma(lambda e: e.dma_start(out=sinT[:], in_=sin_d), writes=['sin'])
    P.dma(lambda e: e.dma_start(out=ng[:], in_=ng_d), writes=['ng'])
    for d in range(2):
        P.dma(lambda e, d=d: e.dma_start(out=tri[:, d, :], in_=tri_d[d]), writes=['tri'])
    P.dma(lambda e: e.dma_start(out=idf[:], in_=id_d), writes=['idf'])
    P.op('pool', lambda e: e.tensor_copy(out=Wb[:], in_=Wf[:]), reads=['Wf'], writes=['Wb'])
    P.op('pool', lambda e: e.tensor_copy(out=idb[:], in_=idf[:]), reads=['idf'], writes=['idb'])
    P.op('pool', lambda e: e.memset(onesm[:], 1.0), writes=['onesm'])
    P.op('pool', lambda e: e.memset(c1[:], 1.0), writes=['c1'])
    P.op('pool', lambda e: e.memset(cln8[:], -math.log(8.0)), writes=['cln8'])
    P.op('pool', lambda e: e.memset(ceps[:], ML_EPS), writes=['ceps'])
    for j in (0, 2):
        P.op('dve', lambda e, j=j: e.tensor_scalar(out=vec[:, :, j:j + 1], in0=vec[:, :, j:j + 1], scalar1=1.0, scalar2=None, op0=ALU.add),
             reads=['vec'], writes=['vec'])

    hin = [P.sb(f'hin{i}', [128, 8, 128]) for i in range(2)]
    af = P.sb('af', [128, 8, 128]); ab = P.sb('ab', [128, 8, 128], BF16)
    psA = P.ps('psA', [128, 512]); psB = P.ps('psB', [128, 264])
    psT = P.ps('psT', [64, 4, 128], BF16)
    psG = P.ps('psG', [128, 8])
    psS = P.ps('psS', [128, 128]); psO = P.ps('psO', [128, 129]); psU = P.ps('psU', [64, 129])
    qk = P.sb('qk', [128, 4, 64]); qkb = P.sb('qkb', [128, 4, 64], BF16)
    r1 = P.sb('r1', [128, 4, 2, 16]); r2 = P.sb('r2', [128, 4, 2, 16])
    qkT = P.sb('qkT', [64, 4, 128], BF16)
    gx = P.sb('gx', [128, 2]); gax = P.sb('gax', [128, 2]); ge = P.sb('ge', [128, 2]); gl = P.sb('gl', [128, 2]); lf = P.sb('lf', [128, 2])
    ig = P.sb('ig', [128, 2]); ebq = P.sb('ebq', [128, 2]); ek = P.sb('ek', [128, 2]); EB = P.sb('EB', [128, 2]); gt = P.sb('gt', [128, 2])
    Vt = P.sb('Vt', [128, 2, 129], BF16)
    osig = P.sb('osig', [128, 256])
    sT = P.sb('sT', [128, 128], BF16)
    Cf = P.sb('Cf', [64, 2, 129]); Cb = P.sb('Cb', [64, 2, 129], BF16); Ct = P.sb('Ct', [64, 129])
    dd = P.sb('dd', [128, 1]); rr = P.sb('rr', [128, 1])
    hout = P.sb('hout', [128, 256]); h0 = P.sb('h0', [128, 256]); hsq = P.sb('hsq', [128, 128]); ms = P.sb('ms', [128, 2])
    fin = P.sb('fin', [128, 256])

    nload = 0
    for d in range(2):
        P.op('pool', lambda e: e.memset(Cf[:], 0.0), writes=['Cf'])
        P.op('pool', lambda e: e.memset(Cb[:], 0.0), writes=['Cb'])
        order = list(range(NCTX)) + list(range(NCTX, NCH))
        if d == 1:
            order = list(range(NCTX - 1, -1, -1)) + list(range(NCH - 1, NCTX - 1, -1))
        for c in order:
            is_lat = c >= NCTX
            lc = c - NCTX
            hb = nload % 2
            nload += 1
            P.dma(lambda e, hb=hb, c=c: e.dma_start(out=hin[hb][:], in_=hT[:, :, c * 128:(c + 1) * 128]), writes=[f'hin{hb}'])
            js, jh = (0, 1) if is_lat else (2, 3)
            P.op('dve', lambda e, hb=hb, js=js: e.tensor_tensor(out=af[:], in0=hin[hb][:], in1=vec[:, :, js:js + 1].to_broadcast([128, 8, 128]), op=ALU.mult),
                 reads=[f'hin{hb}', 'vec'], writes=['af'])
            P.op('pool', lambda e, jh=jh: e.tensor_tensor(out=ab[:], in0=af[:], in1=vec[:, :, jh:jh + 1].to_broadcast([128, 8, 128]), op=ALU.add),
                 reads=['af', 'vec'], writes=['ab'])
            for kt in range(8):
                P.op('pe', lambda e, kt=kt: e.matmul(psA[:, :], lhsT=ab[:, kt, :], rhs=Wb[:, kt, 0:512], start=(kt == 0), stop=(kt == 7)),
                     reads=['ab', 'Wb'], writes=['psA'])
            for kt in range(8):
                P.op('pe', lambda e, kt=kt: e.matmul(psB[:, :], lhsT=ab[:, kt, :], rhs=Wb[:, kt, 512:776], start=(kt == 0), stop=(kt == 7)),
                     reads=['ab', 'Wb'], writes=['psB'])
            P.op('act', lambda e: e.activation(out=qk[:].rearrange('p a b -> p (a b)'), in_=psA[:, 0:256], func=AF.Copy), reads=['psA'], writes=['qk'])
            if is_lat:
                q5 = qk[:].rearrange('p a (h u i) -> p a h u i', h=2, u=2)
                o5 = qkb[:].rearrange('p a (h u i) -> p a h u i', h=2, u=2)
                u1 = q5[:, :, :, 0, :]; u2 = q5[:, :, :, 1, :]
                cosb = cosT[:, lc, :].rearrange('p (h i) -> p h i', h=2).unsqueeze(1).to_broadcast([128, 4, 2, 16])
                sinb = sinT[:, lc, :].rearrange('p (h i) -> p h i', h=2).unsqueeze(1).to_broadcast([128, 4, 2, 16])
                P.op('dve', lambda e, u1=u1, cosb=cosb: e.tensor_tensor(out=r1[:], in0=u1, in1=cosb, op=ALU.mult), reads=['qk', 'cos'], writes=['r1'])
                P.op('pool', lambda e, u2=u2, sinb=sinb: e.tensor_tensor(out=r2[:], in0=u2, in1=sinb, op=ALU.mult), reads=['qk', 'sin'], writes=['r2'])
                P.op('dve', lambda e, o5=o5: e.tensor_tensor(out=o5[:, :, :, 0, :], in0=r1[:], in1=r2[:], op=ALU.subtract), reads=['r1', 'r2'], writes=['qkb'])
                P.op('dve', lambda e, u1=u1, sinb=sinb: e.tensor_tensor(out=r1[:], in0=u1, in1=sinb, op=ALU.mult), reads=['qk', 'sin'], writes=['r1'])
                P.op('pool', lambda e, u2=u2, cosb=cosb: e.tensor_tensor(out=r2[:], in0=u2, in1=cosb, op=ALU.mult), reads=['qk', 'cos'], writes=['r2'])
                P.op('dve', lambda e, o5=o5: e.tensor_tensor(out=o5[:, :, :, 1, :], in0=r1[:], in1=r2[:], op=ALU.add), reads=['r1', 'r2'], writes=['qkb'])
            else:
                P.op('dve', lambda e: e.tensor_copy(out=qkb[:], in_=qk[:]), reads=['qk'], writes=['qkb'])
            jlist = (0, 1, 2, 3) if is_lat else ()
            for j in jlist:
                P.op('pe', lambda e, j=j: e.transpose(psT[:, j, :], qkb[:, j, :], idb[:]), reads=['qkb', 'idb'], writes=['psT'])
            if is_lat:
                P.op('act', lambda e: e.activation(out=qkT[:], in_=psT[:], func=AF.Copy), reads=['psT'], writes=['qkT'])
            gi0 = 256 + 2 * d
            gf0 = 256 + 4 + 2 * d
            P.op('dve', lambda e, gf0=gf0, d=d: e.tensor_tensor(out=gx[:], in0=psB[:, gf0:gf0 + 2], in1=gb[:, 4 + 2 * d:6 + 2 * d], op=ALU.add),
                 reads=['psB', 'gb'], writes=['gx'])
            P.op('dve', lambda e, gi0=gi0, d=d: e.tensor_tensor(out=ig[:], in0=psB[:, gi0:gi0 + 2], in1=gb[:, 2 * d:2 + 2 * d], op=ALU.add),
                 reads=['psB', 'gb'], writes=['ig'])
            P.op('act', lambda e: e.activation(out=gax[:], in_=gx[:], func=AF.Abs), reads=['gx'], writes=['gax'])
            P.op('act', lambda e: e.activation(out=ge[:], in_=gax[:], func=AF.Exp, scale=-1.0), reads=['gax'], writes=['ge'])
            P.op('act', lambda e: e.activation(out=gl[:], in_=ge[:], func=AF.Ln, bias=c1[:, 0:1], scale=1.0), reads=['ge', 'c1'], writes=['gl'])
            P.op('dve', lambda e: e.tensor_single_scalar(out=gax[:], in_=gx[:], scalar=0.0, op=ALU.min), reads=['gx', 'ge'], writes=['gax'])
            P.op('dve', lambda e: e.tensor_tensor(out=lf[:], in0=gax[:], in1=gl[:], op=ALU.subtract), reads=['gax', 'gl'], writes=['lf'])
            P.op('pe', lambda e, d=d: e.matmul(psG[:, 0:2], lhsT=tri[:, d, :], rhs=lf[:], start=True, stop=True), reads=['tri', 'lf'], writes=['psG'])
            P.op('pe', lambda e: e.matmul(psG[:, 2:4], lhsT=onesm[:], rhs=lf[:], start=True, stop=True), reads=['onesm', 'lf'], writes=['psG'])
            P.op('act', lambda e: e.activation(out=ebq[:], in_=psG[:, 0:2], func=AF.Exp, bias=cln8[:, 0:1], scale=1.0), reads=['psG', 'cln8'], writes=['ebq'])
            P.op('act', lambda e: e.activation(out=EB[:], in_=psG[:, 2:4], func=AF.Exp), reads=['psG'], writes=['EB'])
            P.op('dve', lambda e: e.tensor_tensor(out=gt[:], in0=ig[:], in1=psG[:, 0:2], op=ALU.subtract), reads=['ig', 'psG'], writes=['gt'])
            P.op('act', lambda e: e.activation(out=ek[:], in_=gt[:], func=AF.Exp), reads=['gt'], writes=['ek'])
            for h in range(2):
                P.op('dve', lambda e, h=h: e.tensor_scalar(out=Vt[:, h, 0:128], in0=psA[:, 256 + h * 128:256 + (h + 1) * 128], scalar1=ek[:, h:h + 1],
                                                         scalar2=None, op0=ALU.mult), reads=['psA', 'ek'], writes=['Vt'])
                P.op('dve', lambda e, h=h: e.tensor_copy(out=Vt[:, h, 128:129], in_=ek[:, h:h + 1]), reads=['ek'], writes=['Vt'])
            if is_lat and d == 1:
                P.op('act', lambda e: e.activation(out=osig[:], in_=psB[:, 0:256], func=AF.Sigmoid), reads=['psB'], writes=['osig'])
                P.dma(lambda e, lc=lc: e.dma_start(out=h0[:], in_=H0[lc]), reads=[('H0', lc)], writes=['h0'])
            for h in range(2):
                if is_lat:
                    P.op('pe', lambda e, h=h: e.matmul(psS[:, :], lhsT=qkT[:, 2 + h, :], rhs=qkT[:, h, :], start=True, stop=True), reads=['qkT'], writes=['psS'])
                    P.op('dve', lambda e, d=d: e.tensor_tensor(out=sT[:], in0=psS[:], in1=tri[:, d, :], op=ALU.mult), reads=['psS', 'tri'], writes=['sT'])
                    P.op('pe', lambda e, h=h: e.matmul(psO[:, :], lhsT=sT[:], rhs=Vt[:, h, :], start=True, stop=False), reads=['sT', 'Vt'], writes=['psO'])
                    P.op('pe', lambda e, h=h: e.matmul(psO[:, :], lhsT=qkT[:, h, :], rhs=Cb[:, h, :], start=False, stop=True), reads=['qkT', 'Cb'], writes=['psO'])
                    P.op('act', lambda e, h=h: e.activation(out=dd[:], in_=psO[:, 128:129], func=AF.Abs, scale=ebq[:, h:h + 1]),
                         reads=['psO', 'ebq'], writes=['dd'])
                    P.op('dve', lambda e: e.tensor_single_scalar(out=dd[:], in_=dd[:], scalar=1.0, op=ALU.max), reads=['dd'], writes=['dd'])
                    P.op('dve', lambda e: e.reciprocal(out=rr[:], in_=dd[:]), reads=['dd'], writes=['rr'])
                    P.op('dve', lambda e, h=h: e.tensor_tensor(out=rr[:], in0=rr[:], in1=ebq[:, h:h + 1], op=ALU.mult), reads=['rr', 'ebq'], writes=['rr'])
                    P.op('dve', lambda e, h=h: e.tensor_scalar(out=hout[:, h * 128:(h + 1) * 128], in0=psO[:, 0:128], scalar1=rr[:, 0:1], scalar2=None, op0=ALU.mult),
                         reads=['psO', 'rr'], writes=['hout'])
                P.op('pe', lambda e, h=h: e.matmul(psU[:, :], lhsT=qkb[:, 2 + h, :], rhs=Vt[:, h, :], start=True, stop=True), reads=['qkb', 'Vt'], writes=['psU'])
                P.op('dve', lambda e, h=h: e.tensor_tensor(out=Ct[:], in0=Cf[:, h, :], in1=psU[:], op=ALU.add), reads=['Cf', 'psU'], writes=['Ct'])
                P.op('dve', lambda e, h=h: e.tensor_scalar(out=Cf[:, h, :], in0=Ct[:], scalar1=EB[0:64, h:h + 1], scalar2=None, op0=ALU.mult),
                     reads=['Ct', 'EB'], writes=['Cf'])
                P.op('act', lambda e, h=h: e.activation(out=Cb[:, h, :], in_=Cf[:, h, :], func=AF.Copy), reads=['Cf'], writes=['Cb'])
            if is_lat and d == 0:
                P.dma(lambda e, lc=lc: e.dma_start(out=H0[lc], in_=hout[:]), reads=['hout'], writes=[('H0', lc)])
            if is_lat and d == 1:
                P.op('dve', lambda e: e.tensor_tensor(out=hout[:], in0=hout[:], in1=h0[:], op=ALU.add), reads=['hout', 'h0'], writes=['hout'])
                for h in range(2):
                    P.op('act', lambda e, h=h: e.activation(out=hsq[:], in_=hout[:, h * 128:(h + 1) * 128], func=AF.Square), reads=['hout'], writes=['hsq'])
                    P.op('dve', lambda e, h=h: e.reduce_sum(out=ms[:, h:h + 1], in_=hsq[:], axis=AX.X), reads=['hsq'], writes=['ms'])
                P.op('act', lambda e: e.activation(out=ms[:], in_=ms[:], func=AF.Sqrt, bias=ceps[:, 0:1], scale=1.0 / 128.0), reads=['ms', 'ceps'], writes=['ms'])
                P.op('dve', lambda e: e.reciprocal(out=ms[:], in_=ms[:]), reads=['ms'], writes=['ms'])
                for h in range(2):
                    P.op('dve', lambda e, h=h: e.scalar_tensor_tensor(out=fin[:, h * 128:(h + 1) * 128], in0=hout[:, h * 128:(h + 1) * 128], scalar=ms[:, h:h + 1],
                                                                    in1=ng[:, h * 128:(h + 1) * 128], op0=ALU.mult, op1=ALU.mult),
                         reads=['hout', 'ms', 'ng'], writes=['fin'])
                P.op('dve', lambda e: e.tensor_tensor(out=fin[:], in0=fin[:], in1=osig[:], op=ALU.mult), reads=['fin', 'osig'], writes=['fin'])
                P.dma(lambda e, lc=lc: e.dma_start(out=mix[lc * 128:(lc + 1) * 128, :], in_=fin[:]), reads=['fin'], is_out=True)
    return P.finish()


RW_GN_EPS = 64e-5
NCTX = 2
NLAT = 64
NCH = NCTX + NLAT
NEGM = -30000.0


def na_key_tiles(i):
    rs0 = min(max(2 * i - 4, 0), 120)
    rs1 = min(max(2 * i + 1 - 4, 0), 120)
    return list(range(rs0 // 2, (rs1 + 7) // 2 + 1))


def na_bias_index(h, i, j):
    if 2 <= i <= 61:
        return h * 5 + (j - i + 2)
    e = {0: 0, 1: 1, 62: 2, 63: 3}[i]
    jb = j if i < 2 else j - 60
    return 10 + (e * 4 + jb) * 2 + h


import os


def build_ev():
    SKIP_NA = os.environ.get('SKIP_NA') == '1'
    SKIP_RW = os.environ.get('SKIP_RW') == '1'
    STAGE = int(os.environ.get('EV_STAGE', '9'))
    SUB = int(os.environ.get('EV_SUB', '9'))
    T = NCH * 128
    P = Prog()
    hT = P.inp('hT', [128, 8, T + 2])
    vec_d = P.inp('vec', [128, 8, 4]); WA_d = P.inp('WA', [128, 8, 992]); mu_d = P.inp('mu', [128, 608])
    rep_d = P.inp('rep', [128, 1152]); up_d = P.inp('UP', [128, 512]); gup_d = P.inp('GUP', [128, 128])
    tri_d = P.inp('tri', [2, 2, 128, 128]); id_d = P.inp('ident', [128, 128]); nab_d = P.inp('nab', [42, 128, 128])
    mix = P.outp('mix', [T, 256])
    Y0 = P.scratch('Y0', [NCH, 128, 128])

    def dve(fn, r, w): return P.op('dve', fn, reads=r, writes=w)
    def act(fn, r, w): return P.op('act', fn, reads=r, writes=w)
    def pool(fn, r, w): return P.op('pool', fn, reads=r, writes=w)
    def pe(fn, r, w): return P.op('pe', fn, reads=r, writes=w)

    vec = P.sb('vec_sb', [128, 8, 4]); WAf = P.sb('WAf', [128, 8, 992]); mu = P.sb('mu_sb', [128, 608])
    W1 = P.sb('W1', [128, 8, 992], BF16); W2 = P.sb('W2', [128, 8, 608], BF16); Wt_ = P.sb('Wtmp', [128, 8, 608])
    rep = P.sb('rep_sb', [128, 1152]); UPf = P.sb('UPf', [128, 512]); UPb = P.sb('UPb', [128, 512], BF16)
    GUf = P.sb('GUf', [128, 128]); GUb = P.sb('GUb', [128, 128], BF16)
    tri = P.sb('tri_sb', [128, 4, 128]); idf = P.sb('idf', [128, 128]); idb = P.sb('idb', [128, 128], BF16)
    nab = P.sb('nab_sb', [128, 42, 128])
    sg = P.sb('sg', [128, 128], BF16); onesm = P.sb('onesm', [128, 128]); ones1 = P.sb('ones1', [128, 1]); c1 = P.sb('c1', [128, 1]); cm05 = P.sb('cm05', [128, 1]); ceps = P.sb('ceps', [128, 1])
    P.dma(lambda e: e.dma_start(out=vec[:], in_=vec_d), writes=['vec'])
    P.dma(lambda e: e.dma_start(out=WAf[:], in_=WA_d), writes=['WAf'])
    P.dma(lambda e: e.dma_start(out=mu[:], in_=mu_d), writes=['mu'])
    P.dma(lambda e: e.dma_start(out=rep[:], in_=rep_d), writes=['rep'])
    P.dma(lambda e: e.dma_start(out=UPf[:], in_=up_d), writes=['UPf'])
    P.dma(lambda e: e.dma_start(out=GUf[:], in_=gup_d), writes=['GUf'])
    for d in range(2):
        for m in range(2):
            P.dma(lambda e, d=d, m=m: e.dma_start(out=tri[:, d * 2 + m, :], in_=tri_d[d, m]), writes=['tri'])
    P.dma(lambda e: e.dma_start(out=idf[:], in_=id_d), writes=['idf'])
    nabb = P.sb('nabb', [128, 42, 128], BF16)
    for k0 in range(42):
        P.dma(lambda e, k0=k0: e.dma_start(out=nab[:, k0, :], in_=nab_d[k0]), writes=['nab'])
    pool(lambda e: e.tensor_copy(out=nabb[:], in_=nab[:]), ['nab'], ['nabb'])
    pool(lambda e: e.tensor_copy(out=idb[:], in_=idf[:]), ['idf'], ['idb'])
    pool(lambda e: e.tensor_copy(out=UPb[:], in_=UPf[:]), ['UPf'], ['UPb'])
    pool(lambda e: e.tensor_copy(out=GUb[:], in_=GUf[:]), ['GUf'], ['GUb'])
    pool(lambda e: e.memset(ones1[:], 1.0), [], ['ones1'])
    pool(lambda e: e.memset(sg[:], 0.0), [], ['sg'])
    pool(lambda e: e.memset(onesm[:], 1.0), [], ['onesm'])
    pool(lambda e: e.memset(c1[:], 1.0), [], ['c1'])
    pool(lambda e: e.memset(cm05[:], -0.5), [], ['cm05'])
    pool(lambda e: e.memset(ceps[:], RW_GN_EPS), [], ['ceps'])
    for j in (0, 2):
        dve(lambda e, j=j: e.tensor_scalar(out=vec[:, :, j:j + 1], in0=vec[:, :, j:j + 1], scalar1=1.0, scalar2=None, op0=ALU.add), ['vec'], ['vec'])
    dve(lambda e: e.tensor_tensor(out=Wt_[:], in0=WAf[:, :, 0:608], in1=mu[:].unsqueeze(1).to_broadcast([128, 8, 608]), op=ALU.mult), ['WAf', 'mu'], ['Wt'])
    act(lambda e: e.activation(out=W2[:], in_=Wt_[:], func=AF.Copy, scale=0.5), ['Wt'], ['W2'])
    dve(lambda e: e.tensor_tensor(out=W1[:, :, 0:608], in0=WAf[:, :, 0:608], in1=Wt_[:], op=ALU.subtract), ['WAf', 'Wt'], ['W1'])
    pool(lambda e: e.tensor_copy(out=W1[:, :, 608:992], in_=WAf[:, :, 608:992]), ['WAf'], ['W1'])

    psA = P.ps('psA', [128, 512]); psUP = P.ps('psUP', [128, 512])
    psCB = P.ps('psCB', [128, 512]); psC = psCB[:, 0:384]; psB = psCB[:, 384:512]
    psT = P.ps('psT', [128, 1024], BF16)
    psGG = P.ps('psGG', [128, 4, 128]); psG = psGG[:, 0:2, :]; psG2 = psGG[:, 2:4, :]
    psX = P.ps('psX', [128, 512])
    psM = psX[:, 0:128].rearrange('p (a b) -> p a b', a=2); psS = psX[0:64, 128:256].rearrange('p (a b) -> p a b', a=2)
    psCum = psX[:, 256:384]; psTot = psX[:, 384:512]
    psN = P.ps('psN', [128, 4, 128])
    psVG = P.ps('psVG', [128, 512]); psV = psVG[:, 0:130].rearrange('p (a b) -> p a b', a=2); psGate = psVG[:, 256:384]

    hin = [P.sb(f'hin{i}', [128, 8, 130]) for i in range(2)]
    af = P.sb('af', [128, 8, 130]); ab = P.sb('ab', [128, 8, 128], BF16); sbb = P.sb('sbb', [128, 8, 128], BF16)
    rkv = P.sb('rkv', [128, 384])
    L1 = P.sb('L1', [128, 128], BF16); L1T = P.sb('L1T', [128, 128], BF16)
    sgT = P.sb('sgT', [128, 128], BF16)
    gate = P.sb('gate', [128, 128])
    x_ = P.sb('x_', [128, 128]); ax = P.sb('ax', [128, 128]); ex = P.sb('ex', [128, 128]); lx = P.sb('lx', [128, 128]); mn = P.sb('mn', [128, 128])
    ew = P.sb('ew', [128, 128]); a_d = [P.sb(f'a_d{i}', [128, 128]) for i in range(2)]
    kk = P.sb('kk', [128, 128]); sq = P.sb('sq', [128, 128]); ss = P.sb('ss', [128, 2]); rn = P.sb('rn', [128, 2])
    kd = [P.sb(f'kd{i}', [128, 128]) for i in range(2)]; tmp = P.sb('tmp', [128, 128])
    bt = P.sb('bt', [128, 128])
    eP = P.sb('eP', [128, 128]); eM = P.sb('eM', [128, 128]); eQ = P.sb('eQ', [128, 128]); cme = P.sb('cme', [128, 128])
    ops4 = P.sb('ops4', [128, 4, 128], BF16)
    opT = P.sb('opT', [64, 8, 128], BF16)
    vb = P.sb('vb', [128, 128], BF16)
    eTot = P.sb('eTot', [128, 128]); Dg = P.sb('Dg', [64, 2, 64])
    Mf = [P.sb(f'Mf{i}', [128, 2, 128]) for i in range(2)]; Nf = [P.sb(f'Nf{i}', [128, 2, 128]) for i in range(2)]
    Pm = P.sb('Pm', [128, 2, 128]); Pt = P.sb('Pt', [128, 2, 128])
    AkT = P.sb('AkT', [128, 2, 128], BF16); RbT = P.sb('RbT', [128, 2, 128], BF16); RkT = P.sb('RkT', [128, 2, 128], BF16)
    rhs_f = P.sb('rhs_f', [128, 2, 64]); Ub = P.sb('Ub', [128, 2, 64], BF16)
    Sf = P.sb('Sf', [64, 2, 64]); Sb = P.sb('Sb', [64, 2, 64], BF16); St = P.sb('St', [64, 2, 64])
    yv = P.sb('yv', [128, 128]); y0 = P.sb('y0', [128, 128])
    s1 = P.sb('s1', [128, 2]); yc = P.sb('yc', [128, 128]); s2 = P.sb('s2', [128, 2]); bsum = P.sb('bsum', [128, 2])
    rwout = P.sb('rwout', [128, 128])
    qT = [P.sb(f'qT{i}', [64, 2, 128], BF16) for i in range(8)]
    kT = [P.sb(f'kT{i}', [64, 2, 128], BF16) for i in range(8)]
    vA = [P.sb(f'vA{i}', [128, 2, 66], BF16) for i in range(8)]
    qTc = [P.sb(f'qTc{i}', [64, 2, 128], BF16) for i in range(2)]
    kTc = [P.sb(f'kTc{i}', [64, 2, 128], BF16) for i in range(2)]
    vAc = [P.sb(f'vAc{i}', [128, 2, 66], BF16) for i in range(2)]
    qkb = P.sb('qkb', [128, 256], BF16)
    for i_ in range(8):
        pool(lambda e, i_=i_: e.memset(vA[i_][:], 1.0), [], [f'vA{i_}'])
    for i_ in range(2):
        pool(lambda e, i_=i_: e.memset(vAc[i_][:], 1.0), [], [f'vAc{i_}'])
    pT = P.sb('pT', [128, 4, 128], BF16)
    rden = P.sb('rden', [128, 2]); naout = P.sb('naout', [128, 128])

    def na_block(qt, qkey, ktiles, out_row0, bias_fn):
        groups = [ktiles[i:i + 4] for i in range(0, len(ktiles), 4)]
        for h in range(2):
            first = True
            for gi_, grp in enumerate(groups):
                for jj, (kt_, kkey, va_, vkey, jlat) in enumerate(grp):
                    has_b = jlat is not None
                    pe(lambda e, jj=jj, kt_=kt_, h=h, has_b=has_b: e.matmul(psN[:, jj, :], lhsT=kt_[:, h, :], rhs=qt[:, h, :], start=True, stop=(not has_b)),
                       [kkey, qkey], ['psN'])
                    if has_b:
                        bi = bias_fn(h, jlat)
                        pe(lambda e, jj=jj, bi=bi: e.matmul(psN[:, jj, :], lhsT=idb[:], rhs=nabb[:, bi, :], start=False, stop=True), ['idb', 'nabb'], ['psN'])
                n = len(grp)
                act(lambda e, n=n: e.activation(out=pT[:, 0:n, :], in_=psN[:, 0:n, :], func=AF.Exp), ['psN'], ['pT'])
                for jj, (kt_, kkey, va_, vkey, jlat) in enumerate(grp):
                    last = (gi_ == len(groups) - 1) and (jj == n - 1)
                    pe(lambda e, jj=jj, va_=va_, h=h, first=first, last=last: e.matmul(psV[:, h, :], lhsT=pT[:, jj, :], rhs=va_[:, h, 0:65], start=first, stop=last),
                       ['pT', vkey], ['psVG'])
                    first = False
        dve(lambda e: e.reciprocal(out=rden[:], in_=psV[:, :, 64]), ['psVG'], ['rden'])
        for h in range(2):
            dve(lambda e, h=h: e.tensor_scalar(out=naout[:, h * 64:(h + 1) * 64], in0=psV[:, h, 0:64], scalar1=rden[:, h:h + 1], scalar2=None, op0=ALU.mult),
                ['psVG', 'rden'], ['naout'])
        P.dma(lambda e: e.dma_start(out=mix[out_row0:out_row0 + 128, 0:128], in_=naout[:]), reads=['naout'], is_out=True)

    nload = 0
    for d in range(2):
        TI = 2 * d
        TS = TI + 1
        TSo = 2 * (1 - d) + 1
        pool(lambda e: e.memset(Sf[:], 0.0), [], ['Sf'])
        pool(lambda e: e.memset(Sb[:], 0.0), [], ['Sb'])
        order = list(range(NCH)) if d == 0 else (list(range(NCTX - 1, -1, -1)) + list(range(NCH - 1, NCTX - 1, -1)))
        na_done = set()
        for c in order:
            if STAGE <= 1:
                continue
            is_lat = c >= NCTX
            lc = c - NCTX
            hb = nload % 2
            nload += 1
            col0 = c * 128
            P.dma(lambda e, hb=hb, col0=col0: e.dma_start(out=hin[hb][:], in_=hT[:, :, col0:col0 + 130]), writes=[f'hin{hb}'])
            js, jh = (0, 1) if is_lat else (2, 3)
            dve(lambda e, hb=hb, js=js: e.tensor_tensor(out=af[:], in0=hin[hb][:], in1=vec[:, :, js:js + 1].to_broadcast([128, 8, 130]), op=ALU.mult),
                [f'hin{hb}', 'vec'], ['af'])
            pool(lambda e, jh=jh: e.tensor_tensor(out=af[:], in0=af[:], in1=vec[:, :, jh:jh + 1].to_broadcast([128, 8, 130]), op=ALU.add), ['af', 'vec'], ['af'])
            if c == 0 or c == NCTX:
                pool(lambda e: e.memset(af[:, :, 0:1], 0.0), ['af'], ['af'])
            if c == NCTX - 1 or c == NCH - 1:
                pool(lambda e: e.memset(af[:, :, 129:130], 0.0), ['af'], ['af'])
            act(lambda e: e.activation(out=ab[:], in_=af[:, :, 1:129], func=AF.Copy), ['af'], ['ab'])
            dve(lambda e: e.tensor_tensor(out=sbb[:], in0=af[:, :, 0:128], in1=af[:, :, 2:130], op=ALU.add), ['af'], ['sbb'])
            for kt in range(8):
                pe(lambda e, kt=kt: e.matmul(psA[:, :], lhsT=ab[:, kt, :], rhs=W1[:, kt, 0:512], start=(kt == 0), stop=False), ['ab', 'W1'], ['psA'])
            for kt in range(8):
                pe(lambda e, kt=kt: e.matmul(psA[:, :], lhsT=sbb[:, kt, :], rhs=W2[:, kt, 0:512], start=False, stop=(kt == 7)), ['sbb', 'W2'], ['psA'])
            if d == 1:
                for kt in range(8):
                    pe(lambda e, kt=kt: e.matmul(psB[:, 0:96], lhsT=ab[:, kt, :], rhs=W1[:, kt, 512:608], start=(kt == 0), stop=False), ['ab', 'W1'], ['psCB'])
                for kt in range(8):
                    pe(lambda e, kt=kt: e.matmul(psB[:, 0:96], lhsT=sbb[:, kt, :], rhs=W2[:, kt, 512:608], start=False, stop=(kt == 7)), ['sbb', 'W2'], ['psCB'])
            if d == 0:
                for kt in range(8):
                    pe(lambda e, kt=kt: e.matmul(psC[:, :], lhsT=ab[:, kt, :], rhs=W1[:, kt, 608:992], start=(kt == 0), stop=(kt == 7)), ['ab', 'W1'], ['psCB'])
            if STAGE <= 2:
                continue
            if d == 0:
                if is_lat:
                    slot = lc % 8
                    qt_, kt_t, va_ = qT[slot], kT[slot], vA[slot]
                    qk_, kk_, vk_ = f'qT{slot}', f'kT{slot}', f'vA{slot}'
                else:
                    qt_, kt_t, va_ = qTc[c], kTc[c], vAc[c]
                    qk_, kk_, vk_ = f'qTc{c}', f'kTc{c}', f'vAc{c}'
                act(lambda e: e.activation(out=qkb[:, 0:128], in_=psC[:, 0:128], func=AF.Copy, scale=0.125), ['psCB'], ['qkb'])
                act(lambda e: e.activation(out=qkb[:, 128:256], in_=psC[:, 128:256], func=AF.Copy), ['psCB'], ['qkb'])
                for j in range(4):
                    pe(lambda e, j=j: e.transpose(psT[0:64, j * 128:(j + 1) * 128], qkb[:, j * 64:(j + 1) * 64], idb[:]), ['qkb', 'idb'], ['psT'])
                if SUB >= 2:
                    act(lambda e, qt_=qt_: e.activation(out=qt_[:].rearrange('p a b -> p (a b)'), in_=psT[0:64, 0:256], func=AF.Copy), ['psT'], [qk_])
                    act(lambda e, kt_t=kt_t: e.activation(out=kt_t[:].rearrange('p a b -> p (a b)'), in_=psT[0:64, 256:512], func=AF.Copy), ['psT'], [kk_])
                if SUB >= 3:
                    for h in range(2):
                        act(lambda e, va_=va_, h=h: e.activation(out=va_[:, h, 0:64], in_=psC[:, 256 + h * 64:256 + (h + 1) * 64], func=AF.Copy), ['psCB'], [vk_])
            if d == 0 and not SKIP_NA and STAGE >= 4:
                if c == NCTX - 1:
                    for cq in range(NCTX):
                        kts = [(kTc[j], f'kTc{j}', vAc[j], f'vAc{j}', None) for j in range(NCTX)]
                        na_block(qTc[cq], f'qTc{cq}', kts, cq * 128, None)
                if is_lat:
                    for i in range(NLAT):
                        if i in na_done:
                            continue
                        kl = na_key_tiles(i)
                        if max(kl) > lc:
                            continue
                        na_done.add(i)
                        kts = [(kT[j % 8], f'kT{j % 8}', vA[j % 8], f'vA{j % 8}', j) for j in kl]
                        kts += [(kTc[j], f'kTc{j}', vAc[j], f'vAc{j}', None) for j in range(NCTX)]
                        na_block(qT[i % 8], f'qT{i % 8}', kts, (NCTX + i) * 128, lambda h, j, i=i: na_bias_index(h, i, j))
            if SKIP_RW:
                continue
            act(lambda e: e.activation(out=rkv[:], in_=psA[:, 0:384], func=AF.Copy), ['psA'], ['rkv'])
            act(lambda e: e.activation(out=L1[:, 0:64], in_=psA[:, 384:448], func=AF.Tanh), ['psA'], ['L1'])
            act(lambda e: e.activation(out=L1[:, 64:128], in_=psA[:, 448:512], func=AF.Copy), ['psA'], ['L1'])
            pe(lambda e: e.transpose(psT[:, 512:640], L1[:], idb[:]), ['L1', 'idb'], ['psT'])
            act(lambda e: e.activation(out=L1T[:], in_=psT[:, 512:640], func=AF.Copy), ['psT'], ['L1T'])
            pe(lambda e: e.matmul(psUP[:, :], lhsT=L1T[:], rhs=UPb[:], start=True, stop=True), ['L1T', 'UPb'], ['psUP'])
            if d == 1:
                act(lambda e: e.activation(out=sg[:, 0:96], in_=psB[:, 0:96], func=AF.Sigmoid), ['psCB'], ['sg'])
                pe(lambda e: e.transpose(psT[:, 640:768], sg[:], idb[:]), ['sg', 'idb'], ['psT'])
                act(lambda e: e.activation(out=sgT[:], in_=psT[:, 640:768], func=AF.Copy), ['psT'], ['sgT'])
                pe(lambda e: e.matmul(psGate[:, :], lhsT=sgT[:], rhs=GUb[:], start=True, stop=True), ['sgT', 'GUb'], ['psVG'])
                act(lambda e: e.activation(out=gate[:], in_=psGate[:], func=AF.Copy), ['psVG'], ['gate'])
            dve(lambda e, d=d: e.tensor_tensor(out=x_[:], in0=psUP[:, d * 128:(d + 1) * 128], in1=rep[:, d * 128:(d + 1) * 128], op=ALU.add), ['psUP', 'rep'], ['x_'])
            act(lambda e: e.activation(out=ax[:], in_=x_[:], func=AF.Abs), ['x_'], ['ax'])
            act(lambda e: e.activation(out=ex[:], in_=ax[:], func=AF.Exp, scale=-1.0), ['ax'], ['ex'])
            act(lambda e: e.activation(out=lx[:], in_=ex[:], func=AF.Ln, bias=c1[:, 0:1], scale=1.0), ['ex', 'c1'], ['lx'])
            dve(lambda e: e.tensor_single_scalar(out=mn[:], in_=x_[:], scalar=0.0, op=ALU.min), ['x_'], ['mn'])
            dve(lambda e: e.tensor_tensor(out=mn[:], in0=mn[:], in1=lx[:], op=ALU.subtract), ['mn', 'lx'], ['mn'])
            act(lambda e: e.activation(out=ew[:], in_=mn[:], func=AF.Exp, bias=cm05[:, 0:1], scale=1.0), ['mn', 'cm05'], ['ew'])
            for dd_ in ((d,) if d == 0 else (0, 1)):
                dve(lambda e, dd_=dd_: e.tensor_tensor(out=tmp[:], in0=psUP[:, 256 + dd_ * 128:256 + (dd_ + 1) * 128], in1=rep[:, 256 + dd_ * 128:256 + (dd_ + 1) * 128], op=ALU.add),
                    ['psUP', 'rep'], ['tmp'])
                act(lambda e, dd_=dd_: e.activation(out=a_d[dd_][:], in_=tmp[:], func=AF.Sigmoid), ['tmp'], [f'a_d{dd_}'])
                dve(lambda e, dd_=dd_: e.scalar_tensor_tensor(out=tmp[:], in0=a_d[dd_][:], scalar=-1.0, in1=rep[:, 640:768], op0=ALU.add, op1=ALU.mult),
                    [f'a_d{dd_}', 'rep'], ['tmp'])
                dve(lambda e, dd_=dd_: e.scalar_tensor_tensor(out=kd[dd_][:], in0=tmp[:], scalar=1.0, in1=rkv[:, 128:256], op0=ALU.add, op1=ALU.mult),
                    ['tmp', 'rkv'], [f'kd{dd_}'])
            dve(lambda e: e.tensor_tensor(out=kk[:], in0=rkv[:, 128:256], in1=rep[:, 512:640], op=ALU.mult), ['rkv', 'rep'], ['kk'])
            act(lambda e: e.activation(out=sq[:], in_=kk[:], func=AF.Square), ['kk'], ['sq'])
            dve(lambda e: e.reduce_sum(out=ss[:], in_=sq[:].rearrange('p (a b) -> p a b', a=2), axis=AX.X), ['sq'], ['ss'])
            act(lambda e: e.activation(out=ss[:], in_=ss[:], func=AF.Sqrt), ['ss'], ['ss'])
            dve(lambda e: e.tensor_single_scalar(out=ss[:], in_=ss[:], scalar=1e-12, op=ALU.max), ['ss'], ['ss'])
            dve(lambda e: e.reciprocal(out=rn[:], in_=ss[:]), ['ss'], ['rn'])
            dve(lambda e: e.tensor_tensor(out=kk[:].rearrange('p (a b) -> p a b', a=2), in0=kk[:].rearrange('p (a b) -> p a b', a=2),
                                          in1=rn[:].unsqueeze(2).to_broadcast([128, 2, 64]), op=ALU.mult), ['kk', 'rn'], ['kk'])
            dve(lambda e, d=d: e.tensor_tensor(out=bt[:], in0=kk[:], in1=a_d[d][:], op=ALU.mult), ['kk', f'a_d{d}'], ['bt'])
            pe(lambda e, TI=TI: e.matmul(psCum[:, :], lhsT=tri[:, TI, :], rhs=ew[:], start=True, stop=True), ['tri', 'ew'], ['psX'])
            pe(lambda e: e.matmul(psTot[:, :], lhsT=onesm[:], rhs=ew[:], start=True, stop=True), ['onesm', 'ew'], ['psX'])
            act(lambda e: e.activation(out=eTot[:], in_=psTot[:], func=AF.Exp, scale=-1.0), ['psX'], ['eTot'])
            dve(lambda e: e.tensor_tensor(out=Dg[:], in0=eTot[0:64, :].rearrange('p (a b) -> p a b', a=2), in1=idf[0:64, 0:64].unsqueeze(1).to_broadcast([64, 2, 64]), op=ALU.mult),
                ['eTot', 'idf'], ['Dg'])
            act(lambda e: e.activation(out=eP[:], in_=psCum[:], func=AF.Exp, scale=-1.0), ['psX'], ['eP'])
            act(lambda e: e.activation(out=eM[:], in_=psCum[:], func=AF.Exp), ['psX'], ['eM'])
            dve(lambda e: e.tensor_tensor(out=cme[:], in0=ew[:], in1=psCum[:], op=ALU.subtract), ['ew', 'psX'], ['cme'])
            act(lambda e: e.activation(out=eQ[:], in_=cme[:], func=AF.Exp), ['cme'], ['eQ'])
            dve(lambda e: e.scalar_tensor_tensor(out=ops4[:, 0, :], in0=kk[:], scalar=-1.0, in1=eQ[:], op0=ALU.mult, op1=ALU.mult), ['kk', 'eQ'], ['ops4'])
            dve(lambda e: e.tensor_tensor(out=ops4[:, 1, :], in0=bt[:], in1=eM[:], op=ALU.mult), ['bt', 'eM'], ['ops4'])
            pool(lambda e, d=d: e.tensor_tensor(out=ops4[:, 2, :], in0=kd[d][:], in1=eM[:], op=ALU.mult), [f'kd{d}', 'eM'], ['ops4'])
            pool(lambda e: e.tensor_tensor(out=ops4[:, 3, :], in0=rkv[:, 0:128], in1=eP[:], op=ALU.mult), ['rkv', 'eP'], ['ops4'])
            pool(lambda e: e.tensor_copy(out=vb[:], in_=rkv[:, 256:384]), ['rkv'], ['vb'])
            for o in range(4):
                for h in range(2):
                    pe(lambda e, o=o, h=h: e.transpose(psT[0:64, (o * 2 + h) * 128:(o * 2 + h + 1) * 128], ops4[:, o, h * 64:(h + 1) * 64], idb[:]),
                       ['ops4', 'idb'], ['psT'])
            act(lambda e: e.activation(out=opT[:].rearrange('p a b -> p (a b)'), in_=psT[0:64, :], func=AF.Copy), ['psT'], ['opT'])
            AT = lambda h: opT[:, 0 + h, :]
            BT = lambda h: opT[:, 2 + h, :]
            KT = lambda h: opT[:, 4 + h, :]
            RT = lambda h: opT[:, 6 + h, :]
            for h in range(2):
                pe(lambda e, h=h: e.matmul(psG[:, h, :], lhsT=BT(h), rhs=AT(h), start=True, stop=True), ['opT'], ['psGG'])
                pe(lambda e, h=h: e.matmul(psG2[:, h, :], lhsT=AT(h), rhs=BT(h), start=True, stop=True), ['opT'], ['psGG'])
            dve(lambda e, TS=TS: e.tensor_tensor(out=Mf[0][:], in0=psG[:], in1=tri[:, TS, :].unsqueeze(1).to_broadcast([128, 2, 128]), op=ALU.mult), ['psGG', 'tri'], ['Mf0'])
            dve(lambda e, TSo=TSo: e.tensor_tensor(out=Nf[0][:], in0=psG2[:], in1=tri[:, TSo, :].unsqueeze(1).to_broadcast([128, 2, 128]), op=ALU.mult), ['psGG', 'tri'], ['Nf0'])
            for h in range(2):
                pe(lambda e, h=h: e.matmul(psG[:, h, :], lhsT=KT(h), rhs=AT(h), start=True, stop=True), ['opT'], ['psGG'])
                pe(lambda e, h=h: e.matmul(psG2[:, h, :], lhsT=BT(h), rhs=RT(h), start=True, stop=True), ['opT'], ['psGG'])
            dve(lambda e, TS=TS: e.tensor_tensor(out=AkT[:], in0=psG[:], in1=tri[:, TS, :].unsqueeze(1).to_broadcast([128, 2, 128]), op=ALU.mult), ['psGG', 'tri'], ['AkT'])
            dve(lambda e, TI=TI: e.tensor_tensor(out=RbT[:], in0=psG2[:], in1=tri[:, TI, :].unsqueeze(1).to_broadcast([128, 2, 128]), op=ALU.mult), ['psGG', 'tri'], ['RbT'])
            for h in range(2):
                pe(lambda e, h=h: e.matmul(psG[:, h, :], lhsT=KT(h), rhs=RT(h), start=True, stop=True), ['opT'], ['psGG'])
            dve(lambda e, TI=TI: e.tensor_tensor(out=RkT[:], in0=psG[:], in1=tri[:, TI, :].unsqueeze(1).to_broadcast([128, 2, 128]), op=ALU.mult), ['psGG', 'tri'], ['RkT'])
            dve(lambda e: e.tensor_tensor(out=Pm[:], in0=Mf[0][:], in1=idf[:].unsqueeze(1).to_broadcast([128, 2, 128]), op=ALU.add), ['Mf0', 'idf'], ['Pm'])
            pool(lambda e: e.tensor_tensor(out=Pt[:], in0=Nf[0][:], in1=idf[:].unsqueeze(1).to_broadcast([128, 2, 128]), op=ALU.add), ['Nf0', 'idf'], ['Pt'])
            cur = 0
            for lvl in range(6):
                nxt = 1 - cur
                lastl = lvl == 5
                for h in range(2):
                    pe(lambda e, h=h, cur=cur: e.matmul(psG[:, h, :], lhsT=Nf[cur][:, h, :], rhs=Mf[cur][:, h, :], start=True, stop=True), [f'Nf{cur}', f'Mf{cur}'], ['psGG'])
                act(lambda e, nxt=nxt: e.activation(out=Mf[nxt][:], in_=psG[:], func=AF.Copy), ['psGG'], [f'Mf{nxt}'])
                if not lastl:
                    for h in range(2):
                        pe(lambda e, h=h, cur=cur: e.matmul(psG2[:, h, :], lhsT=Mf[cur][:, h, :], rhs=Nf[cur][:, h, :], start=True, stop=True), [f'Nf{cur}', f'Mf{cur}'], ['psGG'])
                    act(lambda e, nxt=nxt: e.activation(out=Nf[nxt][:], in_=psG2[:], func=AF.Copy), ['psGG'], [f'Nf{nxt}'])
                for h in range(2):
                    pe(lambda e, h=h, nxt=nxt: e.matmul(psG[:, h, :], lhsT=Pt[:, h, :], rhs=Mf[nxt][:, h, :], start=True, stop=True), ['Pt', f'Mf{nxt}'], ['psGG'])
                if not lastl:
                    for h in range(2):
                        pe(lambda e, h=h, nxt=nxt: e.matmul(psG2[:, h, :], lhsT=Mf[nxt][:, h, :], rhs=Pt[:, h, :], start=True, stop=True), ['Pt', f'Mf{nxt}'], ['psGG'])
                dve(lambda e: e.tensor_tensor(out=Pm[:], in0=Pm[:], in1=psG[:], op=ALU.add), ['Pm', 'psGG'], ['Pm'])
                if not lastl:
                    dve(lambda e: e.tensor_tensor(out=Pt[:], in0=Pt[:], in1=psG2[:], op=ALU.add), ['Pt', 'psGG'], ['Pt'])
                cur = nxt
            for h in range(2):
                pe(lambda e, h=h: e.matmul(psM[:, h, :], lhsT=AT(h), rhs=Sb[:, h, :], start=True, stop=False), ['opT', 'Sb'], ['psX'])
                pe(lambda e, h=h: e.matmul(psM[:, h, :], lhsT=AkT[:, h, :], rhs=vb[:, h * 64:(h + 1) * 64], start=False, stop=True), ['AkT', 'vb'], ['psX'])
            act(lambda e: e.activation(out=rhs_f[:], in_=psM[:], func=AF.Copy), ['psX'], ['rhs_f'])
            for h in range(2):
                pe(lambda e, h=h: e.matmul(psM[:, h, :], lhsT=Pm[:, h, :], rhs=rhs_f[:, h, :], start=True, stop=True), ['Pm', 'rhs_f'], ['psX'])
            act(lambda e: e.activation(out=Ub[:], in_=psM[:], func=AF.Copy), ['psX'], ['Ub'])
            for h in range(2):
                pe(lambda e, h=h: e.matmul(psM[:, h, :], lhsT=RT(h), rhs=Sb[:, h, :], start=True, stop=False), ['opT', 'Sb'], ['psX'])
                pe(lambda e, h=h: e.matmul(psM[:, h, :], lhsT=RbT[:, h, :], rhs=Ub[:, h, :], start=False, stop=False), ['RbT', 'Ub'], ['psX'])
                pe(lambda e, h=h: e.matmul(psM[:, h, :], lhsT=RkT[:, h, :], rhs=vb[:, h * 64:(h + 1) * 64], start=False, stop=True), ['RkT', 'vb'], ['psX'])
            act(lambda e: e.activation(out=yv[:], in_=psM[:].rearrange('p a b -> p (a b)'), func=AF.Copy), ['psX'], ['yv'])
            for h in range(2):
                pe(lambda e, h=h: e.matmul(psS[:, h, :], lhsT=ops4[:, 1, h * 64:(h + 1) * 64], rhs=Ub[:, h, :], start=True, stop=False), ['ops4', 'Ub'], ['psX'])
                pe(lambda e, h=h: e.matmul(psS[:, h, :], lhsT=ops4[:, 2, h * 64:(h + 1) * 64], rhs=vb[:, h * 64:(h + 1) * 64], start=False, stop=True), ['ops4', 'vb'], ['psX'])
            dve(lambda e: e.tensor_tensor(out=St[:], in0=Sf[:], in1=psS[:], op=ALU.add), ['Sf', 'psX'], ['St'])
            for h in range(2):
                pe(lambda e, h=h: e.matmul(psS[:, h, :], lhsT=Dg[:, h, :], rhs=St[:, h, :], start=True, stop=True), ['Dg', 'St'], ['psX'])
            act(lambda e: e.activation(out=Sf[:], in_=psS[:], func=AF.Copy), ['psX'], ['Sf'])
            act(lambda e: e.activation(out=Sb[:], in_=Sf[:], func=AF.Copy), ['Sf'], ['Sb'])
            if d == 0:
                P.dma(lambda e, c=c: e.dma_start(out=Y0[c], in_=yv[:]), reads=['yv'], writes=[('Y0', c)])
            else:
                P.dma(lambda e, c=c: e.dma_start(out=y0[:], in_=Y0[c]), reads=[('Y0', c)], writes=['y0'])
                dve(lambda e: e.tensor_tensor(out=yv[:], in0=yv[:], in1=y0[:], op=ALU.add), ['yv', 'y0'], ['yv'])
                y3 = yv[:].rearrange('p (a b) -> p a b', a=2)
                yc3 = yc[:].rearrange('p (a b) -> p a b', a=2)
                dve(lambda e: e.reduce_sum(out=s1[:], in_=y3, axis=AX.X), ['yv'], ['s1'])
                dve(lambda e: e.tensor_single_scalar(out=s1[:], in_=s1[:], scalar=1.0 / 64.0, op=ALU.mult), ['s1'], ['s1'])
                dve(lambda e: e.tensor_tensor(out=yc3, in0=y3, in1=s1[:].unsqueeze(2).to_broadcast([128, 2, 64]), op=ALU.subtract), ['yv', 's1'], ['yc'])
                act(lambda e: e.activation(out=sq[:], in_=yc[:], func=AF.Square), ['yc'], ['sq'])
                dve(lambda e: e.reduce_sum(out=s2[:], in_=sq[:].rearrange('p (a b) -> p a b', a=2), axis=AX.X), ['sq'], ['s2'])
                act(lambda e: e.activation(out=s2[:], in_=s2[:], func=AF.Sqrt, bias=ceps[:, 0:1], scale=1.0 / 64.0), ['s2', 'ceps'], ['s2'])
                dve(lambda e: e.reciprocal(out=s2[:], in_=s2[:]), ['s2'], ['s2'])
                dve(lambda e: e.tensor_tensor(out=yc3, in0=yc3, in1=s2[:].unsqueeze(2).to_broadcast([128, 2, 64]), op=ALU.mult), ['yc', 's2'], ['yc'])
                dve(lambda e: e.tensor_tensor(out=yc[:], in0=yc[:], in1=rep[:, 896:1024], op=ALU.mult), ['yc', 'rep'], ['yc'])
                dve(lambda e: e.tensor_tensor(out=yc[:], in0=yc[:], in1=rep[:, 1024:1152], op=ALU.add), ['yc', 'rep'], ['yc'])
                dve(lambda e: e.tensor_tensor(out=tmp[:], in0=kd[0][:], in1=kd[1][:], op=ALU.add), ['kd0', 'kd1'], ['tmp'])
                dve(lambda e: e.tensor_tensor(out=tmp[:], in0=tmp[:], in1=rkv[:, 0:128], op=ALU.mult), ['tmp', 'rkv'], ['tmp'])
                dve(lambda e: e.tensor_tensor(out=tmp[:], in0=tmp[:], in1=rep[:, 768:896], op=ALU.mult), ['tmp', 'rep'], ['tmp'])
                dve(lambda e: e.reduce_sum(out=bsum[:], in_=tmp[:].rearrange('p (a b) -> p a b', a=2), axis=AX.X), ['tmp'], ['bsum'])
                dve(lambda e: e.tensor_tensor(out=tmp[:].rearrange('p (a b) -> p a b', a=2), in0=rkv[:, 256:384].rearrange('p (a b) -> p a b', a=2),
                                              in1=bsum[:].unsqueeze(2).to_broadcast([128, 2, 64]), op=ALU.mult), ['rkv', 'bsum'], ['tmp'])
                dve(lambda e: e.tensor_tensor(out=yc[:], in0=yc[:], in1=tmp[:], op=ALU.add), ['yc', 'tmp'], ['yc'])
                dve(lambda e: e.tensor_tensor(out=rwout[:], in0=yc[:], in1=gate[:], op=ALU.mult), ['yc', 'gate'], ['rwout'])
                P.dma(lambda e, c=c: e.dma_start(out=mix[c * 128:(c + 1) * 128, 128:256], in_=rwout[:]), reads=['rwout'], is_out=True)
    return P.finish()


def to_fm(a):
    T = a.shape[0]
    return np.ascontiguousarray(a.T.reshape(8, 128, T).transpose(1, 0, 2))

def from_fm(b):
    T = b.shape[2]
    return np.ascontiguousarray(b.transpose(1, 0, 2).reshape(1024, T).T)

def vecfm(v):
    return np.ascontiguousarray(v.reshape(8, 128).T)

def mods_inputs(c, c_ctx, ada_w, ada_b):
    A = np.concatenate([ada_w[0], ada_w[1]], axis=1)
    bvec = np.concatenate([ada_b[0], ada_b[1]], axis=0)
    cmat = np.zeros((4, 1024), np.float32)
    cmat[0] = c[0]; cmat[1] = c[1]; cmat[2] = c_ctx
    cT = np.ascontiguousarray(cmat.T.reshape(8, 128, 4).transpose(1, 0, 2))
    maps = []
    for k in range(8):
        Ak = A[:, k * 1536:(k + 1) * 1536]
        aw = np.ascontiguousarray(Ak.reshape(8, 128, 1536).transpose(1, 0, 2))
        ab = np.ascontiguousarray(bvec[k * 1536:(k + 1) * 1536].reshape(12, 128).T)
        maps.append({'cT': cT, 'aw': aw, 'ab': ab})
    return maps

def mods_assemble(results):
    full = np.zeros((12288, 4), np.float32)
    for k, r in enumerate(results):
        m = r['mods']
        full[k * 1536:(k + 1) * 1536] = m.transpose(1, 0, 2).reshape(1536, 4)
    return np.ascontiguousarray(full.T[:3].reshape(3, 2, 6, 1024).transpose(1, 0, 2, 3))

def moe_weight_maps(l, moe_w_gate, moe_w_up, moe_w_down, sh_w_gate, sh_w_up, sh_w_down):
    maps = []
    for c in range(8):
        g = np.concatenate([moe_w_gate[l, 32 * c:32 * c + 32], sh_w_gate[l][None]], 0)
        u = np.concatenate([moe_w_up[l, 32 * c:32 * c + 32], sh_w_up[l][None]], 0)
        d = np.concatenate([moe_w_down[l, 32 * c:32 * c + 32], sh_w_down[l][None]], 0)
        maps.append({'wg': np.ascontiguousarray(g.reshape(33, 8, 128, 256).transpose(0, 2, 1, 3)),
                     'wu': np.ascontiguousarray(u.reshape(33, 8, 128, 256).transpose(0, 2, 1, 3)),
                     'wd': np.ascontiguousarray(d.reshape(33, 2, 128, 1024).transpose(0, 2, 1, 3))})
    return maps

def moe_wt_maps(Wt_all):
    T_all = Wt_all.shape[0]
    out = []
    for c in range(8):
        w = np.zeros((33, T_all), np.float32)
        w[:32] = Wt_all[:, 32 * c:32 * c + 32].T
        if c == 0:
            w[32] = 1.0
        out.append(w)
    return out

def ml_maps(h_lat, h_ctx, mods, od_w_in, ml_gate_b, ml_norm_g, l=1):
    W = od_w_in[0]
    gbm = ml_gate_b[0]
    t = np.arange(8192)
    inv = (10000.0 ** (-np.arange(16, dtype=np.float32) / 16)).astype(np.float32)
    ang_r = (t // 64).astype(np.float32)[:, None] * inv[None]
    ang_c = (t % 64).astype(np.float32)[:, None] * inv[None]
    cos = np.concatenate([np.cos(ang_r), np.cos(ang_c)], 1).astype(np.float32)
    sin = np.concatenate([np.sin(ang_r), np.sin(ang_c)], 1).astype(np.float32)
    cosT = np.ascontiguousarray(cos.reshape(64, 128, 32).transpose(1, 0, 2))
    sinT = np.ascontiguousarray(sin.reshape(64, 128, 32).transpose(1, 0, 2))
    s_ = np.arange(128)
    tri = np.stack([(s_[:, None] <= s_[None, :]), (s_[:, None] >= s_[None, :])]).astype(np.float32)
    ident = np.eye(128, dtype=np.float32)
    maps = []
    for c in range(8):
        b, g = c // 4, c % 4
        cols = np.concatenate([np.arange(2 * g * 64, (2 * g + 2) * 64), 512 + np.arange(2 * g * 64, (2 * g + 2) * 64),
                               1024 + np.arange(2 * g * 128, (2 * g + 2) * 128), 2048 + np.arange(2 * g * 128, (2 * g + 2) * 128),
                               3072 + np.arange(2 * g, 2 * g + 2), 3080 + np.arange(2 * g, 2 * g + 2),
                               3088 + np.arange(2 * g, 2 * g + 2), 3096 + np.arange(2 * g, 2 * g + 2)])
        Wc = W[:, cols]
        gb = np.concatenate([gbm[0, 2 * g:2 * g + 2], gbm[1, 2 * g:2 * g + 2], gbm[2, 2 * g:2 * g + 2], gbm[3, 2 * g:2 * g + 2]])
        vec = np.stack([vecfm(mods[l, b, 1]), vecfm(mods[l, b, 0]), vecfm(mods[l, 2, 1]), vecfm(mods[l, 2, 0])], -1)
        maps.append({'hT': to_fm(np.concatenate([h_ctx[b], h_lat[b]], 0)), 'vec': np.ascontiguousarray(vec),
                     'W': np.ascontiguousarray(Wc.reshape(8, 128, 776).transpose(1, 0, 2)),
                     'gb': np.ascontiguousarray(np.broadcast_to(gb, (128, 8))), 'cosT': cosT, 'sinT': sinT,
                     'ng': np.ascontiguousarray(np.broadcast_to(ml_norm_g[0][2 * g * 128:(2 * g + 2) * 128], (128, 256))),
                     'tri': tri, 'ident': ident})
    return maps

def ml_assemble(results):
    out = np.zeros((2, 8192, 1024), np.float32)
    for c, r in enumerate(results):
        b, g = c // 4, c % 4
        out[b][:, 2 * g * 128:(2 * g + 2) * 128] = r['mix']
    return out

def _na_bias_index(h, i, j):
    if 2 <= i <= 61:
        return h * 5 + (j - i + 2)
    e = {0: 0, 1: 1, 62: 2, 63: 3}[i]
    jb = j if i < 2 else j - 60
    return 10 + (e * 4 + jb) * 2 + h

def _na_bias_tile(rpb_h, i, j):
    kr = np.arange(2)[:, None, None, None]; jk = np.arange(64)[None, :, None, None]
    qr = np.arange(2)[None, None, :, None]; jq = np.arange(64)[None, None, None, :]
    kr_abs = 2 * j + kr; r = 2 * i + qr
    rs = np.clip(r - 4, 0, 120)
    valid = (kr_abs >= rs) & (kr_abs < rs + 8)
    cs = np.clip(jq - 8, 0, 48)
    valid = valid & (jk >= cs) & (jk < cs + 16)
    ro = np.clip(kr_abs - r + 7, 0, 14); co = np.clip(jk - jq, -15, 15) + 15
    ro, co, valid = np.broadcast_arrays(ro, co, valid)
    val = rpb_h[ro, co]
    return np.where(valid, val, np.float32(-30000.0)).astype(np.float32).reshape(128, 128)

def ev_maps(x, ctx, mods, inp, l=0):
    W = inp['ev_w_in'][0]; mu = inp['rw_mu'][0]
    s_ = np.arange(128)
    tri = np.zeros((2, 2, 128, 128), np.float32)
    tri[0, 0] = s_[:, None] <= s_[None, :]; tri[0, 1] = s_[:, None] < s_[None, :]
    tri[1, 0] = s_[:, None] >= s_[None, :]; tri[1, 1] = s_[:, None] > s_[None, :]
    ident = np.eye(128, dtype=np.float32)
    maps = []
    for c in range(8):
        b, g = c // 4, c % 4
        hc = np.arange(2 * g * 64, (2 * g + 2) * 64)
        cols = np.concatenate([1536 + hc, 2048 + hc, 2560 + hc, np.arange(3072, 3200), np.arange(3200, 3296), hc, 512 + hc, 1024 + hc])
        mucols = np.concatenate([hc, 512 + hc, 1024 + hc, np.arange(1536, 1664), np.arange(1664, 1760)])
        rep = np.concatenate([inp['rw_w0'][0][0][hc], inp['rw_w0'][0][1][hc], inp['rw_a0'][0][0][hc], inp['rw_a0'][0][1][hc],
                              inp['rw_k_k'][0][hc], inp['rw_k_a'][0][hc], inp['rw_r_k'][0].reshape(-1)[hc],
                              inp['rw_gn_g'][0][hc], inp['rw_gn_b'][0][hc]])
        UP = np.zeros((128, 512), np.float32)
        UP[0:32, 0:128] = inp['rw_w_up'][0][0][:, hc]; UP[32:64, 128:256] = inp['rw_w_up'][0][1][:, hc]
        UP[64:96, 256:384] = inp['rw_a_up'][0][0][:, hc]; UP[96:128, 384:512] = inp['rw_a_up'][0][1][:, hc]
        nab = np.zeros((42, 128, 128), np.float32)
        for h in range(2):
            rp = inp['na_rpb'][0][2 * g + h]
            for o in range(-2, 3):
                nab[_na_bias_index(h, 10, 10 + o)] = _na_bias_tile(rp, 10, 10 + o)
            for i in (0, 1):
                for j in range(4):
                    nab[_na_bias_index(h, i, j)] = _na_bias_tile(rp, i, j)
            for i in (62, 63):
                for j in range(60, 64):
                    nab[_na_bias_index(h, i, j)] = _na_bias_tile(rp, i, j)
        hfm = to_fm(np.concatenate([ctx[b], x[b]], 0))
        hpad = np.zeros((128, 8, hfm.shape[2] + 2), np.float32); hpad[:, :, 1:-1] = hfm
        vec = np.stack([vecfm(mods[l, b, 1]), vecfm(mods[l, b, 0]), vecfm(mods[l, 2, 1]), vecfm(mods[l, 2, 0])], -1)
        maps.append({'hT': hpad, 'vec': np.ascontiguousarray(vec), 'WA': np.ascontiguousarray(W[:, cols].reshape(8, 128, 992).transpose(1, 0, 2)),
                     'mu': np.ascontiguousarray(np.broadcast_to(mu[mucols], (128, 608))), 'rep': np.ascontiguousarray(np.broadcast_to(rep, (128, 1152))),
                     'UP': UP, 'GUP': np.ascontiguousarray(np.concatenate([inp['rw_g_up'][0][:, hc], np.zeros((32, 128), np.float32)], 0)), 'tri': tri, 'ident': ident, 'nab': nab})
    return maps

def ev_assemble(results):
    lat = np.zeros((2, 8192, 1024), np.float32); ctx = np.zeros((2, 256, 1024), np.float32)
    for c, r in enumerate(results):
        b, g = c // 4, c % 4
        m = r['mix']
        for dst, rows in ((ctx, slice(0, 256)), (lat, slice(256, 8448))):
            dst[b][:, 2 * g * 64:(2 * g + 2) * 64] = m[rows, 0:128]
            dst[b][:, 512 + 2 * g * 64:512 + (2 * g + 2) * 64] = m[rows, 128:256]
    return lat, ctx


_NC_CACHE = {}


def _get(name, fn):
    if name not in _NC_CACHE:
        _NC_CACHE[name] = fn()
    return _NC_CACHE[name]


def _run(nc, maps):
    return run_bass_kernel_spmd(nc, maps, core_ids=list(range(8))).results


def _ffn_layer(l, last, inputs, mods, h_lat, h_ctx, mix_lat, mix_ctx, w_out):
    TL = 2048
    TCX = 0 if last else 64
    T = TL + TCX
    segs = [(0, TL, 0)] + ([] if last else [(TL, T, 1)])
    woutL = np.ascontiguousarray(w_out.reshape(8, 128, 1024).transpose(1, 0, 2))
    routerL = np.ascontiguousarray(inputs['moe_router'][l].reshape(8, 128, 256).transpose(1, 0, 2))
    rbias = np.ascontiguousarray(np.broadcast_to(inputs['moe_bias'][l], (128, 256)))
    maps = []
    for c in range(8):
        b = c // 4; s = (c % 4) * TL; cs = (c % 4) * 64
        if last:
            xs = h_lat[b, s:s + TL]; ms = mix_lat[b, s:s + TL]
        else:
            xs = np.concatenate([h_lat[b, s:s + TL], h_ctx[b, cs:cs + 64]], 0)
            ms = np.concatenate([mix_lat[b, s:s + TL], mix_ctx[b, cs:cs + 64]], 0)
        vec = np.zeros((128, 8, 8), np.float32)
        vec[:, :, 0] = vecfm(inputs['ln_g'][l, 0]); vec[:, :, 1] = vecfm(inputs['ln_b'][l, 0])
        for m, j in ((0, b), (1, 2)):
            vec[:, :, 2 + 3 * m] = vecfm(mods[l, j, 2]); vec[:, :, 3 + 3 * m] = vecfm(mods[l, j, 3]); vec[:, :, 4 + 3 * m] = vecfm(mods[l, j, 4])
        maps.append({'xT': to_fm(xs), 'mixT': to_fm(ms), 'wout': woutL, 'vec': vec, 'router': routerL, 'rbias': rbias})
    pre = _run(_get(('pre', T), lambda: build_pre(T, segs)), maps)
    fT_all = np.concatenate([pre[c]['fT'] for c in range(8)], axis=2)
    Wt_all = np.concatenate([pre[c]['Wt'] for c in range(8)], axis=0)
    T_all = 8 * T
    wm = moe_weight_maps(l, inputs['moe_w_gate'], inputs['moe_w_up'], inputs['moe_w_down'],
                         inputs['sh_w_gate'], inputs['sh_w_up'], inputs['sh_w_down'])
    wt = moe_wt_maps(Wt_all)
    moe = _run(_get(('moe', T_all), lambda: build_moe(T_all, T)), [dict(wm[c], fT=fT_all, WtT=wt[c]) for c in range(8)])
    del wm
    parts = np.stack([moe[c]['part'] for c in range(8)])
    maps = []
    for c in range(8):
        b = c // 4
        vec = np.zeros((128, 8, 4), np.float32)
        vec[:, :, 0] = vecfm(inputs['ln_g'][l, 1]); vec[:, :, 1] = vecfm(inputs['ln_b'][l, 1])
        vec[:, :, 2] = vecfm(mods[l, b, 5]); vec[:, :, 3] = vecfm(mods[l, 2, 5])
        maps.append({'h1T': pre[c]['h1T'], 'parts': np.ascontiguousarray(parts[:, :, :, c * T:(c + 1) * T]), 'vec': vec})
    post = _run(_get(('post', T), lambda: build_post(T, segs)), maps)
    o_lat = np.zeros((2, 8192, 1024), np.float32)
    o_ctx = None if last else np.zeros((2, 256, 1024), np.float32)
    for c in range(8):
        b = c // 4; s = (c % 4) * TL; cs = (c % 4) * 64
        h2 = from_fm(post[c]['h2T'])
        o_lat[b, s:s + TL] = h2[:TL]
        if not last:
            o_ctx[b, cs:cs + 64] = h2[TL:]
    return o_lat, o_ctx


def kernel(**inputs):
    inputs = {k: np.asarray(v) for k, v in inputs.items()}
    x, ctx = inputs['x'], inputs['ctx']
    mods = mods_assemble(_run(_get('mods', build_mods), mods_inputs(inputs['c'], inputs['c_ctx'], inputs['ada_w'], inputs['ada_b'])))
    ev = _run(_get('ev', build_ev), ev_maps(x, ctx, mods, inputs))
    mix_lat, mix_ctx = ev_assemble(ev)
    h_lat, h_ctx = _ffn_layer(0, False, inputs, mods, x, ctx, mix_lat, mix_ctx, inputs['ev_w_out'][0])
    ml = _run(_get('ml', build_ml), ml_maps(h_lat, h_ctx, mods, inputs['od_w_in'], inputs['ml_gate_b'], inputs['ml_norm_g']))
    mix_lat = ml_assemble(ml)
    h_lat, _ = _ffn_layer(1, True, inputs, mods, h_lat, None, mix_lat, None, inputs['od_w_out'][0])
    return h_lat.astype(np.float32)
```

```python
import math


import numpy as np
from contextlib import ExitStack
import concourse.bass as bass
import concourse.mybir as mybir
from concourse.bass_utils import run_bass_kernel_spmd

F32 = mybir.dt.float32
BF16 = mybir.dt.bfloat16
I32 = mybir.dt.int32
U32 = mybir.dt.uint32
AF = mybir.ActivationFunctionType
ALU = mybir.AluOpType
AX = mybir.AxisListType

ENGS = ['pe', 'act', 'dve', 'pool', 'sp']
NDSEM = 24


class Prog:
    def __init__(self, same_engine_sync=True):
        self.nc = bass.Bass('TRN2', target_bir_lowering=False)
        self.stack = ExitStack()
        self.q = {e: [] for e in ENGS}
        self.cnt = {e: 0 for e in ENGS}
        self.seen = {e: {} for e in ENGS}
        self.lastw = {}
        self.readers = {}
        self.ndma = 0
        self.dslot_last = {}
        self.same_engine_sync = same_engine_sync
        self.sems = {}
        self.uid = 0
        self.out_events = []

    def dram(self, name, shape, dtype, kind):
        return self.nc.dram_tensor(name, list(shape), dtype, kind=kind).ap()

    def inp(self, name, shape, dtype=F32):
        return self.dram(name, shape, dtype, 'ExternalInput')

    def outp(self, name, shape, dtype=F32):
        return self.dram(name, shape, dtype, 'ExternalOutput')

    def scratch(self, name, shape, dtype=F32, addr_space=None):
        if addr_space is None:
            return self.dram(name, shape, dtype, 'Internal')
        return self.nc.dram_tensor(name, list(shape), dtype, kind='Internal', addr_space=addr_space).ap()

    def sb(self, name, shape, dtype=F32):
        t = self.stack.enter_context(self.nc.sbuf_tensor(name, list(shape), dtype))
        return t

    def ps(self, name, shape, dtype=F32):
        t = self.stack.enter_context(self.nc.psum_tensor(name, list(shape), dtype))
        return t

    def _deps(self, eng, reads, writes):
        deps = {}

        def add(ev):
            k, v = ev
            if deps.get(k, 0) < v:
                deps[k] = v
        for k in reads:
            if k in self.lastw:
                add(self.lastw[k])
        for k in writes:
            if k in self.lastw:
                add(self.lastw[k])
            for ev in self.readers.get(k, {}).items():
                add(ev)
        waits = []
        for k, v in deps.items():
            if k == eng and (eng == 'pe' or not self.same_engine_sync):
                continue
            if self.seen[eng].get(k, 0) >= v:
                continue
            self.seen[eng][k] = v
            waits.append((k, v))
        return waits

    def _record(self, ev, reads, writes):
        for k in writes:
            self.lastw[k] = ev
            self.readers[k] = {}
        for k in reads:
            if k in writes:
                continue
            r = self.readers.setdefault(k, {})
            if r.get(ev[0], 0) < ev[1]:
                r[ev[0]] = ev[1]

    def op(self, eng, fn, reads=(), writes=()):
        waits = self._deps(eng, reads, writes)
        self.cnt[eng] += 1
        ev = (eng, self.cnt[eng])
        self.q[eng].append((waits, fn, eng, 1))
        self._record(ev, reads, writes)
        return ev

    def dma(self, fn, reads=(), writes=(), queue='sp', is_out=False):
        slot = (queue, self.ndma % NDSEM)
        self.ndma += 1
        key = ('d',) + slot
        prev = self.dslot_last.get(key, 0)
        waits = self._deps(queue, reads, writes)
        if prev and self.seen[queue].get(key, 0) < prev:
            self.seen[queue][key] = prev
            waits.append((key, prev))
        val = prev + 16
        self.dslot_last[key] = val
        ev = (key, val)
        self.q[queue].append((waits, fn, key, 16))
        self._record(ev, reads, writes)
        if is_out:
            self.out_events.append(ev)
        return ev

    def _sem(self, key):
        if key not in self.sems:
            nm = 's_' + '_'.join(str(x) for x in (key if isinstance(key, tuple) else (key,)))
            self.sems[key] = self.stack.enter_context(self.nc.semaphore(nm))
        return self.sems[key]

    def finish(self):
        nc = self.nc
        fin = {}
        for k, v in self.out_events:
            fin[k] = max(fin.get(k, 0), v)
        self.q['sp'].append(([(k, v) for k, v in fin.items()], None, None, 0))
        for e in ENGS:
            self._sem(e)
            for waits, fn, semkey, inc in self.q[e]:
                for k, v in waits:
                    self._sem(k)
                if semkey is not None:
                    self._sem(semkey)
        block = self.stack.enter_context(nc.Block())
        hmap = {'pe': 'tensor', 'act': 'scalar', 'dve': 'vector', 'pool': 'gpsimd', 'sp': 'sync'}

        def make(e):
            def body(h):
                for waits, fn, semkey, inc in self.q[e]:
                    for k, v in waits:
                        h.wait_ge(self.sems[k], v)
                    if fn is not None:
                        fn(h).then_inc(self.sems[semkey], inc)
            return body
        for e in ENGS:
            if self.q[e]:
                getattr(block, hmap[e])(make(e))
        self.stack.close()
        return nc


def run(prog_or_nc, in_maps, n=None, trace=False):
    nc = prog_or_nc
    n = n or len(in_maps)
    return run_bass_kernel_spmd(nc, in_maps, core_ids=list(range(n)), trace=trace)


ALPHA = (2.0 * 2) ** 0.25
CW = 256
LN_EPS = 1e-5


def bcast_mid(ap2d, n):
    P_, w = ap2d.shape
    return ap2d.unsqueeze(1).to_broadcast([P_, n, w])


def build_mods():
    P = Prog()
    cT = P.inp('cT', [128, 8, 4])
    aw = P.inp('aw', [128, 8, 1536])
    ab = P.inp('ab', [128, 12])
    out = P.outp('mods', [128, 12, 4])
    c_sb = P.sb('c_sb', [128, 8, 4]); s_sb = P.sb('s_sb', [128, 8, 4])
    aw_sb = P.sb('aw_sb', [128, 8, 1536]); ab_sb = P.sb('ab_sb', [128, 12])
    o_sb = P.sb('o_sb', [128, 12, 4])
    ps = P.ps('ps', [128, 12, 4])
    P.dma(lambda e: e.dma_start(out=c_sb[:], in_=cT), writes=['c'])
    P.dma(lambda e: e.dma_start(out=aw_sb[:], in_=aw), writes=['aw'])
    P.dma(lambda e: e.dma_start(out=ab_sb[:], in_=ab), writes=['ab'])
    P.op('act', lambda e: e.activation(out=s_sb[:], in_=c_sb[:], func=AF.Silu), reads=['c'], writes=['s'])
    for ft in range(12):
        for kt in range(8):
            P.op('pe', lambda e, ft=ft, kt=kt: e.matmul(ps[:, ft, :], lhsT=aw_sb[:, kt, ft * 128:(ft + 1) * 128],
                                                         rhs=s_sb[:, kt, :], start=(kt == 0), stop=(kt == 7)),
                 reads=['aw', 's'], writes=['ps'])
    for ft in range(12):
        P.op('act', lambda e, ft=ft: e.activation(out=o_sb[:, ft, :], in_=ps[:, ft, :], func=AF.Identity,
                                                   bias=ab_sb[:, ft:ft + 1], scale=1.0),
             reads=['ps', 'ab'], writes=['o'])
    P.dma(lambda e: e.dma_start(out=out, in_=o_sb[:]), reads=['o'], is_out=True)
    return P.finish()


def emit_ln(P, z, zsq, w, consts, tag, out_h, vec, gi, bi):
    onesD, eps_t, ps_m, ps_q, mean_sb, t1, rstd, tt = consts
    P.op('act', lambda e: e.activation(out=zsq[:, :, :w], in_=z[:, :, :w], func=AF.Square), reads=['z' + tag], writes=['zsq'])
    for ft in range(8):
        P.op('pe', lambda e, ft=ft: e.matmul(ps_m[:, :w], lhsT=onesD[:], rhs=z[:, ft, :w], start=(ft == 0), stop=(ft == 7)),
             reads=['z' + tag, 'onesD'], writes=['ps_m'])
    for ft in range(8):
        P.op('pe', lambda e, ft=ft: e.matmul(ps_q[:, :w], lhsT=onesD[:], rhs=zsq[:, ft, :w], start=(ft == 0), stop=(ft == 7)),
             reads=['zsq', 'onesD'], writes=['ps_q'])
    P.op('act', lambda e: e.activation(out=mean_sb[:, :w], in_=ps_m[:, :w], func=AF.Copy), reads=['ps_m'], writes=['mean'])
    P.op('dve', lambda e: e.tensor_tensor(out=t1[:, :w], in0=mean_sb[:, :w], in1=mean_sb[:, :w], op=ALU.mult), reads=['mean'], writes=['t1'])
    P.op('dve', lambda e: e.tensor_tensor(out=t1[:, :w], in0=ps_q[:, :w], in1=t1[:, :w], op=ALU.subtract), reads=['ps_q', 't1'], writes=['t1'])
    P.op('act', lambda e: e.activation(out=t1[:, :w], in_=t1[:, :w], func=AF.Sqrt, bias=eps_t[:, 0:1], scale=1.0), reads=['t1', 'eps'], writes=['t1'])
    P.op('dve', lambda e: e.reciprocal(out=rstd[:, :w], in_=t1[:, :w]), reads=['t1'], writes=['rstd'])
    P.op('dve', lambda e: e.tensor_tensor(out=tt[:, :, :w], in0=z[:, :, :w], in1=bcast_mid(mean_sb[:, :w], 8), op=ALU.subtract),
         reads=['z' + tag, 'mean'], writes=['tt'])
    P.op('dve', lambda e: e.tensor_tensor(out=tt[:, :, :w], in0=tt[:, :, :w], in1=bcast_mid(rstd[:, :w], 8), op=ALU.mult),
         reads=['tt', 'rstd'], writes=['tt'])
    for ft in range(8):
        P.op('act', lambda e, ft=ft: e.activation(out=out_h[:, ft, :w], in_=tt[:, ft, :w], func=AF.Identity,
                                                   scale=vec[:, ft, gi:gi + 1], bias=vec[:, ft, bi:bi + 1]),
             reads=['tt', 'vec'], writes=['h' + tag])


def ln_consts(P):
    onesD = P.sb('onesD', [128, 128]); eps_t = P.sb('eps_t', [128, 1])
    ps_m = P.ps('ps_m', [128, 512]); ps_q = P.ps('ps_q', [128, 512])
    mean_sb = P.sb('mean_sb', [128, 512]); t1 = P.sb('t1', [128, 512]); rstd = P.sb('rstd', [128, 512])
    tt = P.sb('tt', [128, 8, CW])
    P.op('pool', lambda e: e.memset(onesD[:], 1.0 / 1024.0), writes=['onesD'])
    P.op('pool', lambda e: e.memset(eps_t[:], LN_EPS), writes=['eps'])
    return (onesD, eps_t, ps_m, ps_q, mean_sb, t1, rstd, tt)


def chunks_of(segs):
    out = []
    for (a, b, m) in segs:
        c = a
        while c < b:
            w = min(CW, b - c)
            out.append((c, w, m))
            c += w
    return out


def build_pre(T, segs):
    P = Prog()
    xT = P.inp('xT', [128, 8, T]); mixT = P.inp('mixT', [128, 8, T])
    wout = P.inp('wout', [128, 8, 1024]); vec_d = P.inp('vec', [128, 8, 8])
    router = P.inp('router', [128, 8, 256]); rbias = P.inp('rbias', [128, 256])
    h1T = P.outp('h1T', [128, 8, T]); fT = P.outp('fT', [128, 8, T], BF16); Wt = P.outp('Wt', [T, 256])
    wo_f = P.sb('wo_f', [128, 8, 1024]); wo_b = P.sb('wo_b', [128, 8, 1024], BF16)
    vec = P.sb('vec_sb', [128, 8, 8]); rt = P.sb('rt', [128, 8, 256]); rb = P.sb('rb', [128, 256])
    consts = ln_consts(P)
    P.dma(lambda e: e.dma_start(out=wo_f[:], in_=wout), writes=['wo_f'])
    P.dma(lambda e: e.dma_start(out=vec[:], in_=vec_d), writes=['vec'])
    P.dma(lambda e: e.dma_start(out=rt[:], in_=router), writes=['rt'])
    P.dma(lambda e: e.dma_start(out=rb[:], in_=rbias), writes=['rb'])
    P.op('pool', lambda e: e.tensor_copy(out=wo_b[:], in_=wo_f[:]), reads=['wo_f'], writes=['wo_b'])
    for m in range(2):
        P.op('dve', lambda e, m=m: e.tensor_scalar(out=vec[:, :, 4 + 3 * m:5 + 3 * m], in0=vec[:, :, 4 + 3 * m:5 + 3 * m],
                                                   scalar1=1.0, scalar2=None, op0=ALU.add), reads=['vec'], writes=['vec'])
    mx = [P.sb(f'mx{i}', [128, 8, CW]) for i in range(2)]
    xx = [P.sb(f'xx{i}', [128, 8, CW]) for i in range(2)]
    mxb = P.sb('mxb', [128, 8, CW], BF16)
    z = P.sb('z', [128, 8, CW]); zsq = P.sb('zsq', [128, 8, CW])
    h1 = P.sb('h1', [128, 8, CW]); f32t = P.sb('f32t', [128, 8, CW]); fb = P.sb('fb', [128, 8, CW], BF16)
    ps_y = [P.ps(f'ps_y{i}', [128, 512]) for i in range(2)]
    ps_r = P.ps('ps_r', [128, 256])
    s_sb = P.sb('s_sb', [128, 256]); grp = P.sb('grp', [128, 256]); m8 = P.sb('m8', [128, 8, 8])
    gs = P.sb('gs', [128, 8]); g8 = P.sb('g8', [128, 8]); keep = P.sb('keep', [128, 8]); pen = P.sb('pen', [128, 8])
    choice = P.sb('choice', [128, 256]); c8 = P.sb('c8', [128, 8]); sel = P.sb('sel', [128, 256])
    wv = P.sb('wv', [128, 256]); wsum = P.sb('wsum', [128, 1]); rinv = P.sb('rinv', [128, 1]); wt_sb = P.sb('wt_sb', [128, 256])
    for ci, (c0, w, m) in enumerate(chunks_of(segs)):
        b = ci % 2
        P.dma(lambda e, b=b, c0=c0, w=w: e.dma_start(out=mx[b][:, :, :w], in_=mixT[:, :, c0:c0 + w]), writes=[f'mx{b}'])
        P.dma(lambda e, b=b, c0=c0, w=w: e.dma_start(out=xx[b][:, :, :w], in_=xT[:, :, c0:c0 + w]), writes=[f'xx{b}'])
        P.op('pool', lambda e, b=b, w=w: e.tensor_copy(out=mxb[:, :, :w], in_=mx[b][:, :, :w]), reads=[f'mx{b}'], writes=['mxb'])
        P.op('act', lambda e, b=b, w=w: e.activation(out=z[:, :, :w], in_=xx[b][:, :, :w], func=AF.Copy, scale=ALPHA),
             reads=[f'xx{b}'], writes=['z'])
        for ft in range(8):
            pb = ft % 2
            for kt in range(8):
                P.op('pe', lambda e, pb=pb, kt=kt, ft=ft, w=w: e.matmul(ps_y[pb][:, :w], lhsT=wo_b[:, kt, ft * 128:(ft + 1) * 128],
                                                                       rhs=mxb[:, kt, :w], start=(kt == 0), stop=(kt == 7)),
                     reads=['wo_b', 'mxb'], writes=[f'ps_y{pb}'])
            P.op('dve', lambda e, pb=pb, ft=ft, w=w, m=m: e.scalar_tensor_tensor(
                out=z[:, ft, :w], in0=ps_y[pb][:, :w], scalar=vec[:, ft, 2 + 3 * m:3 + 3 * m], in1=z[:, ft, :w],
                op0=ALU.mult, op1=ALU.add), reads=[f'ps_y{pb}', 'vec', 'z'], writes=['z'])
        emit_ln(P, z, zsq, w, consts, '', h1, vec, 0, 1)
        for ft in range(8):
            P.op('act', lambda e, ft=ft, w=w, m=m: e.activation(out=f32t[:, ft, :w], in_=h1[:, ft, :w], func=AF.Identity,
                                                                scale=vec[:, ft, 4 + 3 * m:5 + 3 * m], bias=vec[:, ft, 3 + 3 * m:4 + 3 * m]),
                 reads=['h', 'vec'], writes=['f32t'])
        P.op('pool', lambda e, w=w: e.tensor_copy(out=fb[:, :, :w], in_=f32t[:, :, :w]), reads=['f32t'], writes=['fb'])
        P.dma(lambda e, c0=c0, w=w: e.dma_start(out=h1T[:, :, c0:c0 + w], in_=h1[:, :, :w]), reads=['h'], is_out=True)
        P.dma(lambda e, c0=c0, w=w: e.dma_start(out=fT[:, :, c0:c0 + w], in_=fb[:, :, :w]), reads=['fb'], is_out=True)
        for s0 in range(0, w, 128):
            n = min(128, w - s0)
            for kt in range(8):
                P.op('pe', lambda e, kt=kt, s0=s0, n=n: e.matmul(ps_r[:n, :], lhsT=f32t[:, kt, s0:s0 + n], rhs=rt[:, kt, :],
                                                                start=(kt == 0), stop=(kt == 7)),
                     reads=['f32t', 'rt'], writes=['ps_r'])
            P.op('act', lambda e, n=n: e.activation(out=s_sb[:n, :], in_=ps_r[:n, :], func=AF.Sigmoid), reads=['ps_r'], writes=['s_sb'])
            P.op('dve', lambda e, n=n: e.tensor_tensor(out=grp[:n, :], in0=s_sb[:n, :], in1=rb[:n, :], op=ALU.add),
                 reads=['s_sb', 'rb'], writes=['grp'])
            for gi in range(8):
                P.op('dve', lambda e, n=n, gi=gi: e.max(out=m8[:n, gi, :], in_=grp[:n, gi * 32:(gi + 1) * 32]), reads=['grp'], writes=['m8'])
            P.op('dve', lambda e, n=n: e.tensor_tensor(out=gs[:n, :], in0=m8[:n, :, 0], in1=m8[:n, :, 1], op=ALU.add), reads=['m8'], writes=['gs'])
            P.op('dve', lambda e, n=n: e.max(out=g8[:n, :], in_=gs[:n, :]), reads=['gs'], writes=['g8'])
            P.op('dve', lambda e, n=n: e.tensor_scalar(out=keep[:n, :], in0=gs[:n, :], scalar1=g8[:n, 3:4], scalar2=None, op0=ALU.is_ge),
                 reads=['gs', 'g8'], writes=['keep'])
            P.op('dve', lambda e, n=n: e.tensor_scalar(out=pen[:n, :], in0=keep[:n, :], scalar1=1.0, scalar2=1e30, op0=ALU.subtract, op1=ALU.mult),
                 reads=['keep'], writes=['pen'])
            for gi in range(8):
                P.op('dve', lambda e, n=n, gi=gi: e.tensor_scalar(out=choice[:n, gi * 32:(gi + 1) * 32], in0=grp[:n, gi * 32:(gi + 1) * 32],
                                                                 scalar1=keep[:n, gi:gi + 1], scalar2=pen[:n, gi:gi + 1], op0=ALU.mult, op1=ALU.add),
                     reads=['grp', 'keep', 'pen'], writes=['choice'])
            P.op('dve', lambda e, n=n: e.max(out=c8[:n, :], in_=choice[:n, :]), reads=['choice'], writes=['c8'])
            P.op('dve', lambda e, n=n: e.tensor_scalar(out=sel[:n, :], in0=choice[:n, :], scalar1=c8[:n, 7:8], scalar2=None, op0=ALU.is_ge),
                 reads=['choice', 'c8'], writes=['sel'])
            P.op('dve', lambda e, n=n: e.tensor_tensor(out=wv[:n, :], in0=s_sb[:n, :], in1=sel[:n, :], op=ALU.mult), reads=['s_sb', 'sel'], writes=['wv'])
            P.op('dve', lambda e, n=n: e.reduce_sum(out=wsum[:n, :], in_=wv[:n, :], axis=AX.X), reads=['wv'], writes=['wsum'])
            P.op('dve', lambda e, n=n: e.reciprocal(out=rinv[:n, :], in_=wsum[:n, :]), reads=['wsum'], writes=['rinv'])
            P.op('dve', lambda e, n=n: e.tensor_scalar(out=wt_sb[:n, :], in0=wv[:n, :], scalar1=rinv[:n, 0:1], scalar2=2.5, op0=ALU.mult, op1=ALU.mult),
                 reads=['wv', 'rinv'], writes=['wt_sb'])
            P.dma(lambda e, n=n, r0=c0 + s0: e.dma_start(out=Wt[r0:r0 + n, :], in_=wt_sb[:n, :]), reads=['wt_sb'], is_out=True)
    return P.finish()


def build_moe(T_all, TC, NE=32):
    assert T_all % TC == 0
    P = Prog()
    fT = P.inp('fT', [128, 8, T_all], BF16); WtT = P.inp('WtT', [NE, T_all])
    wg = P.inp('wg', [NE, 128, 8, 256]); wu = P.inp('wu', [NE, 128, 8, 256]); wd = P.inp('wd', [NE, 128, 2, 1024])
    part = P.outp('part', [128, 8, T_all])
    wgb_d = P.scratch('wgb_d', [NE, 128, 8, 256], BF16); wub_d = P.scratch('wub_d', [NE, 128, 8, 256], BF16)
    wdb_d = P.scratch('wdb_d', [NE, 128, 2, 1024], BF16)
    st_f = [P.sb(f'st_f{i}', [128, 2048]) for i in range(2)]
    st_b = [P.sb(f'st_b{i}', [128, 2048], BF16) for i in range(2)]
    k = 0
    for e_ in range(NE):
        for (src, dst) in ((wg, wgb_d), (wu, wub_d), (wd, wdb_d)):
            b = k % 2
            sflat = src[e_].rearrange('p a b -> p (a b)')
            dflat = dst[e_].rearrange('p a b -> p (a b)')
            P.dma(lambda e, b=b, sflat=sflat: e.dma_start(out=st_f[b][:], in_=sflat), writes=[f'st_f{b}'])
            eng = 'pool' if k % 2 == 0 else 'act'
            if eng == 'pool':
                P.op('pool', lambda e, b=b: e.tensor_copy(out=st_b[b][:], in_=st_f[b][:]), reads=[f'st_f{b}'], writes=[f'st_b{b}'])
            else:
                P.op('act', lambda e, b=b: e.activation(out=st_b[b][:], in_=st_f[b][:], func=AF.Copy), reads=[f'st_f{b}'], writes=[f'st_b{b}'])
            P.dma(lambda e, b=b, dflat=dflat: e.dma_start(out=dflat, in_=st_b[b][:]), reads=[f'st_b{b}'], writes=[('wbd', e_, id(dst))])
            k += 1
    ft_sb = [P.sb(f'ft_sb{i}', [128, 8, TC], BF16) for i in range(1)]
    acc = P.sb('acc', [128, 8, TC])
    wgb = [P.sb(f'wgb{i}', [128, 8, 256], BF16) for i in range(2)]
    wub = [P.sb(f'wub{i}', [128, 8, 256], BF16) for i in range(2)]
    wdb = [P.sb(f'wdb{i}', [128, 2, 1024], BF16) for i in range(2)]
    wrow = [P.sb(f'wrow{i}', [1, TC]) for i in range(2)]
    ones1 = P.sb('ones1', [1, 128])
    P.op('pool', lambda e: e.memset(ones1[:], 1.0), writes=['ones1'])
    actb = P.sb('actb', [128, 2, TC], BF16)
    sg = [P.sb(f'sg{i}', [128, 512]) for i in range(2)]
    ps_g = [P.ps(f'ps_g{i}', [128, 512]) for i in range(2)]
    ps_u = [P.ps(f'ps_u{i}', [128, 512]) for i in range(2)]
    ps_w = P.ps('ps_w', [128, 512])
    ps_o = [P.ps(f'ps_o{i}', [128, 512]) for i in range(3)]
    otmp = [P.sb(f'otmp{i}', [128, 512]) for i in range(2)]
    cols = [(c, min(512, TC - c)) for c in range(0, TC, 512)]
    it = 0
    for tc in range(T_all // TC):
        t0 = tc * TC
        fb_ = 0
        P.dma(lambda e, fb_=fb_, t0=t0: e.dma_start(out=ft_sb[fb_][:], in_=fT[:, :, t0:t0 + TC]), writes=[f'ft_sb{fb_}'])
        for e_ in range(NE):
            b = it % 2
            it += 1
            P.dma(lambda e, b=b, e_=e_: e.dma_start(out=wgb[b][:], in_=wgb_d[e_]), reads=[('wbd', e_, id(wgb_d))], writes=[f'wgb{b}'])
            P.dma(lambda e, b=b, e_=e_: e.dma_start(out=wub[b][:], in_=wub_d[e_]), reads=[('wbd', e_, id(wub_d))], writes=[f'wub{b}'])
            P.dma(lambda e, b=b, e_=e_: e.dma_start(out=wdb[b][:], in_=wdb_d[e_]), reads=[('wbd', e_, id(wdb_d))], writes=[f'wdb{b}'])
            P.dma(lambda e, b=b, e_=e_, t0=t0: e.dma_start(out=wrow[b][:], in_=WtT[e_:e_ + 1, t0:t0 + TC]), writes=[f'wrow{b}'])
            q = 0
            for j in range(2):
                for (c0, w) in cols:
                    pb = q % 2
                    q += 1
                    for kt in range(8):
                        P.op('pe', lambda e, pb=pb, kt=kt, j=j, c0=c0, w=w, b=b, fb_=fb_: e.matmul(
                            ps_g[pb][:, :w], lhsT=wgb[b][:, kt, j * 128:(j + 1) * 128], rhs=ft_sb[fb_][:, kt, c0:c0 + w],
                            start=(kt == 0), stop=(kt == 7)), reads=[f'wgb{b}', f'ft_sb{fb_}'], writes=[f'ps_g{pb}'])
                    for kt in range(8):
                        P.op('pe', lambda e, pb=pb, kt=kt, j=j, c0=c0, w=w, b=b, fb_=fb_: e.matmul(
                            ps_u[pb][:, :w], lhsT=wub[b][:, kt, j * 128:(j + 1) * 128], rhs=ft_sb[fb_][:, kt, c0:c0 + w],
                            start=(kt == 0), stop=(kt == 7)), reads=[f'wub{b}', f'ft_sb{fb_}'], writes=[f'ps_u{pb}'])
                    if j == 0:
                        pass
                    P.op('pe', lambda e, c0=c0, w=w, b=b: e.matmul(ps_w[:, :w], lhsT=ones1[:, :], rhs=wrow[b][:, c0:c0 + w], start=True, stop=True),
                         reads=['ones1', f'wrow{b}'], writes=['ps_w'])
                    P.op('act', lambda e, pb=pb, w=w: e.activation(out=sg[pb][:, :w], in_=ps_g[pb][:, :w], func=AF.Silu),
                         reads=[f'ps_g{pb}'], writes=[f'sg{pb}'])
                    P.op('dve', lambda e, pb=pb, w=w: e.tensor_tensor(out=sg[pb][:, :w], in0=sg[pb][:, :w], in1=ps_u[pb][:, :w], op=ALU.mult),
                         reads=[f'sg{pb}', f'ps_u{pb}'], writes=[f'sg{pb}'])
                    P.op('dve', lambda e, pb=pb, w=w, j=j, c0=c0: e.tensor_tensor(out=actb[:, j, c0:c0 + w], in0=sg[pb][:, :w], in1=ps_w[:, :w], op=ALU.mult),
                         reads=[f'sg{pb}', 'ps_w'], writes=['actb'])
            for ft in range(8):
                for (c0, w) in cols:
                    pb = q % 3
                    q += 1
                    for j in range(2):
                        P.op('pe', lambda e, pb=pb, j=j, ft=ft, c0=c0, w=w, b=b: e.matmul(
                            ps_o[pb][:, :w], lhsT=wdb[b][:, j, ft * 128:(ft + 1) * 128], rhs=actb[:, j, c0:c0 + w],
                            start=(j == 0), stop=(j == 1)), reads=[f'wdb{b}', 'actb'], writes=[f'ps_o{pb}'])
                    akey = ('acc', ft, c0)
                    if e_ == 0:
                        P.op('act', lambda e, pb=pb, ft=ft, c0=c0, w=w: e.activation(out=acc[:, ft, c0:c0 + w], in_=ps_o[pb][:, :w], func=AF.Copy),
                             reads=[f'ps_o{pb}'], writes=[akey])
                    elif q % 2 == 0:
                        P.op('dve', lambda e, pb=pb, ft=ft, c0=c0, w=w: e.tensor_tensor(out=acc[:, ft, c0:c0 + w], in0=acc[:, ft, c0:c0 + w],
                                                                                    in1=ps_o[pb][:, :w], op=ALU.add),
                             reads=[f'ps_o{pb}', akey], writes=[akey])
                    else:
                        tb = (q // 2) % 2
                        P.op('act', lambda e, pb=pb, tb=tb, w=w: e.activation(out=otmp[tb][:, :w], in_=ps_o[pb][:, :w], func=AF.Copy),
                             reads=[f'ps_o{pb}'], writes=[f'otmp{tb}'])
                        P.op('pool', lambda e, tb=tb, ft=ft, c0=c0, w=w: e.tensor_tensor(out=acc[:, ft, c0:c0 + w], in0=acc[:, ft, c0:c0 + w],
                                                                                     in1=otmp[tb][:, :w], op=ALU.add),
                             reads=[f'otmp{tb}', akey], writes=[akey])
        P.dma(lambda e, t0=t0: e.dma_start(out=part[:, :, t0:t0 + TC], in_=acc[:]), reads=[('acc', ft, c0) for ft in range(8) for (c0, w) in cols], is_out=True)
    return P.finish()


def build_post(T, segs, NP=8):
    P = Prog()
    h1T = P.inp('h1T', [128, 8, T]); parts = P.inp('parts', [NP, 128, 8, T]); vec_d = P.inp('vec', [128, 8, 4])
    fT = P.inp('fT', [128, 8, T], BF16); sg_d = P.inp('sg', [128, 8, 256]); su_d = P.inp('su', [128, 8, 256]); sd_d = P.inp('sd', [128, 2, 1024])
    h2T = P.outp('h2T', [128, 8, T])
    vec = P.sb('vec_sb', [128, 8, 4])
    consts = ln_consts(P)
    P.dma(lambda e: e.dma_start(out=vec[:], in_=vec_d), writes=['vec'])
    wst = P.sb('wst', [128, 2048]); sgb = P.sb('sgb', [128, 8, 256], BF16); sub = P.sb('sub', [128, 8, 256], BF16); sdb = P.sb('sdb', [128, 2, 1024], BF16)
    for (src, dst, nm) in ((sg_d, sgb, 'sgb'), (su_d, sub, 'sub'), (sd_d, sdb, 'sdb')):
        P.dma(lambda e, src=src: e.dma_start(out=wst[:], in_=src.rearrange('p a b -> p (a b)')), writes=['wst'])
        P.op('pool', lambda e, dst=dst: e.tensor_copy(out=dst[:].rearrange('p a b -> p (a b)'), in_=wst[:]), reads=['wst'], writes=[nm])
    fch = P.sb('fch', [128, 8, CW], BF16); sact = P.sb('sact', [128, 2, CW], BF16); ssg = P.sb('ssg', [128, CW])
    ps_sg = P.ps('ps_sg', [128, 512]); ps_su = P.ps('ps_su', [128, 512]); ps_so = [P.ps(f'ps_so{i}', [128, 512]) for i in range(2)]
    hh = [P.sb(f'hh{i}', [128, 8, CW]) for i in range(2)]
    pp = [P.sb(f'pp{i}', [128, 8, CW]) for i in range(3)]
    ff = P.sb('ff', [128, 8, CW])
    z = P.sb('z', [128, 8, CW]); zsq = P.sb('zsq', [128, 8, CW]); h2 = P.sb('h2', [128, 8, CW])
    k = 0
    for ci, (c0, w, m) in enumerate(chunks_of(segs)):
        b = ci % 2
        P.dma(lambda e, b=b, c0=c0, w=w: e.dma_start(out=hh[b][:, :, :w], in_=h1T[:, :, c0:c0 + w]), writes=[f'hh{b}'])
        for pi in range(NP):
            pb = k % 3
            k += 1
            P.dma(lambda e, pb=pb, pi=pi, c0=c0, w=w: e.dma_start(out=pp[pb][:, :, :w], in_=parts[pi, :, :, c0:c0 + w]), writes=[f'pp{pb}'])
            if pi == 0:
                P.op('pool', lambda e, pb=pb, w=w: e.tensor_copy(out=ff[:, :, :w], in_=pp[pb][:, :, :w]), reads=[f'pp{pb}'], writes=['ff'])
            else:
                P.op('pool', lambda e, pb=pb, w=w: e.tensor_tensor(out=ff[:, :, :w], in0=ff[:, :, :w], in1=pp[pb][:, :, :w], op=ALU.add),
                     reads=[f'pp{pb}', 'ff'], writes=['ff'])
        P.dma(lambda e, c0=c0, w=w: e.dma_start(out=fch[:, :, :w], in_=fT[:, :, c0:c0 + w]), writes=['fch'])
        for j in range(2):
            for kt in range(8):
                P.op('pe', lambda e, j=j, kt=kt, w=w: e.matmul(ps_sg[:, :w], lhsT=sgb[:, kt, j * 128:(j + 1) * 128], rhs=fch[:, kt, :w], start=(kt == 0), stop=(kt == 7)),
                     reads=['sgb', 'fch'], writes=['ps_sg'])
            for kt in range(8):
                P.op('pe', lambda e, j=j, kt=kt, w=w: e.matmul(ps_su[:, :w], lhsT=sub[:, kt, j * 128:(j + 1) * 128], rhs=fch[:, kt, :w], start=(kt == 0), stop=(kt == 7)),
                     reads=['sub', 'fch'], writes=['ps_su'])
            P.op('act', lambda e, w=w: e.activation(out=ssg[:, :w], in_=ps_sg[:, :w], func=AF.Silu), reads=['ps_sg'], writes=['ssg'])
            P.op('dve', lambda e, j=j, w=w: e.tensor_tensor(out=sact[:, j, :w], in0=ssg[:, :w], in1=ps_su[:, :w], op=ALU.mult), reads=['ssg', 'ps_su'], writes=['sact'])
        for ft in range(8):
            pb = ft % 2
            for j in range(2):
                P.op('pe', lambda e, pb=pb, j=j, ft=ft, w=w: e.matmul(ps_so[pb][:, :w], lhsT=sdb[:, j, ft * 128:(ft + 1) * 128], rhs=sact[:, j, :w], start=(j == 0), stop=(j == 1)),
                     reads=['sdb', 'sact'], writes=[f'ps_so{pb}'])
            P.op('dve', lambda e, pb=pb, ft=ft, w=w: e.tensor_tensor(out=ff[:, ft, :w], in0=ff[:, ft, :w], in1=ps_so[pb][:, :w], op=ALU.add),
                 reads=['ff', f'ps_so{pb}'], writes=['ff'])
        P.op('act', lambda e, b=b, w=w: e.activation(out=z[:, :, :w], in_=hh[b][:, :, :w], func=AF.Copy, scale=ALPHA), reads=[f'hh{b}'], writes=['z'])
        for ft in range(8):
            P.op('dve', lambda e, ft=ft, w=w, m=m: e.scalar_tensor_tensor(out=z[:, ft, :w], in0=ff[:, ft, :w], scalar=vec[:, ft, 2 + m:3 + m],
                                                                         in1=z[:, ft, :w], op0=ALU.mult, op1=ALU.add),
                 reads=['ff', 'vec', 'z'], writes=['z'])
        emit_ln(P, z, zsq, w, consts, '', h2, vec, 0, 1)
        P.dma(lambda e, c0=c0, w=w: e.dma_start(out=h2T[:, :, c0:c0 + w], in_=h2[:, :, :w]), reads=['h'], is_out=True)
    return P.finish()


ML_EPS = 1e-6
NCOL = 776


def build_ml(NCTX=2, NLAT=64):
    NCH = NCTX + NLAT
    T = NCH * 128
    P = Prog()
    hT = P.inp('hT', [128, 8, T]); vec_d = P.inp('vec', [128, 8, 4]); W_d = P.inp('W', [128, 8, NCOL])
    gb_d = P.inp('gb', [128, 8]); cos_d = P.inp('cosT', [128, NLAT, 32]); sin_d = P.inp('sinT', [128, NLAT, 32])
    ng_d = P.inp('ng', [128, 256]); tri_d = P.inp('tri', [2, 128, 128]); id_d = P.inp('ident', [128, 128])
    mix = P.outp('mix', [NLAT * 128, 256])
    H0 = P.scratch('H0', [NLAT, 128, 256])

    vec = P.sb('vec_sb', [128, 8, 4]); Wf = P.sb('Wf', [128, 8, NCOL]); Wb = P.sb('Wb', [128, 8, NCOL], BF16)
    gb = P.sb('gb_sb', [128, 8]); cosT = P.sb('cos_sb', [128, NLAT, 32]); sinT = P.sb('sin_sb', [128, NLAT, 32])
    ng = P.sb('ng_sb', [128, 256]); tri = P.sb('tri_sb', [128, 2, 128]); idf = P.sb('idf', [128, 128]); idb = P.sb('idb', [128, 128], BF16)
    onesm = P.sb('onesm', [128, 128]); c1 = P.sb('c1', [128, 1]); cln8 = P.sb('cln8', [128, 1]); ceps = P.sb('ceps', [128, 1])
    P.dma(lambda e: e.dma_start(out=vec[:], in_=vec_d), writes=['vec'])
    P.dma(lambda e: e.dma_start(out=Wf[:], in_=W_d), writes=['Wf'])
    P.dma(lambda e: e.dma_start(out=gb[:], in_=gb_d), writes=['gb'])
    P.dma(lambda e: e.dma_start(out=cosT[:], in_=cos_d), writes=['cos'])
    P.dma(lambda e: e.dma_start(out=sinT[:], in_=sin_d), writes=['sin'])
    P.dma(lambda e: e.dma_start(out=ng[:], in_=ng_d), writes=['ng'])
    for d in range(2):
        P.dma(lambda e, d=d: e.dma_start(out=tri[:, d, :], in_=tri_d[d]), writes=['tri'])
    P.dma(lambda e: e.dma_start(out=idf[:], in_=id_d), writes=['idf'])
    P.op('pool', lambda e: e.tensor_copy(out=Wb[:], in_=Wf[:]), reads=['Wf'], writes=['Wb'])
    P.op('pool', lambda e: e.tensor_copy(out=idb[:], in_=idf[:]), reads=['idf'], writes=['idb'])
    P.op('pool', lambda e: e.memset(onesm[:], 1.0), writes=['onesm'])
    P.op('pool', lambda e: e.memset(c1[:], 1.0), writes=['c1'])
    P.op('pool', lambda e: e.memset(cln8[:], -math.log(8.0)), writes=['cln8'])
    P.op('pool', lambda e: e.memset(ceps[:], ML_EPS), writes=['ceps'])
    for j in (0, 2):
        P.op('dve', lambda e, j=j: e.tensor_scalar(out=vec[:, :, j:j + 1], in0=vec[:, :, j:j + 1], scalar1=1.0, scalar2=None, op0=ALU.add),
             reads=['vec'], writes=['vec'])

    hin = [P.sb(f'hin{i}', [128, 8, 128]) for i in range(2)]
    af = P.sb('af', [128, 8, 128]); ab = P.sb('ab', [128, 8, 128], BF16)
    psA = P.ps('psA', [128, 512]); psB = P.ps('psB', [128, 264])
    psT = P.ps('psT', [64, 4, 128], BF16)
    psG = P.ps('psG', [128, 8])
    psS = P.ps('psS', [128, 128]); psO = P.ps('psO', [128, 129]); psU = P.ps('psU', [64, 129])
    qk = P.sb('qk', [128, 4, 64]); qkb = P.sb('qkb', [128, 4, 64], BF16)
    r1 = P.sb('r1', [128, 4, 2, 16]); r2 = P.sb('r2', [128, 4, 2, 16])
    qkT = P.sb('qkT', [64, 4, 128], BF16)
    gx = P.sb('gx', [128, 2]); gax = P.sb('gax', [128, 2]); ge = P.sb('ge', [128, 2]); gl = P.sb('gl', [128, 2]); lf = P.sb('lf', [128, 2])
    ig = P.sb('ig', [128, 2]); ebq = P.sb('ebq', [128, 2]); ek = P.sb('ek', [128, 2]); EB = P.sb('EB', [128, 2]); gt = P.sb('gt', [128, 2])
    Vt = P.sb('Vt', [128, 2, 129], BF16)
    osig = P.sb('osig', [128, 256])
    sT = P.sb('sT', [128, 128], BF16)
    Cf = P.sb('Cf', [64, 2, 129]); Cb = P.sb('Cb', [64, 2, 129], BF16); Ct = P.sb('Ct', [64, 129])
    dd = P.sb('dd', [128, 1]); rr = P.sb('rr', [128, 1])
    hout = P.sb('hout', [128, 256]); h0 = P.sb('h0', [128, 256]); hsq = P.sb('hsq', [128, 128]); ms = P.sb('ms', [128, 2])
    fin = P.sb('fin', [128, 256])

    nload = 0
    for d in range(2):
        P.op('pool', lambda e: e.memset(Cf[:], 0.0), writes=['Cf'])
        P.op('pool', lambda e: e.memset(Cb[:], 0.0), writes=['Cb'])
        order = list(range(NCTX)) + list(range(NCTX, NCH))
        if d == 1:
            order = list(range(NCTX - 1, -1, -1)) + list(range(NCH - 1, NCTX - 1, -1))
        for c in order:
            is_lat = c >= NCTX
            lc = c - NCTX
            hb = nload % 2
            nload += 1
            P.dma(lambda e, hb=hb, c=c: e.dma_start(out=hin[hb][:], in_=hT[:, :, c * 128:(c + 1) * 128]), writes=[f'hin{hb}'])
            js, jh = (0, 1) if is_lat else (2, 3)
            P.op('dve', lambda e, hb=hb, js=js: e.tensor_tensor(out=af[:], in0=hin[hb][:], in1=vec[:, :, js:js + 1].to_broadcast([128, 8, 128]), op=ALU.mult),
                 reads=[f'hin{hb}', 'vec'], writes=['af'])
            P.op('pool', lambda e, jh=jh: e.tensor_tensor(out=ab[:], in0=af[:], in1=vec[:, :, jh:jh + 1].to_broadcast([128, 8, 128]), op=ALU.add),
                 reads=['af', 'vec'], writes=['ab'])
            for kt in range(8):
                P.op('pe', lambda e, kt=kt: e.matmul(psA[:, :], lhsT=ab[:, kt, :], rhs=Wb[:, kt, 0:512], start=(kt == 0), stop=(kt == 7)),
                     reads=['ab', 'Wb'], writes=['psA'])
            for kt in range(8):
                P.op('pe', lambda e, kt=kt: e.matmul(psB[:, :], lhsT=ab[:, kt, :], rhs=Wb[:, kt, 512:776], start=(kt == 0), stop=(kt == 7)),
                     reads=['ab', 'Wb'], writes=['psB'])
            P.op('act', lambda e: e.activation(out=qk[:].rearrange('p a b -> p (a b)'), in_=psA[:, 0:256], func=AF.Copy), reads=['psA'], writes=['qk'])
            if is_lat:
                q5 = qk[:].rearrange('p a (h u i) -> p a h u i', h=2, u=2)
                o5 = qkb[:].rearrange('p a (h u i) -> p a h u i', h=2, u=2)
                u1 = q5[:, :, :, 0, :]; u2 = q5[:, :, :, 1, :]
                cosb = cosT[:, lc, :].rearrange('p (h i) -> p h i', h=2).unsqueeze(1).to_broadcast([128, 4, 2, 16])
                sinb = sinT[:, lc, :].rearrange('p (h i) -> p h i', h=2).unsqueeze(1).to_broadcast([128, 4, 2, 16])
                P.op('dve', lambda e, u1=u1, cosb=cosb: e.tensor_tensor(out=r1[:], in0=u1, in1=cosb, op=ALU.mult), reads=['qk', 'cos'], writes=['r1'])
                P.op('pool', lambda e, u2=u2, sinb=sinb: e.tensor_tensor(out=r2[:], in0=u2, in1=sinb, op=ALU.mult), reads=['qk', 'sin'], writes=['r2'])
                P.op('dve', lambda e, o5=o5: e.tensor_tensor(out=o5[:, :, :, 0, :], in0=r1[:], in1=r2[:], op=ALU.subtract), reads=['r1', 'r2'], writes=['qkb'])
                P.op('dve', lambda e, u1=u1, sinb=sinb: e.tensor_tensor(out=r1[:], in0=u1, in1=sinb, op=ALU.mult), reads=['qk', 'sin'], writes=['r1'])
                P.op('pool', lambda e, u2=u2, cosb=cosb: e.tensor_tensor(out=r2[:], in0=u2, in1=cosb, op=ALU.mult), reads=['qk', 'cos'], writes=['r2'])
                P.op('dve', lambda e, o5=o5: e.tensor_tensor(out=o5[:, :, :, 1, :], in0=r1[:], in1=r2[:], op=ALU.add), reads=['r1', 'r2'], writes=['qkb'])
            else:
                P.op('dve', lambda e: e.tensor_copy(out=qkb[:], in_=qk[:]), reads=['qk'], writes=['qkb'])
            jlist = (0, 1, 2, 3) if is_lat else ()
            for j in jlist:
                P.op('pe', lambda e, j=j: e.transpose(psT[:, j, :], qkb[:, j, :], idb[:]), reads=['qkb', 'idb'], writes=['psT'])
            if is_lat:
                P.op('act', lambda e: e.activation(out=qkT[:], in_=psT[:], func=AF.Copy), reads=['psT'], writes=['qkT'])
            gi0 = 256 + 2 * d
            gf0 = 256 + 4 + 2 * d
            P.op('dve', lambda e, gf0=gf0, d=d: e.tensor_tensor(out=gx[:], in0=psB[:, gf0:gf0 + 2], in1=gb[:, 4 + 2 * d:6 + 2 * d], op=ALU.add),
                 reads=['psB', 'gb'], writes=['gx'])
            P.op('dve', lambda e, gi0=gi0, d=d: e.tensor_tensor(out=ig[:], in0=psB[:, gi0:gi0 + 2], in1=gb[:, 2 * d:2 + 2 * d], op=ALU.add),
                 reads=['psB', 'gb'], writes=['ig'])
            P.op('act', lambda e: e.activation(out=gax[:], in_=gx[:], func=AF.Abs), reads=['gx'], writes=['gax'])
            P.op('act', lambda e: e.activation(out=ge[:], in_=gax[:], func=AF.Exp, scale=-1.0), reads=['gax'], writes=['ge'])
            P.op('act', lambda e: e.activation(out=gl[:], in_=ge[:], func=AF.Ln, bias=c1[:, 0:1], scale=1.0), reads=['ge', 'c1'], writes=['gl'])
            P.op('dve', lambda e: e.tensor_single_scalar(out=gax[:], in_=gx[:], scalar=0.0, op=ALU.min), reads=['gx', 'ge'], writes=['gax'])
            P.op('dve', lambda e: e.tensor_tensor(out=lf[:], in0=gax[:], in1=gl[:], op=ALU.subtract), reads=['gax', 'gl'], writes=['lf'])
            P.op('pe', lambda e, d=d: e.matmul(psG[:, 0:2], lhsT=tri[:, d, :], rhs=lf[:], start=True, stop=True), reads=['tri', 'lf'], writes=['psG'])
            P.op('pe', lambda e: e.matmul(psG[:, 2:4], lhsT=onesm[:], rhs=lf[:], start=True, stop=True), reads=['onesm', 'lf'], writes=['psG'])
            P.op('act', lambda e: e.activation(out=ebq[:], in_=psG[:, 0:2], func=AF.Exp, bias=cln8[:, 0:1], scale=1.0), reads=['psG', 'cln8'], writes=['ebq'])
            P.op('act', lambda e: e.activation(out=EB[:], in_=psG[:, 2:4], func=AF.Exp), reads=['psG'], writes=['EB'])
            P.op('dve', lambda e: e.tensor_tensor(out=gt[:], in0=ig[:], in1=psG[:, 0:2], op=ALU.subtract), reads=['ig', 'psG'], writes=['gt'])
            P.op('act', lambda e: e.activation(out=ek[:], in_=gt[:], func=AF.Exp), reads=['gt'], writes=['ek'])
            for h in range(2):
                P.op('dve', lambda e, h=h: e.tensor_scalar(out=Vt[:, h, 0:128], in0=psA[:, 256 + h * 128:256 + (h + 1) * 128], scalar1=ek[:, h:h + 1],
                                                         scalar2=None, op0=ALU.mult), reads=['psA', 'ek'], writes=['Vt'])
                P.op('dve', lambda e, h=h: e.tensor_copy(out=Vt[:, h, 128:129], in_=ek[:, h:h + 1]), reads=['ek'], writes=['Vt'])
            if is_lat and d == 1:
                P.op('act', lambda e: e.activation(out=osig[:], in_=psB[:, 0:256], func=AF.Sigmoid), reads=['psB'], writes=['osig'])
                P.dma(lambda e, lc=lc: e.dma_start(out=h0[:], in_=H0[lc]), reads=[('H0', lc)], writes=['h0'])
            for h in range(2):
                if is_lat:
                    P.op('pe', lambda e, h=h: e.matmul(psS[:, :], lhsT=qkT[:, 2 + h, :], rhs=qkT[:, h, :], start=True, stop=True), reads=['qkT'], writes=['psS'])
                    P.op('dve', lambda e, d=d: e.tensor_tensor(out=sT[:], in0=psS[:], in1=tri[:, d, :], op=ALU.mult), reads=['psS', 'tri'], writes=['sT'])
                    P.op('pe', lambda e, h=h: e.matmul(psO[:, :], lhsT=sT[:], rhs=Vt[:, h, :], start=True, stop=False), reads=['sT', 'Vt'], writes=['psO'])
                    P.op('pe', lambda e, h=h: e.matmul(psO[:, :], lhsT=qkT[:, h, :], rhs=Cb[:, h, :], start=False, stop=True), reads=['qkT', 'Cb'], writes=['psO'])
                    P.op('act', lambda e, h=h: e.activation(out=dd[:], in_=psO[:, 128:129], func=AF.Abs, scale=ebq[:, h:h + 1]),
                         reads=['psO', 'ebq'], writes=['dd'])
                    P.op('dve', lambda e: e.tensor_single_scalar(out=dd[:], in_=dd[:], scalar=1.0, op=ALU.max), reads=['dd'], writes=['dd'])
                    P.op('dve', lambda e: e.reciprocal(out=rr[:], in_=dd[:]), reads=['dd'], writes=['rr'])
                    P.op('dve', lambda e, h=h: e.tensor_tensor(out=rr[:], in0=rr[:], in1=ebq[:, h:h + 1], op=ALU.mult), reads=['rr', 'ebq'], writes=['rr'])
                    P.op('dve', lambda e, h=h: e.tensor_scalar(out=hout[:, h * 128:(h + 1) * 128], in0=psO[:, 0:128], scalar1=rr[:, 0:1], scalar2=None, op0=ALU.mult),
                         reads=['psO', 'rr'], writes=['hout'])
                P.op('pe', lambda e, h=h: e.matmul(psU[:, :], lhsT=qkb[:, 2 + h, :], rhs=Vt[:, h, :], start=True, stop=True), reads=['qkb', 'Vt'], writes=['psU'])
                P.op('dve', lambda e, h=h: e.tensor_tensor(out=Ct[:], in0=Cf[:, h, :], in1=psU[:], op=ALU.add), reads=['Cf', 'psU'], writes=['Ct'])
                P.op('dve', lambda e, h=h: e.tensor_scalar(out=Cf[:, h, :], in0=Ct[:], scalar1=EB[0:64, h:h + 1], scalar2=None, op0=ALU.mult),
                     reads=['Ct', 'EB'], writes=['Cf'])
                P.op('act', lambda e, h=h: e.activation(out=Cb[:, h, :], in_=Cf[:, h, :], func=AF.Copy), reads=['Cf'], writes=['Cb'])
            if is_lat and d == 0:
                P.dma(lambda e, lc=lc: e.dma_start(out=H0[lc], in_=hout[:]), reads=['hout'], writes=[('H0', lc)])
            if is_lat and d == 1:
                P.op('dve', lambda e: e.tensor_tensor(out=hout[:], in0=hout[:], in1=h0[:], op=ALU.add), reads=['hout', 'h0'], writes=['hout'])
                for h in range(2):
                    P.op('act', lambda e, h=h: e.activation(out=hsq[:], in_=hout[:, h * 128:(h + 1) * 128], func=AF.Square), reads=['hout'], writes=['hsq'])
                    P.op('dve', lambda e, h=h: e.reduce_sum(out=ms[:, h:h + 1], in_=hsq[:], axis=AX.X), reads=['hsq'], writes=['ms'])
                P.op('act', lambda e: e.activation(out=ms[:], in_=ms[:], func=AF.Sqrt, bias=ceps[:, 0:1], scale=1.0 / 128.0), reads=['ms', 'ceps'], writes=['ms'])
                P.op('dve', lambda e: e.reciprocal(out=ms[:], in_=ms[:]), reads=['ms'], writes=['ms'])
                for h in range(2):
                    P.op('dve', lambda e, h=h: e.scalar_tensor_tensor(out=fin[:, h * 128:(h + 1) * 128], in0=hout[:, h * 128:(h + 1) * 128], scalar=ms[:, h:h + 1],
                                                                    in1=ng[:, h * 128:(h + 1) * 128], op0=ALU.mult, op1=ALU.mult),
                         reads=['hout', 'ms', 'ng'], writes=['fin'])
                P.op('dve', lambda e: e.tensor_tensor(out=fin[:], in0=fin[:], in1=osig[:], op=ALU.mult), reads=['fin', 'osig'], writes=['fin'])
                P.dma(lambda e, lc=lc: e.dma_start(out=mix[lc * 128:(lc + 1) * 128, :], in_=fin[:]), reads=['fin'], is_out=True)
    return P.finish()


RW_GN_EPS = 64e-5
NCTX = 2
NLAT = 64
NCH = NCTX + NLAT
NEGM = -30000.0


def na_key_tiles(i):
    rs0 = min(max(2 * i - 4, 0), 120)
    rs1 = min(max(2 * i + 1 - 4, 0), 120)
    return list(range(rs0 // 2, (rs1 + 7) // 2 + 1))


def na_bias_index(h, i, j):
    if 2 <= i <= 61:
        return h * 5 + (j - i + 2)
    e = {0: 0, 1: 1, 62: 2, 63: 3}[i]
    jb = j if i < 2 else j - 60
    return 10 + (e * 4 + jb) * 2 + h


import os


def build_ev():
    SKIP_NA = os.environ.get('SKIP_NA') == '1'
    SKIP_RW = os.environ.get('SKIP_RW') == '1'
    STAGE = int(os.environ.get('EV_STAGE', '9'))
    SUB = int(os.environ.get('EV_SUB', '9'))
    T = NCH * 128
    P = Prog()
    hT = P.inp('hT', [128, 8, T + 2])
    vec_d = P.inp('vec', [128, 8, 4]); WA_d = P.inp('WA', [128, 8, 992]); mu_d = P.inp('mu', [128, 608])
    rep_d = P.inp('rep', [128, 1152]); up_d = P.inp('UP', [128, 512]); gup_d = P.inp('GUP', [128, 128])
    tri_d = P.inp('tri', [2, 2, 128, 128]); id_d = P.inp('ident', [128, 128]); nab_d = P.inp('nab', [42, 128, 128])
    mix = P.outp('mix', [T, 256])
    Y0 = P.scratch('Y0', [NCH, 128, 128])

    def dve(fn, r, w): return P.op('dve', fn, reads=r, writes=w)
    def act(fn, r, w): return P.op('act', fn, reads=r, writes=w)
    def pool(fn, r, w): return P.op('pool', fn, reads=r, writes=w)
    def pe(fn, r, w): return P.op('pe', fn, reads=r, writes=w)

    vec = P.sb('vec_sb', [128, 8, 4]); WAf = P.sb('WAf', [128, 8, 992]); mu = P.sb('mu_sb', [128, 608])
    W1 = P.sb('W1', [128, 8, 992], BF16); W2 = P.sb('W2', [128, 8, 608], BF16); Wt_ = P.sb('Wtmp', [128, 8, 608])
    rep = P.sb('rep_sb', [128, 1152]); UPf = P.sb('UPf', [128, 512]); UPb = P.sb('UPb', [128, 512], BF16)
    GUf = P.sb('GUf', [128, 128]); GUb = P.sb('GUb', [128, 128], BF16)
    tri = P.sb('tri_sb', [128, 4, 128]); idf = P.sb('idf', [128, 128]); idb = P.sb('idb', [128, 128], BF16)
    nab = P.sb('nab_sb', [128, 42, 128])
    sg = P.sb('sg', [128, 128], BF16); onesm = P.sb('onesm', [128, 128]); ones1 = P.sb('ones1', [128, 1]); c1 = P.sb('c1', [128, 1]); cm05 = P.sb('cm05', [128, 1]); ceps = P.sb('ceps', [128, 1])
    P.dma(lambda e: e.dma_start(out=vec[:], in_=vec_d), writes=['vec'])
    P.dma(lambda e: e.dma_start(out=WAf[:], in_=WA_d), writes=['WAf'])
    P.dma(lambda e: e.dma_start(out=mu[:], in_=mu_d), writes=['mu'])
    P.dma(lambda e: e.dma_start(out=rep[:], in_=rep_d), writes=['rep'])
    P.dma(lambda e: e.dma_start(out=UPf[:], in_=up_d), writes=['UPf'])
    P.dma(lambda e: e.dma_start(out=GUf[:], in_=gup_d), writes=['GUf'])
    for d in range(2):
        for m in range(2):
            P.dma(lambda e, d=d, m=m: e.dma_start(out=tri[:, d * 2 + m, :], in_=tri_d[d, m]), writes=['tri'])
    P.dma(lambda e: e.dma_start(out=idf[:], in_=id_d), writes=['idf'])
    nabb = P.sb('nabb', [128, 42, 128], BF16)
    for k0 in range(42):
        P.dma(lambda e, k0=k0: e.dma_start(out=nab[:, k0, :], in_=nab_d[k0]), writes=['nab'])
    pool(lambda e: e.tensor_copy(out=nabb[:], in_=nab[:]), ['nab'], ['nabb'])
    pool(lambda e: e.tensor_copy(out=idb[:], in_=idf[:]), ['idf'], ['idb'])
    pool(lambda e: e.tensor_copy(out=UPb[:], in_=UPf[:]), ['UPf'], ['UPb'])
    pool(lambda e: e.tensor_copy(out=GUb[:], in_=GUf[:]), ['GUf'], ['GUb'])
    pool(lambda e: e.memset(ones1[:], 1.0), [], ['ones1'])
    pool(lambda e: e.memset(sg[:], 0.0), [], ['sg'])
    pool(lambda e: e.memset(onesm[:], 1.0), [], ['onesm'])
    pool(lambda e: e.memset(c1[:], 1.0), [], ['c1'])
    pool(lambda e: e.memset(cm05[:], -0.5), [], ['cm05'])
    pool(lambda e: e.memset(ceps[:], RW_GN_EPS), [], ['ceps'])
    for j in (0, 2):
        dve(lambda e, j=j: e.tensor_scalar(out=vec[:, :, j:j + 1], in0=vec[:, :, j:j + 1], scalar1=1.0, scalar2=None, op0=ALU.add), ['vec'], ['vec'])
    dve(lambda e: e.tensor_tensor(out=Wt_[:], in0=WAf[:, :, 0:608], in1=mu[:].unsqueeze(1).to_broadcast([128, 8, 608]), op=ALU.mult), ['WAf', 'mu'], ['Wt'])
    act(lambda e: e.activation(out=W2[:], in_=Wt_[:], func=AF.Copy, scale=0.5), ['Wt'], ['W2'])
    dve(lambda e: e.tensor_tensor(out=W1[:, :, 0:608], in0=WAf[:, :, 0:608], in1=Wt_[:], op=ALU.subtract), ['WAf', 'Wt'], ['W1'])
    pool(lambda e: e.tensor_copy(out=W1[:, :, 608:992], in_=WAf[:, :, 608:992]), ['WAf'], ['W1'])

    psA = P.ps('psA', [128, 512]); psUP = P.ps('psUP', [128, 512])
    psCB = P.ps('psCB', [128, 512]); psC = psCB[:, 0:384]; psB = psCB[:, 384:512]
    psT = P.ps('psT', [128, 1024], BF16)
    psGG = P.ps('psGG', [128, 4, 128]); psG = psGG[:, 0:2, :]; psG2 = psGG[:, 2:4, :]
    psX = P.ps('psX', [128, 512])
    psM = psX[:, 0:128].rearrange('p (a b) -> p a b', a=2); psS = psX[0:64, 128:256].rearrange('p (a b) -> p a b', a=2)
    psCum = psX[:, 256:384]; psTot = psX[:, 384:512]
    psN = P.ps('psN', [128, 4, 128])
    psVG = P.ps('psVG', [128, 512]); psV = psVG[:, 0:130].rearrange('p (a b) -> p a b', a=2); psGate = psVG[:, 256:384]

    hin = [P.sb(f'hin{i}', [128, 8, 130]) for i in range(2)]
    af = P.sb('af', [128, 8, 130]); ab = P.sb('ab', [128, 8, 128], BF16); sbb = P.sb('sbb', [128, 8, 128], BF16)
    rkv = P.sb('rkv', [128, 384])
    L1 = P.sb('L1', [128, 128], BF16); L1T = P.sb('L1T', [128, 128], BF16)
    sgT = P.sb('sgT', [128, 128], BF16)
    gate = P.sb('gate', [128, 128])
    x_ = P.sb('x_', [128, 128]); ax = P.sb('ax', [128, 128]); ex = P.sb('ex', [128, 128]); lx = P.sb('lx', [128, 128]); mn = P.sb('mn', [128, 128])
    ew = P.sb('ew', [128, 128]); a_d = [P.sb(f'a_d{i}', [128, 128]) for i in range(2)]
    kk = P.sb('kk', [128, 128]); sq = P.sb('sq', [128, 128]); ss = P.sb('ss', [128, 2]); rn = P.sb('rn', [128, 2])
    kd = [P.sb(f'kd{i}', [128, 128]) for i in range(2)]; tmp = P.sb('tmp', [128, 128])
    bt = P.sb('bt', [128, 128])
    eP = P.sb('eP', [128, 128]); eM = P.sb('eM', [128, 128]); eQ = P.sb('eQ', [128, 128]); cme = P.sb('cme', [128, 128])
    ops4 = P.sb('ops4', [128, 4, 128], BF16)
    opT = P.sb('opT', [64, 8, 128], BF16)
    vb = P.sb('vb', [128, 128], BF16)
    eTot = P.sb('eTot', [128, 128]); Dg = P.sb('Dg', [64, 2, 64])
    Mf = [P.sb(f'Mf{i}', [128, 2, 128]) for i in range(2)]; Nf = [P.sb(f'Nf{i}', [128, 2, 128]) for i in range(2)]
    Pm = P.sb('Pm', [128, 2, 128]); Pt = P.sb('Pt', [128, 2, 128])
    AkT = P.sb('AkT', [128, 2, 128], BF16); RbT = P.sb('RbT', [128, 2, 128], BF16); RkT = P.sb('RkT', [128, 2, 128], BF16)
    rhs_f = P.sb('rhs_f', [128, 2, 64]); Ub = P.sb('Ub', [128, 2, 64], BF16)
    Sf = P.sb('Sf', [64, 2, 64]); Sb = P.sb('Sb', [64, 2, 64], BF16); St = P.sb('St', [64, 2, 64])
    yv = P.sb('yv', [128, 128]); y0 = P.sb('y0', [128, 128])
    s1 = P.sb('s1', [128, 2]); yc = P.sb('yc', [128, 128]); s2 = P.sb('s2', [128, 2]); bsum = P.sb('bsum', [128, 2])
    rwout = P.sb('rwout', [128, 128])
    qT = [P.sb(f'qT{i}', [64, 2, 128], BF16) for i in range(8)]
    kT = [P.sb(f'kT{i}', [64, 2, 128], BF16) for i in range(8)]
    vA = [P.sb(f'vA{i}', [128, 2, 66], BF16) for i in range(8)]
    qTc = [P.sb(f'qTc{i}', [64, 2, 128], BF16) for i in range(2)]
    kTc = [P.sb(f'kTc{i}', [64, 2, 128], BF16) for i in range(2)]
    vAc = [P.sb(f'vAc{i}', [128, 2, 66], BF16) for i in range(2)]
    qkb = P.sb('qkb', [128, 256], BF16)
    for i_ in range(8):
        pool(lambda e, i_=i_: e.memset(vA[i_][:], 1.0), [], [f'vA{i_}'])
    for i_ in range(2):
        pool(lambda e, i_=i_: e.memset(vAc[i_][:], 1.0), [], [f'vAc{i_}'])
    pT = P.sb('pT', [128, 4, 128], BF16)
    rden = P.sb('rden', [128, 2]); naout = P.sb('naout', [128, 128])

    def na_block(qt, qkey, ktiles, out_row0, bias_fn):
        groups = [ktiles[i:i + 4] for i in range(0, len(ktiles), 4)]
        for h in range(2):
            first = True
            for gi_, grp in enumerate(groups):
                for jj, (kt_, kkey, va_, vkey, jlat) in enumerate(grp):
                    has_b = jlat is not None
                    pe(lambda e, jj=jj, kt_=kt_, h=h, has_b=has_b: e.matmul(psN[:, jj, :], lhsT=kt_[:, h, :], rhs=qt[:, h, :], start=True, stop=(not has_b)),
                       [kkey, qkey], ['psN'])
                    if has_b:
                        bi = bias_fn(h, jlat)
                        pe(lambda e, jj=jj, bi=bi: e.matmul(psN[:, jj, :], lhsT=idb[:], rhs=nabb[:, bi, :], start=False, stop=True), ['idb', 'nabb'], ['psN'])
                n = len(grp)
                act(lambda e, n=n: e.activation(out=pT[:, 0:n, :], in_=psN[:, 0:n, :], func=AF.Exp), ['psN'], ['pT'])
                for jj, (kt_, kkey, va_, vkey, jlat) in enumerate(grp):
                    last = (gi_ == len(groups) - 1) and (jj == n - 1)
                    pe(lambda e, jj=jj, va_=va_, h=h, first=first, last=last: e.matmul(psV[:, h, :], lhsT=pT[:, jj, :], rhs=va_[:, h, 0:65], start=first, stop=last),
                       ['pT', vkey], ['psVG'])
                    first = False
        dve(lambda e: e.reciprocal(out=rden[:], in_=psV[:, :, 64]), ['psVG'], ['rden'])
        for h in range(2):
            dve(lambda e, h=h: e.tensor_scalar(out=naout[:, h * 64:(h + 1) * 64], in0=psV[:, h, 0:64], scalar1=rden[:, h:h + 1], scalar2=None, op0=ALU.mult),
                ['psVG', 'rden'], ['naout'])
        P.dma(lambda e: e.dma_start(out=mix[out_row0:out_row0 + 128, 0:128], in_=naout[:]), reads=['naout'], is_out=True)

    nload = 0
    for d in range(2):
        TI = 2 * d
        TS = TI + 1
        TSo = 2 * (1 - d) + 1
        pool(lambda e: e.memset(Sf[:], 0.0), [], ['Sf'])
        pool(lambda e: e.memset(Sb[:], 0.0), [], ['Sb'])
        order = list(range(NCH)) if d == 0 else (list(range(NCTX - 1, -1, -1)) + list(range(NCH - 1, NCTX - 1, -1)))
        na_done = set()
        for c in order:
            if STAGE <= 1:
                continue
            is_lat = c >= NCTX
            lc = c - NCTX
            hb = nload % 2
            nload += 1
            col0 = c * 128
            P.dma(lambda e, hb=hb, col0=col0: e.dma_start(out=hin[hb][:], in_=hT[:, :, col0:col0 + 130]), writes=[f'hin{hb}'])
            js, jh = (0, 1) if is_lat else (2, 3)
            dve(lambda e, hb=hb, js=js: e.tensor_tensor(out=af[:], in0=hin[hb][:], in1=vec[:, :, js:js + 1].to_broadcast([128, 8, 130]), op=ALU.mult),
                [f'hin{hb}', 'vec'], ['af'])
            pool(lambda e, jh=jh: e.tensor_tensor(out=af[:], in0=af[:], in1=vec[:, :, jh:jh + 1].to_broadcast([128, 8, 130]), op=ALU.add), ['af', 'vec'], ['af'])
            if c == 0 or c == NCTX:
                pool(lambda e: e.memset(af[:, :, 0:1], 0.0), ['af'], ['af'])
            if c == NCTX - 1 or c == NCH - 1:
                pool(lambda e: e.memset(af[:, :, 129:130], 0.0), ['af'], ['af'])
            act(lambda e: e.activation(out=ab[:], in_=af[:, :, 1:129], func=AF.Copy), ['af'], ['ab'])
            dve(lambda e: e.tensor_tensor(out=sbb[:], in0=af[:, :, 0:128], in1=af[:, :, 2:130], op=ALU.add), ['af'], ['sbb'])
            for kt in range(8):
                pe(lambda e, kt=kt: e.matmul(psA[:, :], lhsT=ab[:, kt, :], rhs=W1[:, kt, 0:512], start=(kt == 0), stop=False), ['ab', 'W1'], ['psA'])
            for kt in range(8):
                pe(lambda e, kt=kt: e.matmul(psA[:, :], lhsT=sbb[:, kt, :], rhs=W2[:, kt, 0:512], start=False, stop=(kt == 7)), ['sbb', 'W2'], ['psA'])
            if d == 1:
                for kt in range(8):
                    pe(lambda e, kt=kt: e.matmul(psB[:, 0:96], lhsT=ab[:, kt, :], rhs=W1[:, kt, 512:608], start=(kt == 0), stop=False), ['ab', 'W1'], ['psCB'])
                for kt in range(8):
                    pe(lambda e, kt=kt: e.matmul(psB[:, 0:96], lhsT=sbb[:, kt, :], rhs=W2[:, kt, 512:608], start=False, stop=(kt == 7)), ['sbb', 'W2'], ['psCB'])
            if d == 0:
                for kt in range(8):
                    pe(lambda e, kt=kt: e.matmul(psC[:, :], lhsT=ab[:, kt, :], rhs=W1[:, kt, 608:992], start=(kt == 0), stop=(kt == 7)), ['ab', 'W1'], ['psCB'])
            if STAGE <= 2:
                continue
            if d == 0:
                if is_lat:
                    slot = lc % 8
                    qt_, kt_t, va_ = qT[slot], kT[slot], vA[slot]
                    qk_, kk_, vk_ = f'qT{slot}', f'kT{slot}', f'vA{slot}'
                else:
                    qt_, kt_t, va_ = qTc[c], kTc[c], vAc[c]
                    qk_, kk_, vk_ = f'qTc{c}', f'kTc{c}', f'vAc{c}'
                act(lambda e: e.activation(out=qkb[:, 0:128], in_=psC[:, 0:128], func=AF.Copy, scale=0.125), ['psCB'], ['qkb'])
                act(lambda e: e.activation(out=qkb[:, 128:256], in_=psC[:, 128:256], func=AF.Copy), ['psCB'], ['qkb'])
                for j in range(4):
                    pe(lambda e, j=j: e.transpose(psT[0:64, j * 128:(j + 1) * 128], qkb[:, j * 64:(j + 1) * 64], idb[:]), ['qkb', 'idb'], ['psT'])
                if SUB >= 2:
                    act(lambda e, qt_=qt_: e.activation(out=qt_[:].rearrange('p a b -> p (a b)'), in_=psT[0:64, 0:256], func=AF.Copy), ['psT'], [qk_])
                    act(lambda e, kt_t=kt_t: e.activation(out=kt_t[:].rearrange('p a b -> p (a b)'), in_=psT[0:64, 256:512], func=AF.Copy), ['psT'], [kk_])
                if SUB >= 3:
                    for h in range(2):
                        act(lambda e, va_=va_, h=h: e.activation(out=va_[:, h, 0:64], in_=psC[:, 256 + h * 64:256 + (h + 1) * 64], func=AF.Copy), ['psCB'], [vk_])
            if d == 0 and not SKIP_NA and STAGE >= 4:
                if c == NCTX - 1:
                    for cq in range(NCTX):
                        kts = [(kTc[j], f'kTc{j}', vAc[j], f'vAc{j}', None) for j in range(NCTX)]
                        na_block(qTc[cq], f'qTc{cq}', kts, cq * 128, None)
                if is_lat:
                    for i in range(NLAT):
                        if i in na_done:
                            continue
                        kl = na_key_tiles(i)
                        if max(kl) > lc:
                            continue
                        na_done.add(i)
                        kts = [(kT[j % 8], f'kT{j % 8}', vA[j % 8], f'vA{j % 8}', j) for j in kl]
                        kts += [(kTc[j], f'kTc{j}', vAc[j], f'vAc{j}', None) for j in range(NCTX)]
                        na_block(qT[i % 8], f'qT{i % 8}', kts, (NCTX + i) * 128, lambda h, j, i=i: na_bias_index(h, i, j))
            if SKIP_RW:
                continue
            act(lambda e: e.activation(out=rkv[:], in_=psA[:, 0:384], func=AF.Copy), ['psA'], ['rkv'])
            act(lambda e: e.activation(out=L1[:, 0:64], in_=psA[:, 384:448], func=AF.Tanh), ['psA'], ['L1'])
            act(lambda e: e.activation(out=L1[:, 64:128], in_=psA[:, 448:512], func=AF.Copy), ['psA'], ['L1'])
            pe(lambda e: e.transpose(psT[:, 512:640], L1[:], idb[:]), ['L1', 'idb'], ['psT'])
            act(lambda e: e.activation(out=L1T[:], in_=psT[:, 512:640], func=AF.Copy), ['psT'], ['L1T'])
            pe(lambda e: e.matmul(psUP[:, :], lhsT=L1T[:], rhs=UPb[:], start=True, stop=True), ['L1T', 'UPb'], ['psUP'])
            if d == 1:
                act(lambda e: e.activation(out=sg[:, 0:96], in_=psB[:, 0:96], func=AF.Sigmoid), ['psCB'], ['sg'])
                pe(lambda e: e.transpose(psT[:, 640:768], sg[:], idb[:]), ['sg', 'idb'], ['psT'])
                act(lambda e: e.activation(out=sgT[:], in_=psT[:, 640:768], func=AF.Copy), ['psT'], ['sgT'])
                pe(lambda e: e.matmul(psGate[:, :], lhsT=sgT[:], rhs=GUb[:], start=True, stop=True), ['sgT', 'GUb'], ['psVG'])
                act(lambda e: e.activation(out=gate[:], in_=psGate[:], func=AF.Copy), ['psVG'], ['gate'])
            dve(lambda e, d=d: e.tensor_tensor(out=x_[:], in0=psUP[:, d * 128:(d + 1) * 128], in1=rep[:, d * 128:(d + 1) * 128], op=ALU.add), ['psUP', 'rep'], ['x_'])
            act(lambda e: e.activation(out=ax[:], in_=x_[:], func=AF.Abs), ['x_'], ['ax'])
            act(lambda e: e.activation(out=ex[:], in_=ax[:], func=AF.Exp, scale=-1.0), ['ax'], ['ex'])
            act(lambda e: e.activation(out=lx[:], in_=ex[:], func=AF.Ln, bias=c1[:, 0:1], scale=1.0), ['ex', 'c1'], ['lx'])
            dve(lambda e: e.tensor_single_scalar(out=mn[:], in_=x_[:], scalar=0.0, op=ALU.min), ['x_'], ['mn'])
            dve(lambda e: e.tensor_tensor(out=mn[:], in0=mn[:], in1=lx[:], op=ALU.subtract), ['mn', 'lx'], ['mn'])
            act(lambda e: e.activation(out=ew[:], in_=mn[:], func=AF.Exp, bias=cm05[:, 0:1], scale=1.0), ['mn', 'cm05'], ['ew'])
            for dd_ in ((d,) if d == 0 else (0, 1)):
                dve(lambda e, dd_=dd_: e.tensor_tensor(out=tmp[:], in0=psUP[:, 256 + dd_ * 128:256 + (dd_ + 1) * 128], in1=rep[:, 256 + dd_ * 128:256 + (dd_ + 1) * 128], op=ALU.add),
                    ['psUP', 'rep'], ['tmp'])
                act(lambda e, dd_=dd_: e.activation(out=a_d[dd_][:], in_=tmp[:], func=AF.Sigmoid), ['tmp'], [f'a_d{dd_}'])
                dve(lambda e, dd_=dd_: e.scalar_tensor_tensor(out=tmp[:], in0=a_d[dd_][:], scalar=-1.0, in1=rep[:, 640:768], op0=ALU.add, op1=ALU.mult),
                    [f'a_d{dd_}', 'rep'], ['tmp'])
                dve(lambda e, dd_=dd_: e.scalar_tensor_tensor(out=kd[dd_][:], in0=tmp[:], scalar=1.0, in1=rkv[:, 128:256], op0=ALU.add, op1=ALU.mult),
                    ['tmp', 'rkv'], [f'kd{dd_}'])
            dve(lambda e: e.tensor_tensor(out=kk[:], in0=rkv[:, 128:256], in1=rep[:, 512:640], op=ALU.mult), ['rkv', 'rep'], ['kk'])
            act(lambda e: e.activation(out=sq[:], in_=kk[:], func=AF.Square), ['kk'], ['sq'])
            dve(lambda e: e.reduce_sum(out=ss[:], in_=sq[:].rearrange('p (a b) -> p a b', a=2), axis=AX.X), ['sq'], ['ss'])
            act(lambda e: e.activation(out=ss[:], in_=ss[:], func=AF.Sqrt), ['ss'], ['ss'])
            dve(lambda e: e.tensor_single_scalar(out=ss[:], in_=ss[:], scalar=1e-12, op=ALU.max), ['ss'], ['ss'])
            dve(lambda e: e.reciprocal(out=rn[:], in_=ss[:]), ['ss'], ['rn'])
            dve(lambda e: e.tensor_tensor(out=kk[:].rearrange('p (a b) -> p a b', a=2), in0=kk[:].rearrange('p (a b) -> p a b', a=2),
                                          in1=rn[:].unsqueeze(2).to_broadcast([128, 2, 64]), op=ALU.mult), ['kk', 'rn'], ['kk'])
            dve(lambda e, d=d: e.tensor_tensor(out=bt[:], in0=kk[:], in1=a_d[d][:], op=ALU.mult), ['kk', f'a_d{d}'], ['bt'])
            pe(lambda e, TI=TI: e.matmul(psCum[:, :], lhsT=tri[:, TI, :], rhs=ew[:], start=True, stop=True), ['tri', 'ew'], ['psX'])
            pe(lambda e: e.matmul(psTot[:, :], lhsT=onesm[:], rhs=ew[:], start=True, stop=True), ['onesm', 'ew'], ['psX'])
            act(lambda e: e.activation(out=eTot[:], in_=psTot[:], func=AF.Exp, scale=-1.0), ['psX'], ['eTot'])
            dve(lambda e: e.tensor_tensor(out=Dg[:], in0=eTot[0:64, :].rearrange('p (a b) -> p a b', a=2), in1=idf[0:64, 0:64].unsqueeze(1).to_broadcast([64, 2, 64]), op=ALU.mult),
                ['eTot', 'idf'], ['Dg'])
            act(lambda e: e.activation(out=eP[:], in_=psCum[:], func=AF.Exp, scale=-1.0), ['psX'], ['eP'])
            act(lambda e: e.activation(out=eM[:], in_=psCum[:], func=AF.Exp), ['psX'], ['eM'])
            dve(lambda e: e.tensor_tensor(out=cme[:], in0=ew[:], in1=psCum[:], op=ALU.subtract), ['ew', 'psX'], ['cme'])
            act(lambda e: e.activation(out=eQ[:], in_=cme[:], func=AF.Exp), ['cme'], ['eQ'])
            dve(lambda e: e.scalar_tensor_tensor(out=ops4[:, 0, :], in0=kk[:], scalar=-1.0, in1=eQ[:], op0=ALU.mult, op1=ALU.mult), ['kk', 'eQ'], ['ops4'])
            dve(lambda e: e.tensor_tensor(out=ops4[:, 1, :], in0=bt[:], in1=eM[:], op=ALU.mult), ['bt', 'eM'], ['ops4'])
            pool(lambda e, d=d: e.tensor_tensor(out=ops4[:, 2, :], in0=kd[d][:], in1=eM[:], op=ALU.mult), [f'kd{d}', 'eM'], ['ops4'])
            pool(lambda e: e.tensor_tensor(out=ops4[:, 3, :], in0=rkv[:, 0:128], in1=eP[:], op=ALU.mult), ['rkv', 'eP'], ['ops4'])
            pool(lambda e: e.tensor_copy(out=vb[:], in_=rkv[:, 256:384]), ['rkv'], ['vb'])
            for o in range(4):
                for h in range(2):
                    pe(lambda e, o=o, h=h: e.transpose(psT[0:64, (o * 2 + h) * 128:(o * 2 + h + 1) * 128], ops4[:, o, h * 64:(h + 1) * 64], idb[:]),
                       ['ops4', 'idb'], ['psT'])
            act(lambda e: e.activation(out=opT[:].rearrange('p a b -> p (a b)'), in_=psT[0:64, :], func=AF.Copy), ['psT'], ['opT'])
            AT = lambda h: opT[:, 0 + h, :]
            BT = lambda h: opT[:, 2 + h, :]
            KT = lambda h: opT[:, 4 + h, :]
            RT = lambda h: opT[:, 6 + h, :]
            for h in range(2):
                pe(lambda e, h=h: e.matmul(psG[:, h, :], lhsT=BT(h), rhs=AT(h), start=True, stop=True), ['opT'], ['psGG'])
                pe(lambda e, h=h: e.matmul(psG2[:, h, :], lhsT=AT(h), rhs=BT(h), start=True, stop=True), ['opT'], ['psGG'])
            dve(lambda e, TS=TS: e.tensor_tensor(out=Mf[0][:], in0=psG[:], in1=tri[:, TS, :].unsqueeze(1).to_broadcast([128, 2, 128]), op=ALU.mult), ['psGG', 'tri'], ['Mf0'])
            dve(lambda e, TSo=TSo: e.tensor_tensor(out=Nf[0][:], in0=psG2[:], in1=tri[:, TSo, :].unsqueeze(1).to_broadcast([128, 2, 128]), op=ALU.mult), ['psGG', 'tri'], ['Nf0'])
            for h in range(2):
                pe(lambda e, h=h: e.matmul(psG[:, h, :], lhsT=KT(h), rhs=AT(h), start=True, stop=True), ['opT'], ['psGG'])
                pe(lambda e, h=h: e.matmul(psG2[:, h, :], lhsT=BT(h), rhs=RT(h), start=True, stop=True), ['opT'], ['psGG'])
            dve(lambda e, TS=TS: e.tensor_tensor(out=AkT[:], in0=psG[:], in1=tri[:, TS, :].unsqueeze(1).to_broadcast([128, 2, 128]), op=ALU.mult), ['psGG', 'tri'], ['AkT'])
            dve(lambda e, TI=TI: e.tensor_tensor(out=RbT[:], in0=psG2[:], in1=tri[:, TI, :].unsqueeze(1).to_broadcast([128, 2, 128]), op=ALU.mult), ['psGG', 'tri'], ['RbT'])
            for h in range(2):
                pe(lambda e, h=h: e.matmul(psG[:, h, :], lhsT=KT(h), rhs=RT(h), start=True, stop=True), ['opT'], ['psGG'])
            dve(lambda e, TI=TI: e.tensor_tensor(out=RkT[:], in0=psG[:], in1=tri[:, TI, :].unsqueeze(1).to_broadcast([128, 2, 128]), op=ALU.mult), ['psGG', 'tri'], ['RkT'])
            dve(lambda e: e.tensor_tensor(out=Pm[:], in0=Mf[0][:], in1=idf[:].unsqueeze(1).to_broadcast([128, 2, 128]), op=ALU.add), ['Mf0', 'idf'], ['Pm'])
            pool(lambda e: e.tensor_tensor(out=Pt[:], in0=Nf[0][:], in1=idf[:].unsqueeze(1).to_broadcast([128, 2, 128]), op=ALU.add), ['Nf0', 'idf'], ['Pt'])
            cur = 0
            for lvl in range(6):
                nxt = 1 - cur
                lastl = lvl == 5
                for h in range(2):
                    pe(lambda e, h=h, cur=cur: e.matmul(psG[:, h, :], lhsT=Nf[cur][:, h, :], rhs=Mf[cur][:, h, :], start=True, stop=True), [f'Nf{cur}', f'Mf{cur}'], ['psGG'])
                act(lambda e, nxt=nxt: e.activation(out=Mf[nxt][:], in_=psG[:], func=AF.Copy), ['psGG'], [f'Mf{nxt}'])
                if not lastl:
                    for h in range(2):
                        pe(lambda e, h=h, cur=cur: e.matmul(psG2[:, h, :], lhsT=Mf[cur][:, h, :], rhs=Nf[cur][:, h, :], start=True, stop=True), [f'Nf{cur}', f'Mf{cur}'], ['psGG'])
                    act(lambda e, nxt=nxt: e.activation(out=Nf[nxt][:], in_=psG2[:], func=AF.Copy), ['psGG'], [f'Nf{nxt}'])
                for h in range(2):
                    pe(lambda e, h=h, nxt=nxt: e.matmul(psG[:, h, :], lhsT=Pt[:, h, :], rhs=Mf[nxt][:, h, :], start=True, stop=True), ['Pt', f'Mf{nxt}'], ['psGG'])
                if not lastl:
                    for h in range(2):
                        pe(lambda e, h=h, nxt=nxt: e.matmul(psG2[:, h, :], lhsT=Mf[nxt][:, h, :], rhs=Pt[:, h, :], start=True, stop=True), ['Pt', f'Mf{nxt}'], ['psGG'])
                dve(lambda e: e.tensor_tensor(out=Pm[:], in0=Pm[:], in1=psG[:], op=ALU.add), ['Pm', 'psGG'], ['Pm'])
                if not lastl:
                    dve(lambda e: e.tensor_tensor(out=Pt[:], in0=Pt[:], in1=psG2[:], op=ALU.add), ['Pt', 'psGG'], ['Pt'])
                cur = nxt
            for h in range(2):
                pe(lambda e, h=h: e.matmul(psM[:, h, :], lhsT=AT(h), rhs=Sb[:, h, :], start=True, stop=False), ['opT', 'Sb'], ['psX'])
                pe(lambda e, h=h: e.matmul(psM[:, h, :], lhsT=AkT[:, h, :], rhs=vb[:, h * 64:(h + 1) * 64], start=False, stop=True), ['AkT', 'vb'], ['psX'])
            act(lambda e: e.activation(out=rhs_f[:], in_=psM[:], func=AF.Copy), ['psX'], ['rhs_f'])
            for h in range(2):
                pe(lambda e, h=h: e.matmul(psM[:, h, :], lhsT=Pm[:, h, :], rhs=rhs_f[:, h, :], start=True, stop=True), ['Pm', 'rhs_f'], ['psX'])
            act(lambda e: e.activation(out=Ub[:], in_=psM[:], func=AF.Copy), ['psX'], ['Ub'])
            for h in range(2):
                pe(lambda e, h=h: e.matmul(psM[:, h, :], lhsT=RT(h), rhs=Sb[:, h, :], start=True, stop=False), ['opT', 'Sb'], ['psX'])
                pe(lambda e, h=h: e.matmul(psM[:, h, :], lhsT=RbT[:, h, :], rhs=Ub[:, h, :], start=False, stop=False), ['RbT', 'Ub'], ['psX'])
                pe(lambda e, h=h: e.matmul(psM[:, h, :], lhsT=RkT[:, h, :], rhs=vb[:, h * 64:(h + 1) * 64], start=False, stop=True), ['RkT', 'vb'], ['psX'])
            act(lambda e: e.activation(out=yv[:], in_=psM[:].rearrange('p a b -> p (a b)'), func=AF.Copy), ['psX'], ['yv'])
            for h in range(2):
                pe(lambda e, h=h: e.matmul(psS[:, h, :], lhsT=ops4[:, 1, h * 64:(h + 1) * 64], rhs=Ub[:, h, :], start=True, stop=False), ['ops4', 'Ub'], ['psX'])
                pe(lambda e, h=h: e.matmul(psS[:, h, :], lhsT=ops4[:, 2, h * 64:(h + 1) * 64], rhs=vb[:, h * 64:(h + 1) * 64], start=False, stop=True), ['ops4', 'vb'], ['psX'])
            dve(lambda e: e.tensor_tensor(out=St[:], in0=Sf[:], in1=psS[:], op=ALU.add), ['Sf', 'psX'], ['St'])
            for h in range(2):
                pe(lambda e, h=h: e.matmul(psS[:, h, :], lhsT=Dg[:, h, :], rhs=St[:, h, :], start=True, stop=True), ['Dg', 'St'], ['psX'])
            act(lambda e: e.activation(out=Sf[:], in_=psS[:], func=AF.Copy), ['psX'], ['Sf'])
            act(lambda e: e.activation(out=Sb[:], in_=Sf[:], func=AF.Copy), ['Sf'], ['Sb'])
            if d == 0:
                P.dma(lambda e, c=c: e.dma_start(out=Y0[c], in_=yv[:]), reads=['yv'], writes=[('Y0', c)])
            else:
                P.dma(lambda e, c=c: e.dma_start(out=y0[:], in_=Y0[c]), reads=[('Y0', c)], writes=['y0'])
                dve(lambda e: e.tensor_tensor(out=yv[:], in0=yv[:], in1=y0[:], op=ALU.add), ['yv', 'y0'], ['yv'])
                y3 = yv[:].rearrange('p (a b) -> p a b', a=2)
                yc3 = yc[:].rearrange('p (a b) -> p a b', a=2)
                dve(lambda e: e.reduce_sum(out=s1[:], in_=y3, axis=AX.X), ['yv'], ['s1'])
                dve(lambda e: e.tensor_single_scalar(out=s1[:], in_=s1[:], scalar=1.0 / 64.0, op=ALU.mult), ['s1'], ['s1'])
                dve(lambda e: e.tensor_tensor(out=yc3, in0=y3, in1=s1[:].unsqueeze(2).to_broadcast([128, 2, 64]), op=ALU.subtract), ['yv', 's1'], ['yc'])
                act(lambda e: e.activation(out=sq[:], in_=yc[:], func=AF.Square), ['yc'], ['sq'])
                dve(lambda e: e.reduce_sum(out=s2[:], in_=sq[:].rearrange('p (a b) -> p a b', a=2), axis=AX.X), ['sq'], ['s2'])
                act(lambda e: e.activation(out=s2[:], in_=s2[:], func=AF.Sqrt, bias=ceps[:, 0:1], scale=1.0 / 64.0), ['s2', 'ceps'], ['s2'])
                dve(lambda e: e.reciprocal(out=s2[:], in_=s2[:]), ['s2'], ['s2'])
                dve(lambda e: e.tensor_tensor(out=yc3, in0=yc3, in1=s2[:].unsqueeze(2).to_broadcast([128, 2, 64]), op=ALU.mult), ['yc', 's2'], ['yc'])
                dve(lambda e: e.tensor_tensor(out=yc[:], in0=yc[:], in1=rep[:, 896:1024], op=ALU.mult), ['yc', 'rep'], ['yc'])
                dve(lambda e: e.tensor_tensor(out=yc[:], in0=yc[:], in1=rep[:, 1024:1152], op=ALU.add), ['yc', 'rep'], ['yc'])
                dve(lambda e: e.tensor_tensor(out=tmp[:], in0=kd[0][:], in1=kd[1][:], op=ALU.add), ['kd0', 'kd1'], ['tmp'])
                dve(lambda e: e.tensor_tensor(out=tmp[:], in0=tmp[:], in1=rkv[:, 0:128], op=ALU.mult), ['tmp', 'rkv'], ['tmp'])
                dve(lambda e: e.tensor_tensor(out=tmp[:], in0=tmp[:], in1=rep[:, 768:896], op=ALU.mult), ['tmp', 'rep'], ['tmp'])
                dve(lambda e: e.reduce_sum(out=bsum[:], in_=tmp[:].rearrange('p (a b) -> p a b', a=2), axis=AX.X), ['tmp'], ['bsum'])
                dve(lambda e: e.tensor_tensor(out=tmp[:].rearrange('p (a b) -> p a b', a=2), in0=rkv[:, 256:384].rearrange('p (a b) -> p a b', a=2),
                                              in1=bsum[:].unsqueeze(2).to_broadcast([128, 2, 64]), op=ALU.mult), ['rkv', 'bsum'], ['tmp'])
                dve(lambda e: e.tensor_tensor(out=yc[:], in0=yc[:], in1=tmp[:], op=ALU.add), ['yc', 'tmp'], ['yc'])
                dve(lambda e: e.tensor_tensor(out=rwout[:], in0=yc[:], in1=gate[:], op=ALU.mult), ['yc', 'gate'], ['rwout'])
                P.dma(lambda e, c=c: e.dma_start(out=mix[c * 128:(c + 1) * 128, 128:256], in_=rwout[:]), reads=['rwout'], is_out=True)
    return P.finish()


def to_fm(a):
    T = a.shape[0]
    return np.ascontiguousarray(a.T.reshape(8, 128, T).transpose(1, 0, 2))

def from_fm(b):
    T = b.shape[2]
    return np.ascontiguousarray(b.transpose(1, 0, 2).reshape(1024, T).T)

def vecfm(v):
    return np.ascontiguousarray(v.reshape(8, 128).T)

def mods_inputs(c, c_ctx, ada_w, ada_b):
    A = np.concatenate([ada_w[0], ada_w[1]], axis=1)
    bvec = np.concatenate([ada_b[0], ada_b[1]], axis=0)
    cmat = np.zeros((4, 1024), np.float32)
    cmat[0] = c[0]; cmat[1] = c[1]; cmat[2] = c_ctx
    cT = np.ascontiguousarray(cmat.T.reshape(8, 128, 4).transpose(1, 0, 2))
    maps = []
    for k in range(8):
        Ak = A[:, k * 1536:(k + 1) * 1536]
        aw = np.ascontiguousarray(Ak.reshape(8, 128, 1536).transpose(1, 0, 2))
        ab = np.ascontiguousarray(bvec[k * 1536:(k + 1) * 1536].reshape(12, 128).T)
        maps.append({'cT': cT, 'aw': aw, 'ab': ab})
    return maps

def mods_assemble(results):
    full = np.zeros((12288, 4), np.float32)
    for k, r in enumerate(results):
        m = r['mods']
        full[k * 1536:(k + 1) * 1536] = m.transpose(1, 0, 2).reshape(1536, 4)
    return np.ascontiguousarray(full.T[:3].reshape(3, 2, 6, 1024).transpose(1, 0, 2, 3))

def moe_weight_maps(l, moe_w_gate, moe_w_up, moe_w_down):
    maps = []
    for c in range(8):
        g = moe_w_gate[l, 32 * c:32 * c + 32]; u = moe_w_up[l, 32 * c:32 * c + 32]; d = moe_w_down[l, 32 * c:32 * c + 32]
        maps.append({'wg': np.ascontiguousarray(g.reshape(32, 8, 128, 256).transpose(0, 2, 1, 3)),
                     'wu': np.ascontiguousarray(u.reshape(32, 8, 128, 256).transpose(0, 2, 1, 3)),
                     'wd': np.ascontiguousarray(d.reshape(32, 2, 128, 1024).transpose(0, 2, 1, 3))})
    return maps

def moe_wt_maps(Wt_all):
    return [np.ascontiguousarray(Wt_all[:, 32 * c:32 * c + 32].T) for c in range(8)]

def shared_maps(l, sh_w_gate, sh_w_up, sh_w_down):
    return {'sg': np.ascontiguousarray(sh_w_gate[l].reshape(8, 128, 256).transpose(1, 0, 2)),
            'su': np.ascontiguousarray(sh_w_up[l].reshape(8, 128, 256).transpose(1, 0, 2)),
            'sd': np.ascontiguousarray(sh_w_down[l].reshape(2, 128, 1024).transpose(1, 0, 2))}


def ml_maps(h_lat, h_ctx, mods, od_w_in, ml_gate_b, ml_norm_g, l=1):
    W = od_w_in[0]
    gbm = ml_gate_b[0]
    t = np.arange(8192)
    inv = (10000.0 ** (-np.arange(16, dtype=np.float32) / 16)).astype(np.float32)
    ang_r = (t // 64).astype(np.float32)[:, None] * inv[None]
    ang_c = (t % 64).astype(np.float32)[:, None] * inv[None]
    cos = np.concatenate([np.cos(ang_r), np.cos(ang_c)], 1).astype(np.float32)
    sin = np.concatenate([np.sin(ang_r), np.sin(ang_c)], 1).astype(np.float32)
    cosT = np.ascontiguousarray(cos.reshape(64, 128, 32).transpose(1, 0, 2))
    sinT = np.ascontiguousarray(sin.reshape(64, 128, 32).transpose(1, 0, 2))
    s_ = np.arange(128)
    tri = np.stack([(s_[:, None] <= s_[None, :]), (s_[:, None] >= s_[None, :])]).astype(np.float32)
    ident = np.eye(128, dtype=np.float32)
    maps = []
    for c in range(8):
        b, g = c // 4, c % 4
        cols = np.concatenate([np.arange(2 * g * 64, (2 * g + 2) * 64), 512 + np.arange(2 * g * 64, (2 * g + 2) * 64),
                               1024 + np.arange(2 * g * 128, (2 * g + 2) * 128), 2048 + np.arange(2 * g * 128, (2 * g + 2) * 128),
                               3072 + np.arange(2 * g, 2 * g + 2), 3080 + np.arange(2 * g, 2 * g + 2),
                               3088 + np.arange(2 * g, 2 * g + 2), 3096 + np.arange(2 * g, 2 * g + 2)])
        Wc = W[:, cols]
        gb = np.concatenate([gbm[0, 2 * g:2 * g + 2], gbm[1, 2 * g:2 * g + 2], gbm[2, 2 * g:2 * g + 2], gbm[3, 2 * g:2 * g + 2]])
        vec = np.stack([vecfm(mods[l, b, 1]), vecfm(mods[l, b, 0]), vecfm(mods[l, 2, 1]), vecfm(mods[l, 2, 0])], -1)
        maps.append({'hT': to_fm(np.concatenate([h_ctx[b], h_lat[b]], 0)), 'vec': np.ascontiguousarray(vec),
                     'W': np.ascontiguousarray(Wc.reshape(8, 128, 776).transpose(1, 0, 2)),
                     'gb': np.ascontiguousarray(np.broadcast_to(gb, (128, 8))), 'cosT': cosT, 'sinT': sinT,
                     'ng': np.ascontiguousarray(np.broadcast_to(ml_norm_g[0][2 * g * 128:(2 * g + 2) * 128], (128, 256))),
                     'tri': tri, 'ident': ident})
    return maps

def ml_assemble(results):
    out = np.zeros((2, 8192, 1024), np.float32)
    for c, r in enumerate(results):
        b, g = c // 4, c % 4
        out[b][:, 2 * g * 128:(2 * g + 2) * 128] = r['mix']
    return out

def _na_bias_index(h, i, j):
    if 2 <= i <= 61:
        return h * 5 + (j - i + 2)
    e = {0: 0, 1: 1, 62: 2, 63: 3}[i]
    jb = j if i < 2 else j - 60
    return 10 + (e * 4 + jb) * 2 + h

def _na_bias_tile(rpb_h, i, j):
    kr = np.arange(2)[:, None, None, None]; jk = np.arange(64)[None, :, None, None]
    qr = np.arange(2)[None, None, :, None]; jq = np.arange(64)[None, None, None, :]
    kr_abs = 2 * j + kr; r = 2 * i + qr
    rs = np.clip(r - 4, 0, 120)
    valid = (kr_abs >= rs) & (kr_abs < rs + 8)
    cs = np.clip(jq - 8, 0, 48)
    valid = valid & (jk >= cs) & (jk < cs + 16)
    ro = np.clip(kr_abs - r + 7, 0, 14); co = np.clip(jk - jq, -15, 15) + 15
    ro, co, valid = np.broadcast_arrays(ro, co, valid)
    val = rpb_h[ro, co]
    return np.where(valid, val, np.float32(-30000.0)).astype(np.float32).reshape(128, 128)

def ev_maps(x, ctx, mods, inp, l=0):
    W = inp['ev_w_in'][0]; mu = inp['rw_mu'][0]
    s_ = np.arange(128)
    tri = np.zeros((2, 2, 128, 128), np.float32)
    tri[0, 0] = s_[:, None] <= s_[None, :]; tri[0, 1] = s_[:, None] < s_[None, :]
    tri[1, 0] = s_[:, None] >= s_[None, :]; tri[1, 1] = s_[:, None] > s_[None, :]
    ident = np.eye(128, dtype=np.float32)
    maps = []
    for c in range(8):
        b, g = c // 4, c % 4
        hc = np.arange(2 * g * 64, (2 * g + 2) * 64)
        cols = np.concatenate([1536 + hc, 2048 + hc, 2560 + hc, np.arange(3072, 3200), np.arange(3200, 3296), hc, 512 + hc, 1024 + hc])
        mucols = np.concatenate([hc, 512 + hc, 1024 + hc, np.arange(1536, 1664), np.arange(1664, 1760)])
        rep = np.concatenate([inp['rw_w0'][0][0][hc], inp['rw_w0'][0][1][hc], inp['rw_a0'][0][0][hc], inp['rw_a0'][0][1][hc],
                              inp['rw_k_k'][0][hc], inp['rw_k_a'][0][hc], inp['rw_r_k'][0].reshape(-1)[hc],
                              inp['rw_gn_g'][0][hc], inp['rw_gn_b'][0][hc]])
        UP = np.zeros((128, 512), np.float32)
        UP[0:32, 0:128] = inp['rw_w_up'][0][0][:, hc]; UP[32:64, 128:256] = inp['rw_w_up'][0][1][:, hc]
        UP[64:96, 256:384] = inp['rw_a_up'][0][0][:, hc]; UP[96:128, 384:512] = inp['rw_a_up'][0][1][:, hc]
        nab = np.zeros((42, 128, 128), np.float32)
        for h in range(2):
            rp = inp['na_rpb'][0][2 * g + h]
            for o in range(-2, 3):
                nab[_na_bias_index(h, 10, 10 + o)] = _na_bias_tile(rp, 10, 10 + o)
            for i in (0, 1):
                for j in range(4):
                    nab[_na_bias_index(h, i, j)] = _na_bias_tile(rp, i, j)
            for i in (62, 63):
                for j in range(60, 64):
                    nab[_na_bias_index(h, i, j)] = _na_bias_tile(rp, i, j)
        hfm = to_fm(np.concatenate([ctx[b], x[b]], 0))
        hpad = np.zeros((128, 8, hfm.shape[2] + 2), np.float32); hpad[:, :, 1:-1] = hfm
        vec = np.stack([vecfm(mods[l, b, 1]), vecfm(mods[l, b, 0]), vecfm(mods[l, 2, 1]), vecfm(mods[l, 2, 0])], -1)
        maps.append({'hT': hpad, 'vec': np.ascontiguousarray(vec), 'WA': np.ascontiguousarray(W[:, cols].reshape(8, 128, 992).transpose(1, 0, 2)),
                     'mu': np.ascontiguousarray(np.broadcast_to(mu[mucols], (128, 608))), 'rep': np.ascontiguousarray(np.broadcast_to(rep, (128, 1152))),
                     'UP': UP, 'GUP': np.ascontiguousarray(np.concatenate([inp['rw_g_up'][0][:, hc], np.zeros((32, 128), np.float32)], 0)), 'tri': tri, 'ident': ident, 'nab': nab})
    return maps

def ev_assemble(results):
    lat = np.zeros((2, 8192, 1024), np.float32); ctx = np.zeros((2, 256, 1024), np.float32)
    for c, r in enumerate(results):
        b, g = c // 4, c % 4
        m = r['mix']
        for dst, rows in ((ctx, slice(0, 256)), (lat, slice(256, 8448))):
            dst[b][:, 2 * g * 64:(2 * g + 2) * 64] = m[rows, 0:128]
            dst[b][:, 512 + 2 * g * 64:512 + (2 * g + 2) * 64] = m[rows, 128:256]
    return lat, ctx


_NC_CACHE = {}


def _get(name, fn):
    if name not in _NC_CACHE:
        _NC_CACHE[name] = fn()
    return _NC_CACHE[name]


def _run(nc, maps):
    return run_bass_kernel_spmd(nc, maps, core_ids=list(range(8))).results


def _ffn_layer(l, last, inputs, mods, h_lat, h_ctx, mix_lat, mix_ctx, w_out):
    TL = 2048
    TCX = 0 if last else 64
    T = TL + TCX
    segs = [(0, TL, 0)] + ([] if last else [(TL, T, 1)])
    woutL = np.ascontiguousarray(w_out.reshape(8, 128, 1024).transpose(1, 0, 2))
    routerL = np.ascontiguousarray(inputs['moe_router'][l].reshape(8, 128, 256).transpose(1, 0, 2))
    rbias = np.ascontiguousarray(np.broadcast_to(inputs['moe_bias'][l], (128, 256)))
    maps = []
    for c in range(8):
        b = c // 4; s = (c % 4) * TL; cs = (c % 4) * 64
        if last:
            xs = h_lat[b, s:s + TL]; ms = mix_lat[b, s:s + TL]
        else:
            xs = np.concatenate([h_lat[b, s:s + TL], h_ctx[b, cs:cs + 64]], 0)
            ms = np.concatenate([mix_lat[b, s:s + TL], mix_ctx[b, cs:cs + 64]], 0)
        vec = np.zeros((128, 8, 8), np.float32)
        vec[:, :, 0] = vecfm(inputs['ln_g'][l, 0]); vec[:, :, 1] = vecfm(inputs['ln_b'][l, 0])
        for m, j in ((0, b), (1, 2)):
            vec[:, :, 2 + 3 * m] = vecfm(mods[l, j, 2]); vec[:, :, 3 + 3 * m] = vecfm(mods[l, j, 3]); vec[:, :, 4 + 3 * m] = vecfm(mods[l, j, 4])
        maps.append({'xT': to_fm(xs), 'mixT': to_fm(ms), 'wout': woutL, 'vec': vec, 'router': routerL, 'rbias': rbias})
    pre = _run(_get(('pre', T), lambda: build_pre(T, segs)), maps)
    fT_all = np.concatenate([pre[c]['fT'] for c in range(8)], axis=2)
    Wt_all = np.concatenate([pre[c]['Wt'] for c in range(8)], axis=0)
    T_all = 8 * T
    wm = moe_weight_maps(l, inputs['moe_w_gate'], inputs['moe_w_up'], inputs['moe_w_down'])
    shm = shared_maps(l, inputs['sh_w_gate'], inputs['sh_w_up'], inputs['sh_w_down'])
    wt = moe_wt_maps(Wt_all)
    moe = _run(_get(('moe', T_all), lambda: build_moe(T_all, T)), [dict(wm[c], fT=fT_all, WtT=wt[c]) for c in range(8)])
    del wm
    parts = np.stack([moe[c]['part'] for c in range(8)])
    maps = []
    for c in range(8):
        b = c // 4
        vec = np.zeros((128, 8, 4), np.float32)
        vec[:, :, 0] = vecfm(inputs['ln_g'][l, 1]); vec[:, :, 1] = vecfm(inputs['ln_b'][l, 1])
        vec[:, :, 2] = vecfm(mods[l, b, 5]); vec[:, :, 3] = vecfm(mods[l, 2, 5])
        maps.append(dict(shm, h1T=pre[c]['h1T'], parts=np.ascontiguousarray(parts[:, :, :, c * T:(c + 1) * T]), vec=vec, fT=pre[c]['fT']))
    post = _run(_get(('post', T), lambda: build_post(T, segs)), maps)
    o_lat = np.zeros((2, 8192, 1024), np.float32)
    o_ctx = None if last else np.zeros((2, 256, 1024), np.float32)
    for c in range(8):
        b = c // 4; s = (c % 4) * TL; cs = (c % 4) * 64
        h2 = from_fm(post[c]['h2T'])
        o_lat[b, s:s + TL] = h2[:TL]
        if not last:
            o_ctx[b, cs:cs + 64] = h2[TL:]
    return o_lat, o_ctx


def kernel(**inputs):
    inputs = {k: np.asarray(v) for k, v in inputs.items()}
    x, ctx = inputs['x'], inputs['ctx']
    mods = mods_assemble(_run(_get('mods', build_mods), mods_inputs(inputs['c'], inputs['c_ctx'], inputs['ada_w'], inputs['ada_b'])))
    ev = _run(_get('ev', build_ev), ev_maps(x, ctx, mods, inputs))
    mix_lat, mix_ctx = ev_assemble(ev)
    h_lat, h_ctx = _ffn_layer(0, False, inputs, mods, x, ctx, mix_lat, mix_ctx, inputs['ev_w_out'][0])
    ml = _run(_get('ml', build_ml), ml_maps(h_lat, h_ctx, mods, inputs['od_w_in'], inputs['ml_gate_b'], inputs['ml_norm_g']))
    mix_lat = ml_assemble(ml)
    h_lat, _ = _ffn_layer(1, True, inputs, mods, h_lat, None, mix_lat, None, inputs['od_w_out'][0])
    return h_lat.astype(np.float32)
```
